# Optimizing a Trainium2 kernel written in Bass

```python
import math
import jax, jax.numpy as jnp
from jax import lax
import numpy as np

D_MODEL = 1024
BATCH = 8
SEQ = 4096
DEPTH = 2

A_HEADS = 8
A_HEAD_DIM = 64
A_WIDTH = A_HEADS * A_HEAD_DIM
MOBA_BLOCK = 256
MOBA_TOPK = 3
MOBA_QCHUNK = 16
B_HEADS = 8
B_HEAD_DIM = 64
B_WIDTH = B_HEADS * B_HEAD_DIM
IDX_HEADS = 4
IDX_DIM = 64
DSA_TOPK_MAX = 256
DSA_QCHUNK = 128
REL_BUCKETS = 32
REL_MAX_DIST = 128
N_HEADS_TOTAL = A_HEADS + B_HEADS
EPS = 1e-6

SPLIT_SIZES = (A_WIDTH, A_WIDTH, A_WIDTH, A_WIDTH,
               B_WIDTH, B_HEAD_DIM, B_HEAD_DIM, B_WIDTH,
               IDX_HEADS * IDX_DIM, IDX_DIM, IDX_HEADS,
               D_MODEL, D_MODEL)
D_IN = 4 * A_WIDTH + 2 * B_WIDTH + 2 * B_HEAD_DIM + IDX_HEADS * IDX_DIM + IDX_DIM + IDX_HEADS + 2 * D_MODEL

kernel_name = "hybrid_moba_dsa_gated_block"


def _split_points():
    pts, acc = [], 0
    for s in SPLIT_SIZES[:-1]:
        acc += s
        pts.append(acc)
    return pts


def rmsnorm(x, g):
    x32 = x.astype(jnp.float32)
    r = lax.rsqrt(jnp.mean(x32 * x32, axis=-1, keepdims=True) + EPS)
    return (x32 * r).astype(x.dtype) * g


def rel_bucket(n):
    n = jnp.maximum(n, 0)
    max_exact = REL_BUCKETS // 2
    nf = jnp.maximum(n, 1).astype(jnp.float32)
    large = max_exact + (jnp.log(nf / max_exact) / math.log(REL_MAX_DIST / max_exact)
                         * (REL_BUCKETS - max_exact)).astype(jnp.int32)
    large = jnp.minimum(large, REL_BUCKETS - 1)
    return jnp.where(n < max_exact, n, large)


def moba_attention(q, k, v, bias_table):
    B, S, H, Dh = q.shape
    nb = -(-S // MOBA_BLOCK)
    sp = nb * MOBA_BLOCK
    ntop = min(MOBA_TOPK, nb)
    pad = ((0, 0), (0, sp - S), (0, 0), (0, 0))
    kb = jnp.pad(k, pad).reshape(B, nb, MOBA_BLOCK, H, Dh)
    vb = jnp.pad(v, pad).reshape(B, nb, MOBA_BLOCK, H, Dh)
    kmean = jnp.mean(kb.astype(jnp.float32), axis=2).astype(k.dtype)
    kbh = kb.transpose(0, 3, 1, 2, 4)
    vbh = vb.transpose(0, 3, 1, 2, 4)
    scale = Dh ** -0.5
    nc = S // MOBA_QCHUNK
    qc_all = q.reshape(B, nc, MOBA_QCHUNK, H, Dh).transpose(1, 0, 2, 3, 4)
    bi = jnp.arange(B)[:, None, None, None]
    hi = jnp.arange(H)[None, :, None, None]
    offs = jnp.arange(MOBA_BLOCK)

    def chunk(args):
        ci, qi = args
        pos = ci * MOBA_QCHUNK + jnp.arange(MOBA_QCHUNK)
        qblk = (ci * MOBA_QCHUNK) // MOBA_BLOCK
        qh = qi.transpose(0, 2, 1, 3)
        gate = jnp.einsum('bhqd,bnhd->bhqn', qh, kmean).astype(jnp.float32)
        gate = jnp.where(jnp.arange(nb) < qblk, gate, -jnp.inf)
        _, sel = lax.top_k(gate, ntop)
        valid = sel < qblk
        kg = kbh[bi, hi, sel]
        vg = vbh[bi, hi, sel]
        keypos = sel[..., None] * MOBA_BLOCK + offs
        dist = pos[None, None, :, None, None] - keypos
        b_past = bias_table[rel_bucket(dist), jnp.arange(H)[None, :, None, None, None]]
        lp = (jnp.einsum('bhqd,bhqjkd->bhqjk', qh, kg) * scale).astype(jnp.float32) + b_past.astype(jnp.float32)
        lp = jnp.where(valid[..., None], lp, -jnp.inf)
        kown = lax.dynamic_index_in_dim(kbh, qblk, axis=2, keepdims=False)
        vown = lax.dynamic_index_in_dim(vbh, qblk, axis=2, keepdims=False)
        dist_o = pos[:, None] - (qblk * MOBA_BLOCK + offs)[None, :]
        b_own = bias_table[rel_bucket(dist_o)].transpose(2, 0, 1)
        lo = (jnp.einsum('bhqd,bhkd->bhqk', qh, kown) * scale).astype(jnp.float32) + b_own.astype(jnp.float32)
        lo = jnp.where(dist_o >= 0, lo, -jnp.inf)
        logits = jnp.concatenate([lp.reshape(B, H, MOBA_QCHUNK, ntop * MOBA_BLOCK), lo], axis=-1)
        p = jax.nn.softmax(logits, axis=-1).astype(v.dtype)
        pp = p[..., :ntop * MOBA_BLOCK].reshape(B, H, MOBA_QCHUNK, ntop, MOBA_BLOCK)
        po = p[..., ntop * MOBA_BLOCK:]
        return (jnp.einsum('bhqjk,bhqjkd->bqhd', pp, vg)
                + jnp.einsum('bhqk,bhkd->bqhd', po, vown))

    out = lax.map(chunk, (jnp.arange(nc), qc_all))
    return out.transpose(1, 0, 2, 3, 4).reshape(B, S, H, Dh)


def dsa_attention(q, k, v, q_idx, k_idx, w_idx, bias_table):
    B, S, H, Dh = q.shape
    topk = min(DSA_TOPK_MAX, S // 4)
    nc = S // DSA_QCHUNK
    scale = Dh ** -0.5
    idx_scale = IDX_DIM ** -0.5
    w_scale = IDX_HEADS ** -0.5
    qc_all = q.reshape(B, nc, DSA_QCHUNK, H, Dh).transpose(1, 0, 2, 3, 4)
    qic_all = q_idx.reshape(B, nc, DSA_QCHUNK, IDX_HEADS, IDX_DIM).transpose(1, 0, 2, 3, 4)
    wic_all = w_idx.reshape(B, nc, DSA_QCHUNK, IDX_HEADS).transpose(1, 0, 2, 3)
    bi = jnp.arange(B)[:, None, None]
    key_pos = jnp.arange(S)

    def chunk(args):
        ci, qc, qic, wic = args
        pos = ci * DSA_QCHUNK + jnp.arange(DSA_QCHUNK)
        rel = jax.nn.relu(jnp.einsum('bqhd,bsd->bqhs', qic, k_idx) * idx_scale)
        score = jnp.einsum('bqhs,bqh->bqs', rel, wic * w_scale).astype(jnp.float32)
        score = jnp.where(key_pos[None, None, :] <= pos[None, :, None], score, -jnp.inf)
        _, sel = lax.top_k(score, topk)
        valid = sel <= pos[None, :, None]
        kg = k[bi, sel]
        vg = v[bi, sel]
        dist = pos[None, :, None] - sel
        bias = bias_table[rel_bucket(dist)].transpose(0, 3, 1, 2)
        logits = (jnp.einsum('bqhd,bqkd->bhqk', qc, kg) * scale).astype(jnp.float32) + bias.astype(jnp.float32)
        logits = jnp.where(valid[:, None], logits, -jnp.inf)
        p = jax.nn.softmax(logits, axis=-1).astype(v.dtype)
        return jnp.einsum('bhqk,bqkd->bqhd', p, vg)

    out = lax.map(chunk, (jnp.arange(nc), qc_all, qic_all, wic_all))
    return out.transpose(1, 0, 2, 3, 4).reshape(B, S, H, Dh)


def hybrid_layer(x, g_norm, w_in, qn_a, kn_a, qn_b, kn_b, w_br_a, w_br_b, w_out, rel_bias):
    B, S, _ = x.shape
    h = rmsnorm(x, g_norm)
    proj = jnp.einsum('bsd,de->bse', h, w_in)
    (qa, ka, va, ga, qb, kb, vb, gb, qi, ki, wi, ma, mb) = jnp.split(proj, _split_points(), axis=-1)
    qa = rmsnorm(qa.reshape(B, S, A_HEADS, A_HEAD_DIM), qn_a)
    ka = rmsnorm(ka.reshape(B, S, A_HEADS, A_HEAD_DIM), kn_a)
    va = va.reshape(B, S, A_HEADS, A_HEAD_DIM)
    ya = moba_attention(qa, ka, va, rel_bias[:, :A_HEADS]).reshape(B, S, A_WIDTH) * jax.nn.silu(ga)
    qb = rmsnorm(qb.reshape(B, S, B_HEADS, B_HEAD_DIM), qn_b)
    kb = rmsnorm(kb, kn_b)
    qi = qi.reshape(B, S, IDX_HEADS, IDX_DIM)
    yb = dsa_attention(qb, kb, vb, qi, ki, wi, rel_bias[:, A_HEADS:]).reshape(B, S, B_WIDTH) * jax.nn.silu(gb)
    merged = (jax.nn.sigmoid(ma) * jnp.einsum('bsw,wd->bsd', ya, w_br_a)
              + jax.nn.sigmoid(mb) * jnp.einsum('bsw,wd->bsd', yb, w_br_b))
    return x + jnp.einsum('bsd,de->bse', merged, w_out)


def setup_inputs(seed: int = 0) -> dict:
    key = jax.random.key(seed)
    ks = jax.random.split(key, 12)
    f32 = jnp.float32
    x = jax.random.normal(ks[0], (BATCH, SEQ, D_MODEL), f32)
    norm_g = 1.0 + 0.02 * jax.random.normal(ks[1], (DEPTH, D_MODEL), f32)
    w_in = jax.random.normal(ks[2], (DEPTH, D_MODEL, D_IN), f32) * D_MODEL ** -0.5
    q_norm_a = 1.0 + 0.02 * jax.random.normal(ks[3], (DEPTH, A_HEAD_DIM), f32)
    k_norm_a = 1.0 + 0.02 * jax.random.normal(ks[4], (DEPTH, A_HEAD_DIM), f32)
    q_norm_b = 1.0 + 0.02 * jax.random.normal(ks[5], (DEPTH, B_HEAD_DIM), f32)
    k_norm_b = 1.0 + 0.02 * jax.random.normal(ks[6], (DEPTH, B_HEAD_DIM), f32)
    w_branch_a = jax.random.normal(ks[7], (DEPTH, A_WIDTH, D_MODEL), f32) * A_WIDTH ** -0.5
    w_branch_b = jax.random.normal(ks[8], (DEPTH, B_WIDTH, D_MODEL), f32) * B_WIDTH ** -0.5
    w_out = jax.random.normal(ks[9], (DEPTH, D_MODEL, D_MODEL), f32) * D_MODEL ** -0.5
    rel_bias = 0.5 * jax.random.normal(ks[10], (REL_BUCKETS, N_HEADS_TOTAL), f32)
    return {"x": x, "norm_g": norm_g, "w_in": w_in,
            "q_norm_a": q_norm_a, "k_norm_a": k_norm_a,
            "q_norm_b": q_norm_b, "k_norm_b": k_norm_b,
            "w_branch_a": w_branch_a, "w_branch_b": w_branch_b,
            "w_out": w_out, "rel_bias": rel_bias}


def reference(x, norm_g, w_in, q_norm_a, k_norm_a, q_norm_b, k_norm_b,
              w_branch_a, w_branch_b, w_out, rel_bias):
    h = x
    for l in range(DEPTH):
        h = hybrid_layer(h, norm_g[l], w_in[l], q_norm_a[l], k_norm_a[l], q_norm_b[l], k_norm_b[l],
                         w_branch_a[l], w_branch_b[l], w_out[l], rel_bias)
    return h
```

```python
import math
from contextlib import ExitStack

import numpy as np
import concourse.bass as bass
import concourse.mybir as mybir
from concourse.bass_utils import run_bass_kernel_spmd

F32 = mybir.dt.float32
BF16 = mybir.dt.bfloat16
ALU = mybir.AluOpType
AF = mybir.ActivationFunctionType
AX = mybir.AxisListType

S = 4096
D = 1024
NT = S // 128
DIN = 5572
DEPTH = 2
EPS = 1e-6
NEG = -30000.0
QS_W = 4352
NBIS = 22
W0 = 1024.0
TOPK = 256.0
MID0 = 2.0 ** -13
ACT_SHARE_NUM, ACT_SHARE_DEN = 3, 8

C_QA, C_KA, C_VA, C_GA = 0, 512, 1024, 1536
C_QB, C_KB, C_VB, C_GB = 2048, 2560, 2624, 2688
C_QI, C_KI, C_WI, C_MA, C_MB = 3200, 3456, 3520, 3524, 4548


class T:
    __slots__ = ("w", "r")

    def __init__(self):
        self.w = {}
        self.r = {}


class Ctx:
    RING = 8

    def __init__(self, nc, stack):
        self.nc = nc
        self.eng = {"pe": nc.tensor, "act": nc.scalar, "dve": nc.vector, "pool": nc.gpsimd, "sp": nc.sync}
        self.sem = {}
        self.cnt = {}
        for k in ("pe", "act", "dve", "pool"):
            self.sem[k] = stack.enter_context(nc.semaphore("s_" + k))
            self.cnt[k] = 0
        self.waited = {k: {} for k in self.eng}
        self.rings = {}
        self.ringpos = {}
        for q in ("sp", "pool"):
            self.rings[q] = []
            for i in range(self.RING):
                key = "d_%s_%d" % (q, i)
                self.sem[key] = stack.enter_context(nc.semaphore(key))
                self.cnt[key] = 0
                self.rings[q].append(key)
            self.ringpos[q] = 0

    def _wait(self, e, marks):
        need = {}
        for (k, v) in marks:
            if need.get(k, 0) < v:
                need[k] = v
        wd = self.waited[e]
        for k, v in need.items():
            if wd.get(k, 0) < v:
                self.eng[e].wait_ge(self.sem[k], v)
                wd[k] = v

    def _deps(self, e, reads, writes):
        marks = []
        for t in reads:
            for m in t.w.items():
                if m[0] == e and e == "pe":
                    continue
                marks.append(m)
        for t in writes:
            for m in t.w.items():
                if m[0] == e:
                    continue
                marks.append(m)
            for m in t.r.items():
                if m[0] == e:
                    continue
                marks.append(m)
        return marks

    def op(self, e, fn, reads=(), writes=(), wadd=()):
        self._wait(e, self._deps(e, reads, tuple(writes) + tuple(wadd)))
        ins = fn(self.eng[e])
        self.cnt[e] += 1
        ins.then_inc(self.sem[e], 1)
        v = self.cnt[e]
        for t in reads:
            t.r[e] = v
        for t in writes:
            t.w = {e: v}
            t.r = {}
        for t in wadd:
            t.w[e] = v
        return ins

    def dma(self, q, out, in_, reads=(), writes=(), wadd=(), **kw):
        key = self.rings[q][self.ringpos[q] % self.RING]
        self.ringpos[q] += 1
        marks = self._deps("dma", reads, tuple(writes) + tuple(wadd))
        if self.cnt[key] > 0:
            marks.append((key, self.cnt[key]))
        self._wait(q, marks)
        ins = self.eng[q].dma_start(out=out, in_=in_, **kw)
        self.cnt[key] += 16
        ins.then_inc(self.sem[key], 16)
        v = self.cnt[key]
        for t in reads:
            t.r[key] = v
        for t in writes:
            t.w = {key: v}
            t.r = {}
        for t in wadd:
            t.w[key] = v

    def all_marks(self):
        marks = []
        for k, v in self.cnt.items():
            if v > 0:
                marks.append((k, v))
        return marks

    def barrier(self):
        marks = self.all_marks()
        for e in ("pe", "act", "dve", "pool", "sp"):
            self._wait(e, [m for m in marks if m[0] != e])

    def finish(self):
        self._wait("sp", self.all_marks())


def _bucket_table():
    n = np.arange(0, 256)
    max_exact = 16
    nf = np.maximum(n, 1).astype(np.float32)
    large = max_exact + (np.log(nf / np.float32(max_exact)) / np.float32(math.log(128 / max_exact))
                         * np.float32(32 - max_exact)).astype(np.int32)
    large = np.minimum(large, 31)
    return np.where(n < max_exact, n, large)


def _host_consts():
    bk = _bucket_table()
    grev = np.zeros((33, 384), np.float32)
    for m in range(384):
        dist = 255 - m
        if dist >= 0:
            grev[bk[dist], m] = 1.0
        else:
            grev[32, m] = 1.0
    ident = np.eye(128, dtype=np.float32)
    irep = np.concatenate([ident] * 4, axis=1)
    t = np.arange(128)[:, None]
    s = np.arange(128)[None, :]
    causalneg = np.where(s <= t, 0.0, -1e30).astype(np.float32)
    eall = np.zeros((16, 16, 128), np.float32)
    for n in range(16):
        eall[n, n, :] = 1.0
    negrow = np.full((1, 16), NEG * 8.0, np.float32)
    return {"c_grev": grev, "c_ident": ident, "c_irep": irep, "c_causal": causalneg,
            "c_eall": eall.reshape(16, 2048), "c_negrow": negrow}


def build_program():
    nc = bass.Bass("TRN2", target_bir_lowering=False)
    din = lambda n, s, d=F32: nc.dram_tensor(n, s, d, kind="ExternalInput").ap()
    x = din("x", [S, D])
    norm_g = din("norm_g", [DEPTH, D])
    w_in = din("w_in", [DEPTH, D, DIN])
    qn_a = din("q_norm_a", [DEPTH, 64])
    kn_a = din("k_norm_a", [DEPTH, 64])
    qn_b = din("q_norm_b", [DEPTH, 64])
    kn_b = din("k_norm_b", [DEPTH, 64])
    w_bra = din("w_branch_a", [DEPTH, 512, D])
    w_brb = din("w_branch_b", [DEPTH, 512, D])
    w_out = din("w_out", [DEPTH, D, D])
    rel_bias = din("rel_bias", [32, 16])
    c_grev = din("c_grev", [33, 384])
    c_ident = din("c_ident", [128, 128])
    c_irep = din("c_irep", [128, 512])
    c_causal = din("c_causal", [128, 128])
    c_eall = din("c_eall", [16, 2048])
    c_negrow = din("c_negrow", [1, 16])
    out = nc.dram_tensor("out", [S, D], F32, kind="ExternalOutput").ap()
    scr = lambda n, s, d: nc.dram_tensor(n, s, d, kind="Internal").ap()
    qs = scr("qs", [S, QS_W], BF16)
    wis = scr("wis", [S, 4], F32)
    kaT_s = scr("kaT_s", [4, 128, S], BF16)
    x1 = scr("x1", [S, D], F32)

    top = ExitStack()
    with top:
        c = Ctx(nc, top)

        uid = [0]

        def sbt(st, name, shape, dt):
            uid[0] += 1
            return st.enter_context(nc.sbuf_tensor("%s_%d" % (name, uid[0]), shape, dt))

        def pst(st, name, shape, dt):
            uid[0] += 1
            return st.enter_context(nc.psum_tensor("%s_%d" % (name, uid[0]), shape, dt))

        ident_bf = sbt(top, "ident_bf", [128, 128], BF16); t_ident = T()
        irep_bf = sbt(top, "irep_bf", [128, 512], BF16); t_irep = T()
        causal = sbt(top, "causal", [128, 128], F32); t_causal = T()
        eall_bf = sbt(top, "eall_bf", [128, 2048], BF16); t_eall = T()
        bimg_hi = sbt(top, "bimg_hi", [128, 16, 256], BF16)
        t_bimg = T()
        v1a = sbt(top, "v1a", [128, NT, 8, 65], BF16); t_v1a = [T() for _ in range(NT)]
        v1b = sbt(top, "v1b", [128, NT, 65], BF16); t_v1b = [T() for _ in range(NT)]
        kbT = sbt(top, "kbT", [128, S], BF16); t_kbT = [T() for _ in range(NT)]
        kiT = sbt(top, "kiT", [128, S], BF16); t_kiT = [T() for _ in range(NT)]
        kmacc = sbt(top, "kmacc", [128, 4, 16], F32); t_kmacc = T()

        c.dma("pool", ident_bf[:], c_ident, writes=[t_ident])
        c.dma("pool", irep_bf[:], c_irep, writes=[t_irep])
        c.op("dve", lambda e: e.memset(eall_bf[:, :], 0.0), writes=[t_eall])
        c.dma("pool", eall_bf[0:16, :], c_eall, writes=[t_eall])
        t_kz = T()
        c.op("dve", lambda e: e.memset(kbT[64:128, :], 0.0), writes=[t_kz])
        c.op("dve", lambda e: e.memset(kiT[64:128, :], 0.0), writes=[t_kz])
        c.dma("sp", causal[:], c_causal, writes=[t_causal])
        t_ones = T()
        c.op("dve", lambda e: e.memset(v1a[:, :, :, 64:65], 1.0), writes=[t_ones])
        c.op("dve", lambda e: e.memset(v1b[:, :, 64:65], 1.0), writes=[t_ones])

        with ExitStack() as st:
            grev = sbt(st, "grev", [33, 384], F32); t_grev = T()
            reltab = sbt(st, "reltab", [33, 16], F32); t_rel = T()
            r31 = sbt(st, "r31", [32, 16], F32); t_r31 = T()
            bps = pst(st, "bps", [128, 2048], F32); t_bps = T()
            c.dma("sp", grev[:], c_grev, writes=[t_grev])
            c.dma("sp", reltab[0:32, :], rel_bias, writes=[t_rel])
            c.dma("sp", r31[:], rel_bias[31:32, :].to_broadcast([32, 16]), writes=[t_r31])
            c.op("dve", lambda e: e.tensor_tensor(out=reltab[0:32, :], in0=reltab[0:32, :], in1=r31[:], op=ALU.subtract),
                 reads=[t_r31, t_rel], writes=[t_rel])
            c.op("dve", lambda e: e.tensor_scalar(out=reltab[0:32, :], in0=reltab[0:32, :], scalar1=8.0, scalar2=None, op0=ALU.mult),
                 reads=[t_rel], writes=[t_rel])
            c.dma("sp", reltab[32:33, :], c_negrow, reads=[t_rel], writes=[t_rel])
            for half in range(2):
                for el in range(128):
                    e_ = half * 128 + el
                    c.op("pe", lambda e, el=el, e_=e_: e.matmul(bps[:, el * 16:(el + 1) * 16], lhsT=grev[0:33, 255 - e_:255 - e_ + 128],
                                                                 rhs=reltab[0:33, :], start=True, stop=True),
                         reads=[t_grev, t_rel], writes=[t_bps])
                src = bps[:, :].rearrange("p (e h) -> p h e", h=16)
                hi_v = bimg_hi[:, :, half * 128:(half + 1) * 128]
                c.op("dve", lambda e: e.tensor_copy(out=hi_v, in_=src), reads=[t_bps], wadd=[t_bimg])
            c.barrier()

        for l in range(DEPTH):
            x_src = x if l == 0 else x1
            o_dst = x1 if l == 0 else out
            with ExitStack() as st:
                w_bf = sbt(st, "w_bf", [128, 8, DIN], BF16); t_w = [T() for _ in range(8)]
                gbc = sbt(st, "gbc", [128, D], F32); t_gbc = T()
                gq_a = sbt(st, "gq_a", [128, 64], F32)
                gk_a = sbt(st, "gk_a", [128, 64], F32)
                gq_b = sbt(st, "gq_b", [128, 64], F32)
                gk_b = sbt(st, "gk_b", [128, 64], F32)
                t_gs = T()
                xb = [sbt(st, "xb%d" % i, [128, D], F32) for i in range(2)]; t_xb = [T(), T()]
                sqj = sbt(st, "sqj", [128, D], BF16); t_sqj = T()
                stat = sbt(st, "stat", [128, 8], F32); t_stat = T()
                hb = sbt(st, "hb", [128, D], BF16); t_hb = T()
                hT = [sbt(st, "hT%d" % i, [128, 8, 128], BF16) for i in range(2)]; t_hT = [T(), T()]
                qt = [sbt(st, "qt%d" % i, [128, QS_W], BF16) for i in range(2)]; t_qt = [T(), T()]
                sqf = sbt(st, "sqf", [128, 512], F32); t_sqf = T()
                hst = sbt(st, "hst", [128, 32], F32); t_hst = T()
                tmpf = sbt(st, "tmpf", [128, 512], F32); t_tmpf = T()
                kan = sbt(st, "kan", [128, 512], BF16); t_kan = T()
                kTt = [sbt(st, "kTt%d" % i, [128, 4, 128], BF16) for i in range(2)]; t_kTt = [T(), T()]
                kred = sbt(st, "kred", [128, 4], F32); t_kred = T()
                sm = sbt(st, "sm", [128, 128], BF16); t_sm = T()
                wit = [sbt(st, "witp%d" % i, [128, 4], F32) for i in range(2)]; t_wit = [T(), T()]
                tp = pst(st, "tp", [128, 1024], BF16); t_tp = T()
                tp2 = pst(st, "tp2", [128, 1024], BF16); t_tp2 = T()
                pj = [pst(st, "pj%d" % i, [128, 512], F32) for i in range(3)]; t_pj = [T(), T(), T()]

                for kc in range(8):
                    for (c0, c1) in ((0, 2048), (2048, 4096), (4096, DIN)):
                        c.dma("pool", w_bf[:, kc, c0:c1], w_in[l, kc * 128:(kc + 1) * 128, c0:c1], wadd=[t_w[kc]])
                c.dma("sp", gbc[:], norm_g[l:l + 1, :].to_broadcast([128, D]), writes=[t_gbc])
                for (g_sb, g_dr) in ((gq_a, qn_a), (gk_a, kn_a), (gq_b, qn_b), (gk_b, kn_b)):
                    c.dma("sp", g_sb[:], g_dr[l:l + 1, :].to_broadcast([128, 64]), wadd=[t_gs])
                c.op("dve", lambda e: e.memset(kmacc[:], 0.0), writes=[t_kmacc])

                pjn = [0]

                def project(i, col0, ncols):
                    b = pjn[0] % 3
                    pjn[0] += 1
                    for kc in range(8):
                        c.op("pe", lambda e, kc=kc: e.matmul(pj[b][:, 0:ncols], lhsT=hT[i % 2][:, kc, :],
                                                              rhs=w_bf[:, kc, col0:col0 + ncols],
                                                              start=(kc == 0), stop=(kc == 7)),
                             reads=[t_hT[i % 2], t_w[kc]], writes=[t_pj[b]])
                    return pj[b], t_pj[b]

                def rsqrt_small(ap_in, ap_out, n, inv_n):
                    c.op("dve", lambda e: e.tensor_scalar(out=ap_out, in0=ap_in, scalar1=inv_n, scalar2=EPS,
                                                          op0=ALU.mult, op1=ALU.add), reads=[t_hst], writes=[t_hst])
                    c.op("act", lambda e: e.activation(out=ap_out, in_=ap_out, func=AF.Ln), reads=[t_hst], writes=[t_hst])
                    c.op("act", lambda e: e.activation(out=ap_out, in_=ap_out, func=AF.Exp, scale=-0.5),
                         reads=[t_hst], writes=[t_hst])

                def headnorm(ps_ap, t_ps, H, g_sb, out_ap, t_out, wadd_out=False):
                    W = H * 64
                    c.op("act", lambda e: e.activation(out=sqf[:, 0:W], in_=ps_ap, func=AF.Square),
                         reads=[t_ps], writes=[t_sqf])
                    c.op("dve", lambda e: e.tensor_reduce(out=hst[:, 0:H], in_=sqf[:, 0:W].rearrange("p (h d) -> p h d", d=64),
                                                          axis=AX.X, op=ALU.add), reads=[t_sqf], writes=[t_hst])
                    rsqrt_small(hst[:, 0:H], hst[:, 0:H], H, 1.0 / 64.0)
                    c.op("dve", lambda e: e.tensor_tensor(out=tmpf[:, 0:W].rearrange("p (h d) -> p h d", d=64),
                                                          in0=ps_ap.rearrange("p (h d) -> p h d", d=64),
                                                          in1=hst[:, 0:H].unsqueeze(2).to_broadcast([128, H, 64]), op=ALU.mult),
                         reads=[t_ps, t_hst], writes=[t_tmpf])
                    kw = dict(wadd=[t_out]) if wadd_out else dict(writes=[t_out])
                    c.op("dve", lambda e: e.tensor_tensor(out=out_ap.rearrange("p (h d) -> p h d", d=64),
                                                          in0=tmpf[:, 0:W].rearrange("p (h d) -> p h d", d=64),
                                                          in1=g_sb[:, :].unsqueeze(1).to_broadcast([128, H, 64]), op=ALU.mult),
                         reads=[t_tmpf, t_gs], **kw)

                for i in range(NT):
                    rows = slice(i * 128, (i + 1) * 128)
                    xt, t_xt = xb[i % 2], t_xb[i % 2]
                    q, t_q = qt[i % 2], t_qt[i % 2]
                    c.dma("sp", xt[:], x_src[rows, :], writes=[t_xt])
                    c.op("act", lambda e: e.activation(out=sqj[:], in_=xt[:], func=AF.Square, accum_out=stat[:, 0:1]),
                         reads=[t_xt], writes=[t_sqj, t_stat])
                    c.op("dve", lambda e: e.tensor_scalar(out=stat[:, 1:2], in0=stat[:, 0:1], scalar1=1.0 / D, scalar2=EPS,
                                                          op0=ALU.mult, op1=ALU.add), reads=[t_stat], writes=[t_stat])
                    c.op("act", lambda e: e.activation(out=stat[:, 2:3], in_=stat[:, 1:2], func=AF.Ln), reads=[t_stat], writes=[t_stat])
                    c.op("act", lambda e: e.activation(out=stat[:, 3:4], in_=stat[:, 2:3], func=AF.Exp, scale=-0.5),
                         reads=[t_stat], writes=[t_stat])
                    c.op("dve", lambda e: e.scalar_tensor_tensor(out=hb[:], in0=xt[:], scalar=stat[:, 3:4], in1=gbc[:],
                                                                 op0=ALU.mult, op1=ALU.mult),
                         reads=[t_xt, t_stat, t_gbc], writes=[t_hb])
                    for kc in range(8):
                        c.op("pe", lambda e, kc=kc: e.transpose(tp[:, kc * 128:(kc + 1) * 128], hb[:, kc * 128:(kc + 1) * 128], ident_bf[:]),
                             reads=[t_hb, t_ident], writes=[t_tp])
                    c.op("act", lambda e: e.activation(out=hT[i % 2][:, :, :], in_=tp[:, :].rearrange("p (k t) -> p k t", t=128), func=AF.Copy),
                         reads=[t_tp], writes=[t_hT[i % 2]])
                    ps, tps = project(i, C_QA, 512)
                    headnorm(ps[:, 0:512], tps, 8, gq_a, q[:, 0:512], t_q)
                    ps, tps = project(i, C_KA, 512)
                    headnorm(ps[:, 0:512], tps, 8, gk_a, kan[:, :], t_kan)
                    kt, t_kt = kTt[i % 2], t_kTt[i % 2]
                    for p4 in range(4):
                        c.op("pe", lambda e, p4=p4: e.transpose(tp2[:, p4 * 128:(p4 + 1) * 128], kan[:, p4 * 128:(p4 + 1) * 128], ident_bf[:]),
                             reads=[t_kan, t_ident], writes=[t_tp2])
                    c.op("act", lambda e: e.activation(out=kt[:, :, :], in_=tp2[:, 0:512].rearrange("p (k t) -> p k t", t=128), func=AF.Copy),
                         reads=[t_tp2], writes=[t_kt])
                    for p4 in range(4):
                        c.dma("pool", kaT_s[p4, :, i * 128:(i + 1) * 128], kt[:, p4, :], reads=[t_kt])
                    c.op("dve", lambda e: e.tensor_reduce(out=kred[:, :], in_=kt[:, :, :], axis=AX.X, op=ALU.add),
                         reads=[t_kt], writes=[t_kred])
                    nblk = i // 2
                    c.op("dve", lambda e: e.tensor_tensor(out=kmacc[:, :, nblk], in0=kmacc[:, :, nblk], in1=kred[:, :], op=ALU.add),
                         reads=[t_kred, t_kmacc], writes=[t_kmacc])
                    ps, tps = project(i, C_QB, 512)
                    headnorm(ps[:, 0:512], tps, 8, gq_b, q[:, 512:1024], t_q, wadd_out=True)
                    ps, tps = project(i, C_KB, 128)
                    headnorm(ps[:, 0:64], tps, 1, gk_b, sm[:, 0:64], t_sm)
                    c.op("dve", lambda e: e.tensor_copy(out=v1b[:, i, 0:64], in_=ps[:, 64:128]), reads=[tps], writes=[t_v1b[i]])
                    ps2, tps2 = project(i, C_QI, 324)
                    c.op("dve", lambda e: e.tensor_copy(out=q[:, 4096:4352], in_=ps2[:, 0:256]), reads=[tps2], wadd=[t_q])
                    c.op("dve", lambda e: e.tensor_copy(out=sm[:, 64:128], in_=ps2[:, 256:320]), reads=[tps2], wadd=[t_sm])
                    c.op("dve", lambda e: e.tensor_copy(out=wit[i % 2][:, :], in_=ps2[:, 320:324]), reads=[tps2], writes=[t_wit[i % 2]])
                    c.dma("pool", wis[rows, :], wit[i % 2][:, :], reads=[t_wit[i % 2]])
                    c.op("pe", lambda e: e.transpose(tp2[0:64, 512:640], sm[:, 0:64], ident_bf[:]), reads=[t_sm, t_ident], writes=[t_tp2])
                    c.op("pe", lambda e: e.transpose(tp2[0:64, 640:768], sm[:, 64:128], ident_bf[:]), reads=[t_sm, t_ident], writes=[t_tp2])
                    c.op("act", lambda e: e.activation(out=kbT[0:64, i * 128:(i + 1) * 128], in_=tp2[0:64, 512:640], func=AF.Copy),
                         reads=[t_tp2], writes=[t_kbT[i]])
                    c.op("act", lambda e: e.activation(out=kiT[0:64, i * 128:(i + 1) * 128], in_=tp2[0:64, 640:768], func=AF.Copy),
                         reads=[t_tp2], writes=[t_kiT[i]])
                    ps, tps = project(i, C_VA, 512)
                    c.op("act", lambda e: e.activation(out=v1a[:, i, :, 0:64], in_=ps[:, 0:512].rearrange("p (h d) -> p h d", d=64), func=AF.Copy),
                         reads=[tps], writes=[t_v1a[i]])
                    ps, tps = project(i, C_GA, 512)
                    c.op("act", lambda e: e.activation(out=q[:, 1024:1536], in_=ps[:, 0:512], func=AF.Silu), reads=[tps], wadd=[t_q])
                    ps, tps = project(i, C_GB, 512)
                    c.op("act", lambda e: e.activation(out=q[:, 1536:2048], in_=ps[:, 0:512], func=AF.Silu), reads=[tps], wadd=[t_q])
                    for gi, cm in enumerate((C_MA, C_MA + 512, C_MB, C_MB + 512)):
                        ps, tps = project(i, cm, 512)
                        c.op("act", lambda e, gi=gi: e.activation(out=q[:, 2048 + gi * 512:2048 + (gi + 1) * 512], in_=ps[:, 0:512], func=AF.Sigmoid),
                             reads=[tps], wadd=[t_q])
                    c.dma("pool", qs[rows, :], q[:, :], reads=[t_q])
                c.barrier()

            with ExitStack() as st:
                kaT2 = sbt(st, "kaT2", [128, 4, S], BF16); t_kaT2 = T()
                km_bf = sbt(st, "km_bf", [128, 4, 16], BF16); t_km = T()
                wbra = sbt(st, "wbra", [128, 4, D], BF16); t_wbra = T()
                wbrb = sbt(st, "wbrb", [128, 4, D], BF16); t_wbrb = T()
                wo = sbt(st, "wo", [128, 8, D], BF16); t_wo = T()
                qm = sbt(st, "qm", [128, 1024], BF16); t_qm = T()
                qmi = sbt(st, "qmi", [128, 256], BF16); t_qmi = T()
                gt = sbt(st, "gt", [128, 3072], BF16); t_gt = T()
                wit2 = [sbt(st, "wit2%d" % k, [128, 4], F32) for k in range(2)]; t_wit2 = [T(), T()]
                xt = sbt(st, "xt", [128, D], F32); t_xt = T()
                qpad = sbt(st, "qpad", [128, 4, 2, 128], BF16); t_qpad = T()
                qbT = sbt(st, "qbT", [128, 8, 128], BF16); t_qbT = T()
                qiT = [sbt(st, "qiT%d" % k, [128, 4, 128], BF16) for k in range(2)]; t_qiT = [T(), T()]
                score = sbt(st, "score", [128, S], F32); t_score = T()
                mbias = [sbt(st, "mbias%d" % k, [128, S], BF16) for k in range(2)]; t_mbias = [T(), T()]
                rl = sbt(st, "rl", [128, 512], F32); t_rl = T()
                bs = sbt(st, "bs", [128, 8], F32)
                t_cnt = T(); t_tmp = T(); t_mid = T(); t_thr = T()
                sg = sbt(st, "sg", [128, 2], F32); t_sg = T()
                gatem = sbt(st, "gatem", [128, 8, 16], F32); t_gatem = T()
                top8 = sbt(st, "top8", [128, 8, 8], F32); t_top8 = T()
                thr3 = sbt(st, "thr3", [128, 8], F32); t_thr3 = T()
                mbf = sbt(st, "mbf", [128, 8, 16], F32); t_mbf = T()
                mb16 = sbt(st, "mb16", [128, 8, 16], BF16); t_mb16 = T()
                mbT = sbt(st, "mbT", [128, 8, 128], BF16); t_mbT = T()
                pT = [sbt(st, "pT%d" % k, [128, 512], BF16) for k in range(3)]; t_pT = [T(), T(), T()]
                accs = [sbt(st, "accs%d" % k, [128, 520], F32) for k in range(2)]; t_accs = [T(), T()]
                rec = sbt(st, "rec", [128, 8], F32); t_rec = T()
                ytmp = sbt(st, "ytmp", [128, 512], F32); t_ytmp = T()
                yag = sbt(st, "yag", [128, 512], BF16); t_yag = T()
                ybg = sbt(st, "ybg", [128, 512], BF16); t_ybg = T()
                yaT = sbt(st, "yaT", [128, 4, 128], BF16); t_yaT = T()
                ybT = sbt(st, "ybT", [128, 4, 128], BF16); t_ybT = T()
                mtmp2 = sbt(st, "mtmp2", [128, 512], F32); t_mtmp2 = T()
                mrg = sbt(st, "mrg", [128, D], BF16); t_mrg = T()
                mT = sbt(st, "mT", [128, 8, 128], BF16); t_mT = T()
                stp = [pst(st, "stp%d" % k, [128, 512], F32) for k in range(3)]; t_stp = [T(), T(), T()]
                acc = [pst(st, "acc%d" % k, [128, 512], F32) for k in range(2)]; t_acc = [T(), T()]
                m0 = pst(st, "m0", [128, 512], F32); t_m0 = T()
                m1 = pst(st, "m1", [128, 1024], BF16); t_m1 = T()
                mo = pst(st, "mo", [128, 512], F32); t_mo = T()

                for p4 in range(4):
                    c.dma("sp", kaT2[:, p4, :], kaT_s[p4, :, :], wadd=[t_kaT2])
                for kc in range(4):
                    c.dma("pool", wbra[:, kc, :], w_bra[l, kc * 128:(kc + 1) * 128, :], wadd=[t_wbra])
                    c.dma("pool", wbrb[:, kc, :], w_brb[l, kc * 128:(kc + 1) * 128, :], wadd=[t_wbrb])
                for kc in range(8):
                    c.dma("pool", wo[:, kc, :], w_out[l, kc * 128:(kc + 1) * 128, :], wadd=[t_wo])
                c.op("dve", lambda e: e.tensor_scalar(out=km_bf[:, :, :], in0=kmacc[:, :, :], scalar1=1.0 / 256.0, scalar2=None, op0=ALU.mult),
                     reads=[t_kmacc], writes=[t_km])
                c.op("dve", lambda e: e.memset(qpad[:, :, :, :], 0.0), writes=[t_qpad])
                c.op("dve", lambda e: e.memset(qbT[:, :, :], 0.0), writes=[t_qbT])
                c.op("dve", lambda e: e.memset(mbT[:, :, :], 0.0), writes=[t_mbT])
                for k in range(2):
                    c.op("dve", lambda e, k=k: e.memset(qiT[k][:, :, :], 0.0), writes=[t_qiT[k]])

                def rows_of(i):
                    return slice(i * 128, (i + 1) * 128)

                def gen_A(i):
                    rows = rows_of(i)
                    nk = 128 * (i + 1)
                    b = i % 2
                    c.dma("sp", qmi[:, :], qs[rows, 4096:4352], writes=[t_qmi])
                    c.dma("sp", wit2[b][:, :], wis[rows, :], writes=[t_wit2[b]])
                    for h in range(4):
                        c.op("pe", lambda e, h=h: e.transpose(m1[0:64, h * 128:(h + 1) * 128], qmi[:, h * 64:(h + 1) * 64], ident_bf[:]),
                             reads=[t_qmi, t_ident], writes=[t_m1])
                    c.op("act", lambda e: e.activation(out=qiT[b][0:64, :, :], in_=m1[0:64, 0:512].rearrange("p (k t) -> p k t", t=128), func=AF.Copy),
                         reads=[t_m1], wadd=[t_qiT[b]])
                    yield
                    for c0 in range(0, nk, 512):
                        n = min(512, nk - c0)
                        kdeps = [t_kiT[jj] for jj in range(c0 // 128, (c0 + n) // 128)]
                        for hh in range(4):
                            c.op("pe", lambda e, hh=hh: e.matmul(m0[:, 0:n], lhsT=qiT[b][:, hh, :], rhs=kiT[:, c0:c0 + n], start=True, stop=True),
                                 reads=[t_qiT[b]] + kdeps, writes=[t_m0])
                            c.op("act", lambda e: e.activation(out=rl[:, 0:n], in_=m0[:, 0:n], func=AF.Relu),
                                 reads=[t_m0], writes=[t_rl])
                            if hh == 0:
                                c.op("dve", lambda e: e.tensor_scalar(out=score[:, c0:c0 + n], in0=rl[:, 0:n], scalar1=wit2[b][:, 0:1],
                                                                      scalar2=None, op0=ALU.mult),
                                     reads=[t_rl, t_wit2[b]], writes=[t_score])
                            else:
                                c.op("dve", lambda e, hh=hh: e.scalar_tensor_tensor(out=score[:, c0:c0 + n], in0=rl[:, 0:n],
                                                                                   scalar=wit2[b][:, hh:hh + 1], in1=score[:, c0:c0 + n],
                                                                                   op0=ALU.mult, op1=ALU.add),
                                     reads=[t_rl, t_wit2[b], t_score], writes=[t_score])
                            yield
                    c.op("dve", lambda e: e.tensor_tensor(out=score[:, i * 128:(i + 1) * 128], in0=score[:, i * 128:(i + 1) * 128],
                                                          in1=causal[:, :], op=ALU.add),
                         reads=[t_score, t_causal], writes=[t_score])
                    if i >= 2:
                        n_act = 128 * (((i + 1) * ACT_SHARE_NUM) // ACT_SHARE_DEN)
                        n1 = nk - n_act
                        c.op("dve", lambda e: e.memset(bs[:, 2:3], MID0), writes=[t_mid])
                        w = W0
                        for k in range(NBIS):
                            c.op("dve", lambda e: e.tensor_scalar(out=mbias[b][:, 0:n1], in0=score[:, 0:n1], scalar1=bs[:, 2:3], scalar2=None,
                                                                  op0=ALU.is_ge, op1=ALU.add, accum_out=bs[:, 0:1]),
                                 reads=[t_score, t_mid], writes=[t_cnt], wadd=[t_mbias[b]])
                            if n_act > 0:
                                c.op("act", lambda e: e.activation(out=mbias[b][:, n1:nk], in_=score[:, n1:nk], func=AF.Sign,
                                                                   bias=bs[:, 2:3], scale=-1.0, accum_out=sg[:, 0:1]),
                                     reads=[t_score, t_mid], writes=[t_sg], wadd=[t_mbias[b]])
                                c.op("dve", lambda e: e.scalar_tensor_tensor(out=bs[:, 4:5], in0=bs[:, 0:1], scalar=2.0, in1=sg[:, 0:1],
                                                                             op0=ALU.mult, op1=ALU.subtract),
                                     reads=[t_cnt, t_sg], writes=[t_tmp])
                                c.op("dve", lambda e: e.tensor_scalar(out=bs[:, 1:2], in0=bs[:, 4:5], scalar1=2.0 * TOPK - n_act, scalar2=0.5,
                                                                      op0=ALU.is_ge, op1=ALU.subtract), reads=[t_tmp], writes=[t_tmp])
                            else:
                                c.op("dve", lambda e: e.tensor_scalar(out=bs[:, 1:2], in0=bs[:, 0:1], scalar1=TOPK, scalar2=0.5,
                                                                      op0=ALU.is_ge, op1=ALU.subtract), reads=[t_cnt], writes=[t_tmp])
                            c.op("dve", lambda e, w=w: e.scalar_tensor_tensor(out=bs[:, 2:3], in0=bs[:, 1:2], scalar=w, in1=bs[:, 2:3],
                                                                              op0=ALU.mult, op1=ALU.add), reads=[t_tmp, t_mid], writes=[t_mid])
                            w = w / 2.0
                            yield
                        c.op("dve", lambda e, w=w: e.tensor_scalar(out=bs[:, 3:4], in0=bs[:, 2:3], scalar1=-w, scalar2=None, op0=ALU.add),
                             reads=[t_mid], writes=[t_thr])
                    else:
                        c.op("dve", lambda e: e.memset(bs[:, 3:4], -1e29), writes=[t_thr])
                    c.op("dve", lambda e: e.tensor_scalar(out=mbias[b][:, 0:nk], in0=score[:, 0:nk], scalar1=bs[:, 3:4], scalar2=NEG,
                                                          op0=ALU.is_lt, op1=ALU.mult), reads=[t_score, t_thr], writes=[t_mbias[b]])
                    yield

                def prepB(i):
                    rows = rows_of(i)
                    qblk = i // 2
                    c.dma("sp", qm[:, :], qs[rows, 0:1024], writes=[t_qm])
                    for p4 in range(4):
                        c.op("pe", lambda e, p4=p4: e.transpose(m1[:, p4 * 128:(p4 + 1) * 128], qm[:, p4 * 128:(p4 + 1) * 128], ident_bf[:]),
                             reads=[t_qm, t_ident], writes=[t_m1])
                    c.op("act", lambda e: e.activation(out=qpad[0:64, :, 0, :], in_=m1[0:64, 0:512].rearrange("p (k t) -> p k t", t=128), func=AF.Copy),
                         reads=[t_m1], wadd=[t_qpad])
                    c.op("act", lambda e: e.activation(out=qpad[64:128, :, 1, :], in_=m1[64:128, 0:512].rearrange("p (k t) -> p k t", t=128), func=AF.Copy),
                         reads=[t_m1], wadd=[t_qpad])
                    for h in range(8):
                        c.op("pe", lambda e, h=h: e.transpose(m1[0:64, h * 128:(h + 1) * 128], qm[:, 512 + h * 64:512 + (h + 1) * 64], ident_bf[:]),
                             reads=[t_qm, t_ident], writes=[t_m1])
                    c.op("act", lambda e: e.activation(out=qbT[0:64, :, :], in_=m1[0:64, :].rearrange("p (k t) -> p k t", t=128), func=AF.Copy),
                         reads=[t_m1], wadd=[t_qbT])
                    for h in range(8):
                        c.op("pe", lambda e, h=h: e.matmul(m0[:, h * 16:(h + 1) * 16], lhsT=qpad[:, h // 2, h % 2, :], rhs=km_bf[:, h // 2, :],
                                                           start=True, stop=True),
                             reads=[t_qpad, t_km], writes=[t_m0])
                    c.op("dve", lambda e: e.tensor_copy(out=gatem[:, :, :], in_=m0[:, 0:128].rearrange("p (h n) -> p h n", n=16)),
                         reads=[t_m0], writes=[t_gatem])
                    c.op("dve", lambda e: e.memset(gatem[:, :, qblk:16], -1e30), reads=[t_gatem], writes=[t_gatem])
                    for h in range(8):
                        c.op("dve", lambda e, h=h: e.max(out=top8[:, h, :], in_=gatem[:, h, :]), reads=[t_gatem], wadd=[t_top8])
                    c.op("dve", lambda e: e.tensor_scalar(out=thr3[:, :].unsqueeze(2), in0=top8[:, :, 2:3], scalar1=-1e29, scalar2=None, op0=ALU.max),
                         reads=[t_top8], writes=[t_thr3])
                    c.op("dve", lambda e: e.tensor_tensor(out=mbf[:, :, :], in0=gatem[:, :, :],
                                                          in1=thr3[:, :].unsqueeze(2).to_broadcast([128, 8, 16]), op=ALU.is_lt),
                         reads=[t_gatem, t_thr3, t_top8], writes=[t_mbf])
                    c.op("dve", lambda e: e.tensor_scalar(out=mb16[:, :, :], in0=mbf[:, :, :], scalar1=NEG, scalar2=None, op0=ALU.mult),
                         reads=[t_mbf], writes=[t_mb16])
                    c.op("dve", lambda e: e.memset(mb16[:, :, qblk:qblk + 1], 0.0), reads=[t_mb16], writes=[t_mb16])
                    for h in range(8):
                        c.op("pe", lambda e, h=h: e.transpose(m1[0:16, h * 128:(h + 1) * 128], mb16[:, h, :], ident_bf[:]),
                             reads=[t_mb16, t_ident], writes=[t_m1])
                    c.op("act", lambda e: e.activation(out=mbT[0:16, :, :], in_=m1[0:16, :].rearrange("p (k t) -> p k t", t=128), func=AF.Copy),
                         reads=[t_m1], wadd=[t_mbT])

                def gen_C(i):
                    rows = rows_of(i)
                    c.dma("sp", gt[:, :], qs[rows, 1024:4096], writes=[t_gt])
                    c.dma("sp", xt[:, :], x_src[rows, :], writes=[t_xt])
                    for (br, g0, y_out, t_y) in ((0, 0, yag, t_yag), (1, 512, ybg, t_ybg)):
                        av = accs[br][:, :].rearrange("p (h d) -> p h d", d=65)
                        c.op("dve", lambda e: e.reciprocal(out=rec[:, :].unsqueeze(2), in_=av[:, :, 64:65]),
                             reads=[t_accs[br]], writes=[t_rec])
                        c.op("dve", lambda e: e.tensor_tensor(out=ytmp[:, :].rearrange("p (h d) -> p h d", d=64), in0=av[:, :, 0:64],
                                                              in1=rec[:, :].unsqueeze(2).to_broadcast([128, 8, 64]), op=ALU.mult),
                             reads=[t_accs[br], t_rec], writes=[t_ytmp])
                        c.op("dve", lambda e: e.tensor_tensor(out=y_out[:, :], in0=ytmp[:, :], in1=gt[:, g0:g0 + 512], op=ALU.mult),
                             reads=[t_ytmp, t_gt], writes=[t_y])
                        yield
                    for (ysrc, t_ys, yT, t_yT) in ((yag, t_yag, yaT, t_yaT), (ybg, t_ybg, ybT, t_ybT)):
                        for kc in range(4):
                            c.op("pe", lambda e, kc=kc, ysrc=ysrc: e.transpose(m1[:, kc * 128:(kc + 1) * 128], ysrc[:, kc * 128:(kc + 1) * 128], ident_bf[:]),
                                 reads=[t_ys, t_ident], writes=[t_m1])
                        c.op("act", lambda e, yT=yT: e.activation(out=yT[:, :, :], in_=m1[:, 0:512].rearrange("p (k t) -> p k t", t=128), func=AF.Copy),
                             reads=[t_m1], writes=[t_yT])
                        yield
                    for cc in range(2):
                        cs = slice(cc * 512, (cc + 1) * 512)
                        for kc in range(4):
                            c.op("pe", lambda e, kc=kc: e.matmul(mo[:, :], lhsT=yaT[:, kc, :], rhs=wbra[:, kc, cs], start=(kc == 0), stop=(kc == 3)),
                                 reads=[t_yaT, t_wbra], writes=[t_mo])
                        c.op("dve", lambda e: e.tensor_tensor(out=ytmp[:, :], in0=mo[:, :], in1=gt[:, 1024 + cc * 512:1024 + (cc + 1) * 512], op=ALU.mult),
                             reads=[t_mo, t_gt], writes=[t_ytmp])
                        yield
                        for kc in range(4):
                            c.op("pe", lambda e, kc=kc: e.matmul(mo[:, :], lhsT=ybT[:, kc, :], rhs=wbrb[:, kc, cs], start=(kc == 0), stop=(kc == 3)),
                                 reads=[t_ybT, t_wbrb], writes=[t_mo])
                        c.op("dve", lambda e: e.tensor_tensor(out=mtmp2[:, :], in0=mo[:, :], in1=gt[:, 2048 + cc * 512:2048 + (cc + 1) * 512], op=ALU.mult),
                             reads=[t_mo, t_gt], writes=[t_mtmp2])
                        c.op("dve", lambda e: e.tensor_tensor(out=mrg[:, cs], in0=ytmp[:, :], in1=mtmp2[:, :], op=ALU.add),
                             reads=[t_ytmp, t_mtmp2], wadd=[t_mrg])
                        yield
                    for kc in range(8):
                        c.op("pe", lambda e, kc=kc: e.transpose(m1[:, kc * 128:(kc + 1) * 128], mrg[:, kc * 128:(kc + 1) * 128], ident_bf[:]),
                             reads=[t_mrg, t_ident], writes=[t_m1])
                    c.op("act", lambda e: e.activation(out=mT[:, :, :], in_=m1[:, :].rearrange("p (k t) -> p k t", t=128), func=AF.Copy),
                         reads=[t_m1], writes=[t_mT])
                    yield
                    for cc in range(2):
                        cs = slice(cc * 512, (cc + 1) * 512)
                        for kc in range(8):
                            c.op("pe", lambda e, kc=kc: e.matmul(mo[:, :], lhsT=mT[:, kc, :], rhs=wo[:, kc, cs], start=(kc == 0), stop=(kc == 7)),
                                 reads=[t_mT, t_wo], writes=[t_mo])
                        c.op("dve", lambda e: e.tensor_tensor(out=xt[:, cs], in0=mo[:, :], in1=xt[:, cs], op=ALU.add),
                             reads=[t_mo], wadd=[t_xt])
                        yield
                    c.dma("pool", o_dst[rows, :], xt[:, :], reads=[t_xt])
                    yield

                ucnt = [0]

                def make_units(i):
                    units = []
                    for br in range(2):
                        for j in range(i + 1):
                            for hf in range(2):
                                units.append((br, j, hf))
                    return units

                def emit_scores(i, u, slot):
                    br, j, hf = u
                    near = j >= i - 1
                    e0 = 128 * (i - j)
                    o = stp[slot][:, :]
                    ts = t_stp[slot]
                    ks = slice(j * 128, (j + 1) * 128)
                    if br == 0:
                        c.op("pe", lambda e: e.matmul(o, lhsT=eall_bf[:, (j // 2) * 128:(j // 2 + 1) * 128],
                                                      rhs=mbT[:, hf * 4:(hf + 1) * 4, :].rearrange("p k t -> p (k t)"),
                                                      start=True, stop=False, skip_group_check=True),
                             reads=[t_eall, t_mbT], writes=[ts])
                        if near:
                            c.op("pe", lambda e: e.matmul(o, lhsT=ident_bf[:, :], rhs=bimg_hi[:, hf * 4:(hf + 1) * 4, e0:e0 + 128],
                                                          start=False, stop=False, skip_group_check=True),
                                 reads=[t_ident, t_bimg], writes=[ts])
                        for hq in range(4):
                            h = hf * 4 + hq
                            c.op("pe", lambda e, h=h, hq=hq: e.matmul(stp[slot][:, hq * 128:(hq + 1) * 128], lhsT=kaT2[:, h // 2, ks],
                                                                      rhs=qpad[:, h // 2, h % 2, :], start=False, stop=(hq == 3),
                                                                      skip_group_check=True),
                                 reads=[t_kaT2, t_qpad], writes=[ts])
                    else:
                        c.op("pe", lambda e: e.matmul(o, lhsT=kbT[:, ks], rhs=qbT[:, hf * 4:(hf + 1) * 4, :].rearrange("p k t -> p (k t)"),
                                                      start=True, stop=False, skip_group_check=True),
                             reads=[t_kbT[j], t_qbT], writes=[ts])
                        if near:
                            c.op("pe", lambda e: e.matmul(o, lhsT=ident_bf[:, :], rhs=bimg_hi[:, 8 + hf * 4:8 + (hf + 1) * 4, e0:e0 + 128],
                                                          start=False, stop=False, skip_group_check=True),
                                 reads=[t_ident, t_bimg], writes=[ts])
                        c.op("pe", lambda e: e.matmul(o, lhsT=mbias[i % 2][:, ks], rhs=irep_bf[:, :], start=False, stop=True,
                                                      skip_group_check=True),
                             reads=[t_mbias[i % 2], t_irep], writes=[ts])

                def emit_exp(slot):
                    c.op("act", lambda e: e.activation(out=pT[slot][:, :], in_=stp[slot][:, :], func=AF.Exp, scale=0.125),
                         reads=[t_stp[slot]], writes=[t_pT[slot]])

                def emit_pv(i, u, slot):
                    br, j, hf = u
                    for hq in range(4):
                        h = hf * 4 + hq
                        rhs = v1a[:, j, h, :] if br == 0 else v1b[:, j, :]
                        tv = t_v1a[j] if br == 0 else t_v1b[j]
                        c.op("pe", lambda e, hq=hq, rhs=rhs: e.matmul(acc[hf][:, hq * 65:hq * 65 + 65], lhsT=pT[slot][:, hq * 128:(hq + 1) * 128],
                                                                      rhs=rhs, start=(j == 0 and hq == 0), stop=(j == i and hq == 3),
                                                                      skip_group_check=True),
                             reads=[t_pT[slot], tv, t_ones], writes=[t_acc[hf]])
                    if j == i:
                        c.op("act", lambda e: e.activation(out=accs[br][:, hf * 260:(hf + 1) * 260], in_=acc[hf][:, 0:260], func=AF.Copy),
                             reads=[t_acc[hf]], wadd=[t_accs[br]])

                def interleave(gens):
                    gens = list(gens)
                    while gens:
                        for g in list(gens):
                            try:
                                next(g)
                                yield
                            except StopIteration:
                                gens.remove(g)

                def count_A(i):
                    nk = 128 * (i + 1)
                    return 3 + 4 * ((nk + 511) // 512) + (NBIS if i >= 2 else 0)

                for _ in gen_A(0):
                    pass
                prepB(0)
                for i in range(NT):
                    gens = []
                    nbg = 0
                    if i - 1 >= 0:
                        gens.append(gen_C(i - 1))
                        nbg += 12
                    if i + 1 < NT:
                        gens.append(gen_A(i + 1))
                        nbg += count_A(i + 1)
                    bg = interleave(gens)
                    units = make_units(i)
                    nU = len(units)
                    quota = -(-nbg // nU) if nU else nbg
                    slots = [(ucnt[0] + k) % 3 for k in range(nU)]
                    ucnt[0] += nU
                    emit_scores(i, units[0], slots[0])
                    for k in range(nU):
                        if k + 1 < nU:
                            emit_scores(i, units[k + 1], slots[k + 1])
                            if k + 2 == nU and i + 1 < NT:
                                prepB(i + 1)
                        emit_exp(slots[k])
                        emit_pv(i, units[k], slots[k])
                        for _ in range(quota):
                            try:
                                next(bg)
                            except StopIteration:
                                break
                    for _ in bg:
                        pass
                for _ in gen_C(NT - 1):
                    pass
                c.barrier()
        c.finish()
    return nc


_CACHE = {}


def kernel(x, norm_g, w_in, q_norm_a, k_norm_a, q_norm_b, k_norm_b, w_branch_a, w_branch_b, w_out, rel_bias):
    if "nc" not in _CACHE:
        _CACHE["nc"] = build_program()
    nc = _CACHE["nc"]
    f = lambda a: np.ascontiguousarray(np.asarray(a, dtype=np.float32))
    shared = {"norm_g": f(norm_g), "w_in": f(w_in), "q_norm_a": f(q_norm_a), "k_norm_a": f(k_norm_a),
              "q_norm_b": f(q_norm_b), "k_norm_b": f(k_norm_b), "w_branch_a": f(w_branch_a),
              "w_branch_b": f(w_branch_b), "w_out": f(w_out), "rel_bias": f(rel_bias)}
    shared.update(_host_consts())
    xs = f(x)
    in_maps = []
    for b in range(8):
        m = dict(shared)
        m["x"] = xs[b]
        in_maps.append(m)
    res = run_bass_kernel_spmd(nc, in_maps, core_ids=list(range(8)))
    return np.stack([np.asarray(r["out"]) for r in res.results], axis=0).astype(np.float32)
```

```python
import math
from contextlib import ExitStack

import numpy as np
import concourse.bass as bass
import concourse.mybir as mybir
from concourse.bass_utils import run_bass_kernel_spmd

F32 = mybir.dt.float32
BF16 = mybir.dt.bfloat16
ALU = mybir.AluOpType
AF = mybir.ActivationFunctionType
AX = mybir.AxisListType

S = 4096
D = 1024
NT = S // 128
DIN = 5572
DEPTH = 2
EPS = 1e-6
NEG = -30000.0
QS_W = 4352
NBIS = 22
W0 = 1024.0
TOPK = 256.0
MID0 = 2.0 ** -13
ACT_SHARE_NUM, ACT_SHARE_DEN = 0, 8

C_QA, C_KA, C_VA, C_GA = 0, 512, 1024, 1536
C_QB, C_KB, C_VB, C_GB = 2048, 2560, 2624, 2688
C_QI, C_KI, C_WI, C_MA, C_MB = 3200, 3456, 3520, 3524, 4548


class T:
    __slots__ = ("w", "r")

    def __init__(self):
        self.w = {}
        self.r = {}


class Ctx:
    RING = 8

    def __init__(self, nc, stack):
        self.nc = nc
        self.eng = {"pe": nc.tensor, "act": nc.scalar, "dve": nc.vector, "pool": nc.gpsimd, "sp": nc.sync}
        self.sem = {}
        self.cnt = {}
        for k in ("pe", "act", "dve", "pool"):
            self.sem[k] = stack.enter_context(nc.semaphore("s_" + k))
            self.cnt[k] = 0
        self.waited = {k: {} for k in self.eng}
        self.rings = {}
        self.ringpos = {}
        for q in ("sp", "pool"):
            self.rings[q] = []
            for i in range(self.RING):
                key = "d_%s_%d" % (q, i)
                self.sem[key] = stack.enter_context(nc.semaphore(key))
                self.cnt[key] = 0
                self.rings[q].append(key)
            self.ringpos[q] = 0

    def _wait(self, e, marks):
        need = {}
        for (k, v) in marks:
            if need.get(k, 0) < v:
                need[k] = v
        wd = self.waited[e]
        for k, v in need.items():
            if wd.get(k, 0) < v:
                self.eng[e].wait_ge(self.sem[k], v)
                wd[k] = v

    def _deps(self, e, reads, writes):
        marks = []
        for t in reads:
            for m in t.w.items():
                if m[0] == e and e == "pe":
                    continue
                marks.append(m)
        for t in writes:
            for m in t.w.items():
                if m[0] == e:
                    continue
                marks.append(m)
            for m in t.r.items():
                if m[0] == e:
                    continue
                marks.append(m)
        return marks

    def op(self, e, fn, reads=(), writes=(), wadd=()):
        self._wait(e, self._deps(e, reads, tuple(writes) + tuple(wadd)))
        ins = fn(self.eng[e])
        self.cnt[e] += 1
        ins.then_inc(self.sem[e], 1)
        v = self.cnt[e]
        for t in reads:
            t.r[e] = v
        for t in writes:
            t.w = {e: v}
            t.r = {}
        for t in wadd:
            t.w[e] = v
        return ins

    def dma(self, q, out, in_, reads=(), writes=(), wadd=(), **kw):
        key = self.rings[q][self.ringpos[q] % self.RING]
        self.ringpos[q] += 1
        marks = self._deps("dma", reads, tuple(writes) + tuple(wadd))
        if self.cnt[key] > 0:
            marks.append((key, self.cnt[key]))
        self._wait(q, marks)
        ins = self.eng[q].dma_start(out=out, in_=in_, **kw)
        self.cnt[key] += 16
        ins.then_inc(self.sem[key], 16)
        v = self.cnt[key]
        for t in reads:
            t.r[key] = v
        for t in writes:
            t.w = {key: v}
            t.r = {}
        for t in wadd:
            t.w[key] = v

    def all_marks(self):
        marks = []
        for k, v in self.cnt.items():
            if v > 0:
                marks.append((k, v))
        return marks

    def barrier(self):
        marks = self.all_marks()
        for e in ("pe", "act", "dve", "pool", "sp"):
            self._wait(e, [m for m in marks if m[0] != e])

    def finish(self):
        self._wait("sp", self.all_marks())


def _bucket_table():
    n = np.arange(0, 256)
    max_exact = 16
    nf = np.maximum(n, 1).astype(np.float32)
    large = max_exact + (np.log(nf / np.float32(max_exact)) / np.float32(math.log(128 / max_exact))
                         * np.float32(32 - max_exact)).astype(np.int32)
    large = np.minimum(large, 31)
    return np.where(n < max_exact, n, large)


def _host_consts():
    bk = _bucket_table()
    grev = np.zeros((33, 384), np.float32)
    for m in range(384):
        dist = 255 - m
        if dist >= 0:
            grev[bk[dist], m] = 1.0
        else:
            grev[32, m] = 1.0
    ident = np.eye(128, dtype=np.float32)
    irep = np.concatenate([ident] * 4, axis=1)
    t = np.arange(128)[:, None]
    s = np.arange(128)[None, :]
    causalneg = np.where(s <= t, 0.0, -1e30).astype(np.float32)
    eall = np.zeros((16, 16, 128), np.float32)
    for n in range(16):
        eall[n, n, :] = 1.0
    negrow = np.full((1, 16), NEG * 8.0, np.float32)
    return {"c_grev": grev, "c_ident": ident, "c_irep": irep, "c_causal": causalneg,
            "c_eall": eall.reshape(16, 2048), "c_negrow": negrow}


def build_program():
    nc = bass.Bass("TRN2", target_bir_lowering=False)
    din = lambda n, s, d=F32: nc.dram_tensor(n, s, d, kind="ExternalInput").ap()
    x = din("x", [S, D])
    norm_g = din("norm_g", [DEPTH, D])
    w_in = din("w_in", [DEPTH, D, DIN])
    qn_a = din("q_norm_a", [DEPTH, 64])
    kn_a = din("k_norm_a", [DEPTH, 64])
    qn_b = din("q_norm_b", [DEPTH, 64])
    kn_b = din("k_norm_b", [DEPTH, 64])
    w_bra = din("w_branch_a", [DEPTH, 512, D])
    w_brb = din("w_branch_b", [DEPTH, 512, D])
    w_out = din("w_out", [DEPTH, D, D])
    rel_bias = din("rel_bias", [32, 16])
    c_grev = din("c_grev", [33, 384])
    c_ident = din("c_ident", [128, 128])
    c_irep = din("c_irep", [128, 512])
    c_causal = din("c_causal", [128, 128])
    c_eall = din("c_eall", [16, 2048])
    c_negrow = din("c_negrow", [1, 16])
    out = nc.dram_tensor("out", [S, D], F32, kind="ExternalOutput").ap()
    scr = lambda n, s, d: nc.dram_tensor(n, s, d, kind="Internal").ap()
    qs = scr("qs", [S, QS_W], BF16)
    wis = scr("wis", [S, 4], F32)
    kaT_s = scr("kaT_s", [4, 128, S], BF16)
    x1 = scr("x1", [S, D], F32)

    top = ExitStack()
    with top:
        c = Ctx(nc, top)

        uid = [0]

        def sbt(st, name, shape, dt):
            uid[0] += 1
            return st.enter_context(nc.sbuf_tensor("%s_%d" % (name, uid[0]), shape, dt))

        def pst(st, name, shape, dt):
            uid[0] += 1
            return st.enter_context(nc.psum_tensor("%s_%d" % (name, uid[0]), shape, dt))

        ident_bf = sbt(top, "ident_bf", [128, 128], BF16); t_ident = T()
        irep_bf = sbt(top, "irep_bf", [128, 512], BF16); t_irep = T()
        causal = sbt(top, "causal", [128, 128], F32); t_causal = T()
        eall_bf = sbt(top, "eall_bf", [128, 2048], BF16); t_eall = T()
        bimg_hi = sbt(top, "bimg_hi", [128, 16, 256], BF16)
        t_bimg = T()
        v1a = sbt(top, "v1a", [128, NT, 8, 65], BF16); t_v1a = [T() for _ in range(NT)]
        v1b = sbt(top, "v1b", [128, NT, 65], BF16); t_v1b = [T() for _ in range(NT)]
        kbT = sbt(top, "kbT", [128, S], BF16); t_kbT = [T() for _ in range(NT)]
        kiT = sbt(top, "kiT", [128, S], BF16); t_kiT = [T() for _ in range(NT)]
        kmacc = sbt(top, "kmacc", [128, 4, 16], F32); t_kmacc = T()

        c.dma("pool", ident_bf[:], c_ident, writes=[t_ident])
        c.dma("pool", irep_bf[:], c_irep, writes=[t_irep])
        c.op("dve", lambda e: e.memset(eall_bf[:, :], 0.0), writes=[t_eall])
        c.dma("pool", eall_bf[0:16, :], c_eall, writes=[t_eall])
        t_kz = T()
        c.op("dve", lambda e: e.memset(kbT[64:128, :], 0.0), writes=[t_kz])
        c.op("dve", lambda e: e.memset(kiT[64:128, :], 0.0), writes=[t_kz])
        c.dma("sp", causal[:], c_causal, writes=[t_causal])
        t_ones = T()
        c.op("dve", lambda e: e.memset(v1a[:, :, :, 64:65], 1.0), writes=[t_ones])
        c.op("dve", lambda e: e.memset(v1b[:, :, 64:65], 1.0), writes=[t_ones])

        with ExitStack() as st:
            grev = sbt(st, "grev", [33, 384], F32); t_grev = T()
            reltab = sbt(st, "reltab", [33, 16], F32); t_rel = T()
            r31 = sbt(st, "r31", [32, 16], F32); t_r31 = T()
            bps = pst(st, "bps", [128, 2048], F32); t_bps = T()
            c.dma("sp", grev[:], c_grev, writes=[t_grev])
            c.dma("sp", reltab[0:32, :], rel_bias, writes=[t_rel])
            c.dma("sp", r31[:], rel_bias[31:32, :].to_broadcast([32, 16]), writes=[t_r31])
            c.op("dve", lambda e: e.tensor_tensor(out=reltab[0:32, :], in0=reltab[0:32, :], in1=r31[:], op=ALU.subtract),
                 reads=[t_r31, t_rel], writes=[t_rel])
            c.op("dve", lambda e: e.tensor_scalar(out=reltab[0:32, :], in0=reltab[0:32, :], scalar1=8.0, scalar2=None, op0=ALU.mult),
                 reads=[t_rel], writes=[t_rel])
            c.dma("sp", reltab[32:33, :], c_negrow, reads=[t_rel], writes=[t_rel])
            for half in range(2):
                for el in range(128):
                    e_ = half * 128 + el
                    c.op("pe", lambda e, el=el, e_=e_: e.matmul(bps[:, el * 16:(el + 1) * 16], lhsT=grev[0:33, 255 - e_:255 - e_ + 128],
                                                                 rhs=reltab[0:33, :], start=True, stop=True),
                         reads=[t_grev, t_rel], writes=[t_bps])
                src = bps[:, :].rearrange("p (e h) -> p h e", h=16)
                hi_v = bimg_hi[:, :, half * 128:(half + 1) * 128]
                c.op("dve", lambda e: e.tensor_copy(out=hi_v, in_=src), reads=[t_bps], wadd=[t_bimg])
            c.barrier()

        for l in range(DEPTH):
            x_src = x if l == 0 else x1
            o_dst = x1 if l == 0 else out
            with ExitStack() as st:
                w_bf = sbt(st, "w_bf", [128, 8, DIN], BF16); t_w = [T() for _ in range(8)]
                gbc = sbt(st, "gbc", [128, D], F32); t_gbc = T()
                gq_a = sbt(st, "gq_a", [128, 64], F32)
                gk_a = sbt(st, "gk_a", [128, 64], F32)
                gq_b = sbt(st, "gq_b", [128, 64], F32)
                gk_b = sbt(st, "gk_b", [128, 64], F32)
                t_gs = T()
                xb = [sbt(st, "xb%d" % i, [128, D], F32) for i in range(2)]; t_xb = [T(), T()]
                sqj = sbt(st, "sqj", [128, D], BF16); t_sqj = T()
                stat = sbt(st, "stat", [128, 8], F32); t_stat = T()
                hb = sbt(st, "hb", [128, D], BF16); t_hb = T()
                hT = [sbt(st, "hT%d" % i, [128, 8, 128], BF16) for i in range(2)]; t_hT = [T(), T()]
                qt = [sbt(st, "qt%d" % i, [128, QS_W], BF16) for i in range(2)]; t_qt = [T(), T()]
                sqf = sbt(st, "sqf", [128, 512], F32); t_sqf = T()
                hst = sbt(st, "hst", [128, 32], F32); t_hst = T()
                tmpf = sbt(st, "tmpf", [128, 512], F32); t_tmpf = T()
                kan = sbt(st, "kan", [128, 512], BF16); t_kan = T()
                kTt = [sbt(st, "kTt%d" % i, [128, 4, 128], BF16) for i in range(2)]; t_kTt = [T(), T()]
                kred = sbt(st, "kred", [128, 4], F32); t_kred = T()
                sm = sbt(st, "sm", [128, 128], BF16); t_sm = T()
                wit = [sbt(st, "witp%d" % i, [128, 4], F32) for i in range(2)]; t_wit = [T(), T()]
                tp = pst(st, "tp", [128, 1024], BF16); t_tp = T()
                tp2 = pst(st, "tp2", [128, 1024], BF16); t_tp2 = T()
                pj = [pst(st, "pj%d" % i, [128, 512], F32) for i in range(3)]; t_pj = [T(), T(), T()]

                for kc in range(8):
                    for (c0, c1) in ((0, 2048), (2048, 4096), (4096, DIN)):
                        c.dma("pool", w_bf[:, kc, c0:c1], w_in[l, kc * 128:(kc + 1) * 128, c0:c1], wadd=[t_w[kc]])
                c.dma("sp", gbc[:], norm_g[l:l + 1, :].to_broadcast([128, D]), writes=[t_gbc])
                for (g_sb, g_dr) in ((gq_a, qn_a), (gk_a, kn_a), (gq_b, qn_b), (gk_b, kn_b)):
                    c.dma("sp", g_sb[:], g_dr[l:l + 1, :].to_broadcast([128, 64]), wadd=[t_gs])
                c.op("dve", lambda e: e.memset(kmacc[:], 0.0), writes=[t_kmacc])

                pjn = [0]

                def project(i, col0, ncols):
                    b = pjn[0] % 3
                    pjn[0] += 1
                    for kc in range(8):
                        c.op("pe", lambda e, kc=kc: e.matmul(pj[b][:, 0:ncols], lhsT=hT[i % 2][:, kc, :],
                                                              rhs=w_bf[:, kc, col0:col0 + ncols],
                                                              start=(kc == 0), stop=(kc == 7)),
                             reads=[t_hT[i % 2], t_w[kc]], writes=[t_pj[b]])
                    return pj[b], t_pj[b]

                def rsqrt_small(ap_in, ap_out, n, inv_n):
                    c.op("dve", lambda e: e.tensor_scalar(out=ap_out, in0=ap_in, scalar1=inv_n, scalar2=EPS,
                                                          op0=ALU.mult, op1=ALU.add), reads=[t_hst], writes=[t_hst])
                    c.op("act", lambda e: e.activation(out=ap_out, in_=ap_out, func=AF.Ln), reads=[t_hst], writes=[t_hst])
                    c.op("act", lambda e: e.activation(out=ap_out, in_=ap_out, func=AF.Exp, scale=-0.5),
                         reads=[t_hst], writes=[t_hst])

                def headnorm(ps_ap, t_ps, H, g_sb, out_ap, t_out, wadd_out=False):
                    W = H * 64
                    c.op("act", lambda e: e.activation(out=sqf[:, 0:W], in_=ps_ap, func=AF.Square),
                         reads=[t_ps], writes=[t_sqf])
                    c.op("dve", lambda e: e.tensor_reduce(out=hst[:, 0:H], in_=sqf[:, 0:W].rearrange("p (h d) -> p h d", d=64),
                                                          axis=AX.X, op=ALU.add), reads=[t_sqf], writes=[t_hst])
                    rsqrt_small(hst[:, 0:H], hst[:, 0:H], H, 1.0 / 64.0)
                    c.op("dve", lambda e: e.tensor_tensor(out=tmpf[:, 0:W].rearrange("p (h d) -> p h d", d=64),
                                                          in0=ps_ap.rearrange("p (h d) -> p h d", d=64),
                                                          in1=hst[:, 0:H].unsqueeze(2).to_broadcast([128, H, 64]), op=ALU.mult),
                         reads=[t_ps, t_hst], writes=[t_tmpf])
                    kw = dict(wadd=[t_out]) if wadd_out else dict(writes=[t_out])
                    c.op("dve", lambda e: e.tensor_tensor(out=out_ap.rearrange("p (h d) -> p h d", d=64),
                                                          in0=tmpf[:, 0:W].rearrange("p (h d) -> p h d", d=64),
                                                          in1=g_sb[:, :].unsqueeze(1).to_broadcast([128, H, 64]), op=ALU.mult),
                         reads=[t_tmpf, t_gs], **kw)

                for i in range(NT):
                    rows = slice(i * 128, (i + 1) * 128)
                    xt, t_xt = xb[i % 2], t_xb[i % 2]
                    q, t_q = qt[i % 2], t_qt[i % 2]
                    c.dma("sp", xt[:], x_src[rows, :], writes=[t_xt])
                    c.op("act", lambda e: e.activation(out=sqj[:], in_=xt[:], func=AF.Square, accum_out=stat[:, 0:1]),
                         reads=[t_xt], writes=[t_sqj, t_stat])
                    c.op("dve", lambda e: e.tensor_scalar(out=stat[:, 1:2], in0=stat[:, 0:1], scalar1=1.0 / D, scalar2=EPS,
                                                          op0=ALU.mult, op1=ALU.add), reads=[t_stat], writes=[t_stat])
                    c.op("act", lambda e: e.activation(out=stat[:, 2:3], in_=stat[:, 1:2], func=AF.Ln), reads=[t_stat], writes=[t_stat])
                    c.op("act", lambda e: e.activation(out=stat[:, 3:4], in_=stat[:, 2:3], func=AF.Exp, scale=-0.5),
                         reads=[t_stat], writes=[t_stat])
                    c.op("dve", lambda e: e.scalar_tensor_tensor(out=hb[:], in0=xt[:], scalar=stat[:, 3:4], in1=gbc[:],
                                                                 op0=ALU.mult, op1=ALU.mult),
                         reads=[t_xt, t_stat, t_gbc], writes=[t_hb])
                    for kc in range(8):
                        c.op("pe", lambda e, kc=kc: e.transpose(tp[:, kc * 128:(kc + 1) * 128], hb[:, kc * 128:(kc + 1) * 128], ident_bf[:]),
                             reads=[t_hb, t_ident], writes=[t_tp])
                    c.op("act", lambda e: e.activation(out=hT[i % 2][:, :, :], in_=tp[:, :].rearrange("p (k t) -> p k t", t=128), func=AF.Copy),
                         reads=[t_tp], writes=[t_hT[i % 2]])
                    ps, tps = project(i, C_QA, 512)
                    headnorm(ps[:, 0:512], tps, 8, gq_a, q[:, 0:512], t_q)
                    ps, tps = project(i, C_KA, 512)
                    headnorm(ps[:, 0:512], tps, 8, gk_a, kan[:, :], t_kan)
                    kt, t_kt = kTt[i % 2], t_kTt[i % 2]
                    for p4 in range(4):
                        c.op("pe", lambda e, p4=p4: e.transpose(tp2[:, p4 * 128:(p4 + 1) * 128], kan[:, p4 * 128:(p4 + 1) * 128], ident_bf[:]),
                             reads=[t_kan, t_ident], writes=[t_tp2])
                    c.op("act", lambda e: e.activation(out=kt[:, :, :], in_=tp2[:, 0:512].rearrange("p (k t) -> p k t", t=128), func=AF.Copy),
                         reads=[t_tp2], writes=[t_kt])
                    for p4 in range(4):
                        c.dma("pool", kaT_s[p4, :, i * 128:(i + 1) * 128], kt[:, p4, :], reads=[t_kt])
                    c.op("dve", lambda e: e.tensor_reduce(out=kred[:, :], in_=kt[:, :, :], axis=AX.X, op=ALU.add),
                         reads=[t_kt], writes=[t_kred])
                    nblk = i // 2
                    c.op("dve", lambda e: e.tensor_tensor(out=kmacc[:, :, nblk], in0=kmacc[:, :, nblk], in1=kred[:, :], op=ALU.add),
                         reads=[t_kred, t_kmacc], writes=[t_kmacc])
                    ps, tps = project(i, C_QB, 512)
                    headnorm(ps[:, 0:512], tps, 8, gq_b, q[:, 512:1024], t_q, wadd_out=True)
                    ps, tps = project(i, C_KB, 128)
                    headnorm(ps[:, 0:64], tps, 1, gk_b, sm[:, 0:64], t_sm)
                    c.op("dve", lambda e: e.tensor_copy(out=v1b[:, i, 0:64], in_=ps[:, 64:128]), reads=[tps], writes=[t_v1b[i]])
                    ps2, tps2 = project(i, C_QI, 324)
                    c.op("dve", lambda e: e.tensor_copy(out=q[:, 4096:4352], in_=ps2[:, 0:256]), reads=[tps2], wadd=[t_q])
                    c.op("dve", lambda e: e.tensor_copy(out=sm[:, 64:128], in_=ps2[:, 256:320]), reads=[tps2], wadd=[t_sm])
                    c.op("dve", lambda e: e.tensor_copy(out=wit[i % 2][:, :], in_=ps2[:, 320:324]), reads=[tps2], writes=[t_wit[i % 2]])
                    c.dma("pool", wis[rows, :], wit[i % 2][:, :], reads=[t_wit[i % 2]])
                    c.op("pe", lambda e: e.transpose(tp2[0:64, 512:640], sm[:, 0:64], ident_bf[:]), reads=[t_sm, t_ident], writes=[t_tp2])
                    c.op("pe", lambda e: e.transpose(tp2[0:64, 640:768], sm[:, 64:128], ident_bf[:]), reads=[t_sm, t_ident], writes=[t_tp2])
                    c.op("act", lambda e: e.activation(out=kbT[0:64, i * 128:(i + 1) * 128], in_=tp2[0:64, 512:640], func=AF.Copy),
                         reads=[t_tp2], writes=[t_kbT[i]])
                    c.op("act", lambda e: e.activation(out=kiT[0:64, i * 128:(i + 1) * 128], in_=tp2[0:64, 640:768], func=AF.Copy),
                         reads=[t_tp2], writes=[t_kiT[i]])
                    ps, tps = project(i, C_VA, 512)
                    c.op("act", lambda e: e.activation(out=v1a[:, i, :, 0:64], in_=ps[:, 0:512].rearrange("p (h d) -> p h d", d=64), func=AF.Copy),
                         reads=[tps], writes=[t_v1a[i]])
                    ps, tps = project(i, C_GA, 512)
                    c.op("act", lambda e: e.activation(out=q[:, 1024:1536], in_=ps[:, 0:512], func=AF.Silu), reads=[tps], wadd=[t_q])
                    ps, tps = project(i, C_GB, 512)
                    c.op("act", lambda e: e.activation(out=q[:, 1536:2048], in_=ps[:, 0:512], func=AF.Silu), reads=[tps], wadd=[t_q])
                    for gi, cm in enumerate((C_MA, C_MA + 512, C_MB, C_MB + 512)):
                        ps, tps = project(i, cm, 512)
                        c.op("act", lambda e, gi=gi: e.activation(out=q[:, 2048 + gi * 512:2048 + (gi + 1) * 512], in_=ps[:, 0:512], func=AF.Sigmoid),
                             reads=[tps], wadd=[t_q])
                    c.dma("pool", qs[rows, :], q[:, :], reads=[t_q])
                c.barrier()

            with ExitStack() as st:
                kaT2 = sbt(st, "kaT2", [128, 4, S], BF16); t_kaT2 = T()
                km_bf = sbt(st, "km_bf", [128, 4, 16], BF16); t_km = T()
                wbra = sbt(st, "wbra", [128, 4, D], BF16); t_wbra = T()
                wbrb = sbt(st, "wbrb", [128, 4, D], BF16); t_wbrb = T()
                wo = sbt(st, "wo", [128, 8, D], BF16); t_wo = T()
                qm = sbt(st, "qm", [128, 1024], BF16); t_qm = T()
                qmi = sbt(st, "qmi", [128, 256], BF16); t_qmi = T()
                gt = sbt(st, "gt", [128, 3072], BF16); t_gt = T()
                wit2 = [sbt(st, "wit2%d" % k, [128, 4], F32) for k in range(2)]; t_wit2 = [T(), T()]
                xt = sbt(st, "xt", [128, D], F32); t_xt = T()
                qpad = sbt(st, "qpad", [128, 4, 2, 128], BF16); t_qpad = T()
                qbT = sbt(st, "qbT", [128, 8, 128], BF16); t_qbT = T()
                qiT = [sbt(st, "qiT%d" % k, [128, 4, 128], BF16) for k in range(2)]; t_qiT = [T(), T()]
                score = sbt(st, "score", [128, S], F32); t_score = T()
                mbias = [sbt(st, "mbias%d" % k, [128, S], BF16) for k in range(2)]; t_mbias = [T(), T()]
                rl = sbt(st, "rl", [128, 512], F32); t_rl = T()
                bs = sbt(st, "bs", [128, 8], F32)
                t_cnt = T(); t_tmp = T(); t_mid = T(); t_thr = T()
                sg = sbt(st, "sg", [128, 2], F32); t_sg = T()
                gatem = sbt(st, "gatem", [128, 8, 16], F32); t_gatem = T()
                top8 = sbt(st, "top8", [128, 8, 8], F32); t_top8 = T()
                thr3 = sbt(st, "thr3", [128, 8], F32); t_thr3 = T()
                mbf = sbt(st, "mbf", [128, 8, 16], F32); t_mbf = T()
                mb16 = sbt(st, "mb16", [128, 8, 16], BF16); t_mb16 = T()
                mbT = sbt(st, "mbT", [128, 8, 128], BF16); t_mbT = T()
                pT = [sbt(st, "pT%d" % k, [128, 512], BF16) for k in range(3)]; t_pT = [T(), T(), T()]
                accs = [sbt(st, "accs%d" % k, [128, 520], F32) for k in range(2)]; t_accs = [T(), T()]
                rec = sbt(st, "rec", [128, 8], F32); t_rec = T()
                ytmp = sbt(st, "ytmp", [128, 512], F32); t_ytmp = T()
                yag = sbt(st, "yag", [128, 512], BF16); t_yag = T()
                ybg = sbt(st, "ybg", [128, 512], BF16); t_ybg = T()
                yaT = sbt(st, "yaT", [128, 4, 128], BF16); t_yaT = T()
                ybT = sbt(st, "ybT", [128, 4, 128], BF16); t_ybT = T()
                mtmp2 = sbt(st, "mtmp2", [128, 512], F32); t_mtmp2 = T()
                mrg = sbt(st, "mrg", [128, D], BF16); t_mrg = T()
                mT = sbt(st, "mT", [128, 8, 128], BF16); t_mT = T()
                stp = [pst(st, "stp%d" % k, [128, 512], F32) for k in range(3)]; t_stp = [T(), T(), T()]
                acc = [pst(st, "acc%d" % k, [128, 512], F32) for k in range(2)]; t_acc = [T(), T()]
                m0 = pst(st, "m0", [128, 512], F32); t_m0 = T()
                m1 = pst(st, "m1", [128, 1024], BF16); t_m1 = T()
                mo = pst(st, "mo", [128, 512], F32); t_mo = T()

                for p4 in range(4):
                    c.dma("sp", kaT2[:, p4, :], kaT_s[p4, :, :], wadd=[t_kaT2])
                for kc in range(4):
                    c.dma("pool", wbra[:, kc, :], w_bra[l, kc * 128:(kc + 1) * 128, :], wadd=[t_wbra])
                    c.dma("pool", wbrb[:, kc, :], w_brb[l, kc * 128:(kc + 1) * 128, :], wadd=[t_wbrb])
                for kc in range(8):
                    c.dma("pool", wo[:, kc, :], w_out[l, kc * 128:(kc + 1) * 128, :], wadd=[t_wo])
                c.op("dve", lambda e: e.tensor_scalar(out=km_bf[:, :, :], in0=kmacc[:, :, :], scalar1=1.0 / 256.0, scalar2=None, op0=ALU.mult),
                     reads=[t_kmacc], writes=[t_km])
                c.op("dve", lambda e: e.memset(qpad[:, :, :, :], 0.0), writes=[t_qpad])
                c.op("dve", lambda e: e.memset(qbT[:, :, :], 0.0), writes=[t_qbT])
                c.op("dve", lambda e: e.memset(mbT[:, :, :], 0.0), writes=[t_mbT])
                for k in range(2):
                    c.op("dve", lambda e, k=k: e.memset(qiT[k][:, :, :], 0.0), writes=[t_qiT[k]])

                def rows_of(i):
                    return slice(i * 128, (i + 1) * 128)

                def gen_A(i):
                    rows = rows_of(i)
                    nk = 128 * (i + 1)
                    b = i % 2
                    c.dma("sp", qmi[:, :], qs[rows, 4096:4352], writes=[t_qmi])
                    c.dma("sp", wit2[b][:, :], wis[rows, :], writes=[t_wit2[b]])
                    for h in range(4):
                        c.op("pe", lambda e, h=h: e.transpose(m1[0:64, h * 128:(h + 1) * 128], qmi[:, h * 64:(h + 1) * 64], ident_bf[:]),
                             reads=[t_qmi, t_ident], writes=[t_m1])
                    c.op("act", lambda e: e.activation(out=qiT[b][0:64, :, :], in_=m1[0:64, 0:512].rearrange("p (k t) -> p k t", t=128), func=AF.Copy),
                         reads=[t_m1], wadd=[t_qiT[b]])
                    yield
                    for c0 in range(0, nk, 512):
                        n = min(512, nk - c0)
                        kdeps = [t_kiT[jj] for jj in range(c0 // 128, (c0 + n) // 128)]
                        for hh in range(4):
                            c.op("pe", lambda e, hh=hh: e.matmul(m0[:, 0:n], lhsT=qiT[b][:, hh, :], rhs=kiT[:, c0:c0 + n], start=True, stop=True),
                                 reads=[t_qiT[b]] + kdeps, writes=[t_m0])
                            c.op("act", lambda e: e.activation(out=rl[:, 0:n], in_=m0[:, 0:n], func=AF.Relu),
                                 reads=[t_m0], writes=[t_rl])
                            if hh == 0:
                                c.op("dve", lambda e: e.tensor_scalar(out=score[:, c0:c0 + n], in0=rl[:, 0:n], scalar1=wit2[b][:, 0:1],
                                                                      scalar2=None, op0=ALU.mult),
                                     reads=[t_rl, t_wit2[b]], writes=[t_score])
                            else:
                                c.op("dve", lambda e, hh=hh: e.scalar_tensor_tensor(out=score[:, c0:c0 + n], in0=rl[:, 0:n],
                                                                                   scalar=wit2[b][:, hh:hh + 1], in1=score[:, c0:c0 + n],
                                                                                   op0=ALU.mult, op1=ALU.add),
                                     reads=[t_rl, t_wit2[b], t_score], writes=[t_score])
                            yield
                    c.op("dve", lambda e: e.tensor_tensor(out=score[:, i * 128:(i + 1) * 128], in0=score[:, i * 128:(i + 1) * 128],
                                                          in1=causal[:, :], op=ALU.add),
                         reads=[t_score, t_causal], writes=[t_score])
                    if i >= 2:
                        n_act = 128 * (((i + 1) * ACT_SHARE_NUM) // ACT_SHARE_DEN)
                        n1 = nk - n_act
                        c.op("dve", lambda e: e.memset(bs[:, 2:3], MID0), writes=[t_mid])
                        w = W0
                        for k in range(NBIS):
                            c.op("dve", lambda e: e.tensor_scalar(out=mbias[b][:, 0:n1], in0=score[:, 0:n1], scalar1=bs[:, 2:3], scalar2=None,
                                                                  op0=ALU.is_ge, op1=ALU.add, accum_out=bs[:, 0:1]),
                                 reads=[t_score, t_mid], writes=[t_cnt], wadd=[t_mbias[b]])
                            if n_act > 0:
                                c.op("act", lambda e: e.activation(out=mbias[b][:, n1:nk], in_=score[:, n1:nk], func=AF.Sign,
                                                                   bias=bs[:, 2:3], scale=-1.0, accum_out=sg[:, 0:1]),
                                     reads=[t_score, t_mid], writes=[t_sg], wadd=[t_mbias[b]])
                                c.op("dve", lambda e: e.scalar_tensor_tensor(out=bs[:, 4:5], in0=bs[:, 0:1], scalar=2.0, in1=sg[:, 0:1],
                                                                             op0=ALU.mult, op1=ALU.subtract),
                                     reads=[t_cnt, t_sg], writes=[t_tmp])
                                c.op("dve", lambda e: e.tensor_scalar(out=bs[:, 1:2], in0=bs[:, 4:5], scalar1=2.0 * TOPK - n_act, scalar2=0.5,
                                                                      op0=ALU.is_ge, op1=ALU.subtract), reads=[t_tmp], writes=[t_tmp])
                            else:
                                c.op("dve", lambda e: e.tensor_scalar(out=bs[:, 1:2], in0=bs[:, 0:1], scalar1=TOPK, scalar2=0.5,
                                                                      op0=ALU.is_ge, op1=ALU.subtract), reads=[t_cnt], writes=[t_tmp])
                            c.op("dve", lambda e, w=w: e.scalar_tensor_tensor(out=bs[:, 2:3], in0=bs[:, 1:2], scalar=w, in1=bs[:, 2:3],
                                                                              op0=ALU.mult, op1=ALU.add), reads=[t_tmp, t_mid], writes=[t_mid])
                            w = w / 2.0
                            yield
                        c.op("dve", lambda e, w=w: e.tensor_scalar(out=bs[:, 3:4], in0=bs[:, 2:3], scalar1=-w, scalar2=None, op0=ALU.add),
                             reads=[t_mid], writes=[t_thr])
                    else:
                        c.op("dve", lambda e: e.memset(bs[:, 3:4], -1e29), writes=[t_thr])
                    c.op("dve", lambda e: e.tensor_scalar(out=mbias[b][:, 0:nk], in0=score[:, 0:nk], scalar1=bs[:, 3:4], scalar2=NEG,
                                                          op0=ALU.is_lt, op1=ALU.mult), reads=[t_score, t_thr], writes=[t_mbias[b]])
                    yield

                def prepB(i):
                    rows = rows_of(i)
                    qblk = i // 2
                    c.dma("sp", qm[:, :], qs[rows, 0:1024], writes=[t_qm])
                    for p4 in range(4):
                        c.op("pe", lambda e, p4=p4: e.transpose(m1[:, p4 * 128:(p4 + 1) * 128], qm[:, p4 * 128:(p4 + 1) * 128], ident_bf[:]),
                             reads=[t_qm, t_ident], writes=[t_m1])
                    c.op("act", lambda e: e.activation(out=qpad[0:64, :, 0, :], in_=m1[0:64, 0:512].rearrange("p (k t) -> p k t", t=128), func=AF.Copy),
                         reads=[t_m1], wadd=[t_qpad])
                    c.op("act", lambda e: e.activation(out=qpad[64:128, :, 1, :], in_=m1[64:128, 0:512].rearrange("p (k t) -> p k t", t=128), func=AF.Copy),
                         reads=[t_m1], wadd=[t_qpad])
                    for h in range(8):
                        c.op("pe", lambda e, h=h: e.transpose(m1[0:64, h * 128:(h + 1) * 128], qm[:, 512 + h * 64:512 + (h + 1) * 64], ident_bf[:]),
                             reads=[t_qm, t_ident], writes=[t_m1])
                    c.op("act", lambda e: e.activation(out=qbT[0:64, :, :], in_=m1[0:64, :].rearrange("p (k t) -> p k t", t=128), func=AF.Copy),
                         reads=[t_m1], wadd=[t_qbT])
                    for h in range(8):
                        c.op("pe", lambda e, h=h: e.matmul(m0[:, h * 16:(h + 1) * 16], lhsT=qpad[:, h // 2, h % 2, :], rhs=km_bf[:, h // 2, :],
                                                           start=True, stop=True),
                             reads=[t_qpad, t_km], writes=[t_m0])
                    c.op("dve", lambda e: e.tensor_copy(out=gatem[:, :, :], in_=m0[:, 0:128].rearrange("p (h n) -> p h n", n=16)),
                         reads=[t_m0], writes=[t_gatem])
                    c.op("dve", lambda e: e.memset(gatem[:, :, qblk:16], -1e30), reads=[t_gatem], writes=[t_gatem])
                    for h in range(8):
                        c.op("dve", lambda e, h=h: e.max(out=top8[:, h, :], in_=gatem[:, h, :]), reads=[t_gatem], wadd=[t_top8])
                    c.op("dve", lambda e: e.tensor_scalar(out=thr3[:, :].unsqueeze(2), in0=top8[:, :, 2:3], scalar1=-1e29, scalar2=None, op0=ALU.max),
                         reads=[t_top8], writes=[t_thr3])
                    c.op("dve", lambda e: e.tensor_tensor(out=mbf[:, :, :], in0=gatem[:, :, :],
                                                          in1=thr3[:, :].unsqueeze(2).to_broadcast([128, 8, 16]), op=ALU.is_lt),
                         reads=[t_gatem, t_thr3, t_top8], writes=[t_mbf])
                    c.op("dve", lambda e: e.tensor_scalar(out=mb16[:, :, :], in0=mbf[:, :, :], scalar1=NEG, scalar2=None, op0=ALU.mult),
                         reads=[t_mbf], writes=[t_mb16])
                    c.op("dve", lambda e: e.memset(mb16[:, :, qblk:qblk + 1], 0.0), reads=[t_mb16], writes=[t_mb16])
                    for h in range(8):
                        c.op("pe", lambda e, h=h: e.transpose(m1[0:16, h * 128:(h + 1) * 128], mb16[:, h, :], ident_bf[:]),
                             reads=[t_mb16, t_ident], writes=[t_m1])
                    c.op("act", lambda e: e.activation(out=mbT[0:16, :, :], in_=m1[0:16, :].rearrange("p (k t) -> p k t", t=128), func=AF.Copy),
                         reads=[t_m1], wadd=[t_mbT])

                def gen_C(i):
                    rows = rows_of(i)
                    c.dma("sp", gt[:, :], qs[rows, 1024:4096], writes=[t_gt])
                    c.dma("sp", xt[:, :], x_src[rows, :], writes=[t_xt])
                    for (br, g0, y_out, t_y) in ((0, 0, yag, t_yag), (1, 512, ybg, t_ybg)):
                        av = accs[br][:, :].rearrange("p (h d) -> p h d", d=65)
                        c.op("dve", lambda e: e.reciprocal(out=rec[:, :].unsqueeze(2), in_=av[:, :, 64:65]),
                             reads=[t_accs[br]], writes=[t_rec])
                        c.op("dve", lambda e: e.tensor_tensor(out=ytmp[:, :].rearrange("p (h d) -> p h d", d=64), in0=av[:, :, 0:64],
                                                              in1=rec[:, :].unsqueeze(2).to_broadcast([128, 8, 64]), op=ALU.mult),
                             reads=[t_accs[br], t_rec], writes=[t_ytmp])
                        c.op("dve", lambda e: e.tensor_tensor(out=y_out[:, :], in0=ytmp[:, :], in1=gt[:, g0:g0 + 512], op=ALU.mult),
                             reads=[t_ytmp, t_gt], writes=[t_y])
                        yield
                    for (ysrc, t_ys, yT, t_yT) in ((yag, t_yag, yaT, t_yaT), (ybg, t_ybg, ybT, t_ybT)):
                        for kc in range(4):
                            c.op("pe", lambda e, kc=kc, ysrc=ysrc: e.transpose(m1[:, kc * 128:(kc + 1) * 128], ysrc[:, kc * 128:(kc + 1) * 128], ident_bf[:]),
                                 reads=[t_ys, t_ident], writes=[t_m1])
                        c.op("act", lambda e, yT=yT: e.activation(out=yT[:, :, :], in_=m1[:, 0:512].rearrange("p (k t) -> p k t", t=128), func=AF.Copy),
                             reads=[t_m1], writes=[t_yT])
                        yield
                    for cc in range(2):
                        cs = slice(cc * 512, (cc + 1) * 512)
                        for kc in range(4):
                            c.op("pe", lambda e, kc=kc: e.matmul(mo[:, :], lhsT=yaT[:, kc, :], rhs=wbra[:, kc, cs], start=(kc == 0), stop=(kc == 3)),
                                 reads=[t_yaT, t_wbra], writes=[t_mo])
                        c.op("dve", lambda e: e.tensor_tensor(out=ytmp[:, :], in0=mo[:, :], in1=gt[:, 1024 + cc * 512:1024 + (cc + 1) * 512], op=ALU.mult),
                             reads=[t_mo, t_gt], writes=[t_ytmp])
                        yield
                        for kc in range(4):
                            c.op("pe", lambda e, kc=kc: e.matmul(mo[:, :], lhsT=ybT[:, kc, :], rhs=wbrb[:, kc, cs], start=(kc == 0), stop=(kc == 3)),
                                 reads=[t_ybT, t_wbrb], writes=[t_mo])
                        c.op("dve", lambda e: e.tensor_tensor(out=mtmp2[:, :], in0=mo[:, :], in1=gt[:, 2048 + cc * 512:2048 + (cc + 1) * 512], op=ALU.mult),
                             reads=[t_mo, t_gt], writes=[t_mtmp2])
                        c.op("dve", lambda e: e.tensor_tensor(out=mrg[:, cs], in0=ytmp[:, :], in1=mtmp2[:, :], op=ALU.add),
                             reads=[t_ytmp, t_mtmp2], wadd=[t_mrg])
                        yield
                    for kc in range(8):
                        c.op("pe", lambda e, kc=kc: e.transpose(m1[:, kc * 128:(kc + 1) * 128], mrg[:, kc * 128:(kc + 1) * 128], ident_bf[:]),
                             reads=[t_mrg, t_ident], writes=[t_m1])
                    c.op("act", lambda e: e.activation(out=mT[:, :, :], in_=m1[:, :].rearrange("p (k t) -> p k t", t=128), func=AF.Copy),
                         reads=[t_m1], writes=[t_mT])
                    yield
                    for cc in range(2):
                        cs = slice(cc * 512, (cc + 1) * 512)
                        for kc in range(8):
                            c.op("pe", lambda e, kc=kc: e.matmul(mo[:, :], lhsT=mT[:, kc, :], rhs=wo[:, kc, cs], start=(kc == 0), stop=(kc == 7)),
                                 reads=[t_mT, t_wo], writes=[t_mo])
                        c.op("dve", lambda e: e.tensor_tensor(out=xt[:, cs], in0=mo[:, :], in1=xt[:, cs], op=ALU.add),
                             reads=[t_mo], wadd=[t_xt])
                        yield
                    c.dma("pool", o_dst[rows, :], xt[:, :], reads=[t_xt])
                    yield

                ucnt = [0]

                def make_units(i):
                    units = []
                    for br in range(2):
                        for j in range(i + 1):
                            for hf in range(2):
                                units.append((br, j, hf))
                    return units

                def emit_scores(i, u, slot):
                    br, j, hf = u
                    near = j >= i - 1
                    e0 = 128 * (i - j)
                    o = stp[slot][:, :]
                    ts = t_stp[slot]
                    ks = slice(j * 128, (j + 1) * 128)
                    if br == 0:
                        c.op("pe", lambda e: e.matmul(o, lhsT=eall_bf[:, (j // 2) * 128:(j // 2 + 1) * 128],
                                                      rhs=mbT[:, hf * 4:(hf + 1) * 4, :].rearrange("p k t -> p (k t)"),
                                                      start=True, stop=False, skip_group_check=True),
                             reads=[t_eall, t_mbT], writes=[ts])
                        if near:
                            c.op("pe", lambda e: e.matmul(o, lhsT=ident_bf[:, :], rhs=bimg_hi[:, hf * 4:(hf + 1) * 4, e0:e0 + 128],
                                                          start=False, stop=False, skip_group_check=True),
                                 reads=[t_ident, t_bimg], writes=[ts])
                        for hq in range(4):
                            h = hf * 4 + hq
                            c.op("pe", lambda e, h=h, hq=hq: e.matmul(stp[slot][:, hq * 128:(hq + 1) * 128], lhsT=kaT2[:, h // 2, ks],
                                                                      rhs=qpad[:, h // 2, h % 2, :], start=False, stop=(hq == 3),
                                                                      skip_group_check=True),
                                 reads=[t_kaT2, t_qpad], writes=[ts])
                    else:
                        c.op("pe", lambda e: e.matmul(o, lhsT=kbT[:, ks], rhs=qbT[:, hf * 4:(hf + 1) * 4, :].rearrange("p k t -> p (k t)"),
                                                      start=True, stop=False, skip_group_check=True),
                             reads=[t_kbT[j], t_qbT], writes=[ts])
                        if near:
                            c.op("pe", lambda e: e.matmul(o, lhsT=ident_bf[:, :], rhs=bimg_hi[:, 8 + hf * 4:8 + (hf + 1) * 4, e0:e0 + 128],
                                                          start=False, stop=False, skip_group_check=True),
                                 reads=[t_ident, t_bimg], writes=[ts])
                        c.op("pe", lambda e: e.matmul(o, lhsT=mbias[i % 2][:, ks], rhs=irep_bf[:, :], start=False, stop=True,
                                                      skip_group_check=True),
                             reads=[t_mbias[i % 2], t_irep], writes=[ts])

                def emit_exp(slot):
                    c.op("act", lambda e: e.activation(out=pT[slot][:, :], in_=stp[slot][:, :], func=AF.Exp, scale=0.125),
                         reads=[t_stp[slot]], writes=[t_pT[slot]])

                def emit_pv(i, u, slot):
                    br, j, hf = u
                    for hq in range(4):
                        h = hf * 4 + hq
                        rhs = v1a[:, j, h, :] if br == 0 else v1b[:, j, :]
                        tv = t_v1a[j] if br == 0 else t_v1b[j]
                        c.op("pe", lambda e, hq=hq, rhs=rhs: e.matmul(acc[hf][:, hq * 65:hq * 65 + 65], lhsT=pT[slot][:, hq * 128:(hq + 1) * 128],
                                                                      rhs=rhs, start=(j == 0 and hq == 0), stop=(j == i and hq == 3),
                                                                      skip_group_check=True),
                             reads=[t_pT[slot], tv, t_ones], writes=[t_acc[hf]])
                    if j == i:
                        c.op("act", lambda e: e.activation(out=accs[br][:, hf * 260:(hf + 1) * 260], in_=acc[hf][:, 0:260], func=AF.Copy),
                             reads=[t_acc[hf]], wadd=[t_accs[br]])

                def interleave(gens):
                    gens = list(gens)
                    while gens:
                        for g in list(gens):
                            try:
                                next(g)
                                yield
                            except StopIteration:
                                gens.remove(g)

                def count_A(i):
                    nk = 128 * (i + 1)
                    return 3 + 4 * ((nk + 511) // 512) + (NBIS if i >= 2 else 0)

                for _ in gen_A(0):
                    pass
                prepB(0)
                for i in range(NT):
                    gens = []
                    nbg = 0
                    if i - 1 >= 0:
                        gens.append(gen_C(i - 1))
                        nbg += 12
                    if i + 1 < NT:
                        gens.append(gen_A(i + 1))
                        nbg += count_A(i + 1)
                    bg = interleave(gens)
                    units = make_units(i)
                    nU = len(units)
                    quota = -(-nbg // nU) if nU else nbg
                    slots = [(ucnt[0] + k) % 3 for k in range(nU)]
                    ucnt[0] += nU
                    emit_scores(i, units[0], slots[0])
                    for k in range(nU):
                        if k + 1 < nU:
                            emit_scores(i, units[k + 1], slots[k + 1])
                            if k + 2 == nU and i + 1 < NT:
                                prepB(i + 1)
                        emit_exp(slots[k])
                        emit_pv(i, units[k], slots[k])
                        for _ in range(quota):
                            try:
                                next(bg)
                            except StopIteration:
                                break
                    for _ in bg:
                        pass
                for _ in gen_C(NT - 1):
                    pass
                c.barrier()
        c.finish()
    return nc


_CACHE = {}


def kernel(x, norm_g, w_in, q_norm_a, k_norm_a, q_norm_b, k_norm_b, w_branch_a, w_branch_b, w_out, rel_bias):
    if "nc" not in _CACHE:
        _CACHE["nc"] = build_program()
    nc = _CACHE["nc"]
    f = lambda a: np.ascontiguousarray(np.asarray(a, dtype=np.float32))
    shared = {"norm_g": f(norm_g), "w_in": f(w_in), "q_norm_a": f(q_norm_a), "k_norm_a": f(k_norm_a),
              "q_norm_b": f(q_norm_b), "k_norm_b": f(k_norm_b), "w_branch_a": f(w_branch_a),
              "w_branch_b": f(w_branch_b), "w_out": f(w_out), "rel_bias": f(rel_bias)}
    shared.update(_host_consts())
    xs = f(x)
    in_maps = []
    for b in range(8):
        m = dict(shared)
        m["x"] = xs[b]
        in_maps.append(m)
    res = run_bass_kernel_spmd(nc, in_maps, core_ids=list(range(8)))
    return np.stack([np.asarray(r["out"]) for r in res.results], axis=0).astype(np.float32)
```

```python
import math
from contextlib import ExitStack

import numpy as np
import concourse.bass as bass
import concourse.mybir as mybir
from concourse.bass_utils import run_bass_kernel_spmd

F32 = mybir.dt.float32
BF16 = mybir.dt.bfloat16
ALU = mybir.AluOpType
AF = mybir.ActivationFunctionType
AX = mybir.AxisListType

S = 4096
D = 1024
NT = S // 128
DIN = 5572
DEPTH = 2
EPS = 1e-6
NEG = -30000.0
QS_W = 4352
NBIS = 22
W0 = 1024.0
TOPK = 256.0
MID0 = 2.0 ** -13
ACT_SHARE_NUM, ACT_SHARE_DEN = 0, 8

C_QA, C_KA, C_VA, C_GA = 0, 512, 1024, 1536
C_QB, C_KB, C_VB, C_GB = 2048, 2560, 2624, 2688
C_QI, C_KI, C_WI, C_MA, C_MB = 3200, 3456, 3520, 3524, 4548


class T:
    __slots__ = ("w", "r")

    def __init__(self):
        self.w = {}
        self.r = {}


class Ctx:
    RING = 8

    def __init__(self, nc, stack):
        self.nc = nc
        self.eng = {"pe": nc.tensor, "act": nc.scalar, "dve": nc.vector, "pool": nc.gpsimd, "sp": nc.sync}
        self.sem = {}
        self.cnt = {}
        for k in ("pe", "act", "dve", "pool"):
            self.sem[k] = stack.enter_context(nc.semaphore("s_" + k))
            self.cnt[k] = 0
        self.waited = {k: {} for k in self.eng}
        self.rings = {}
        self.ringpos = {}
        for q in ("sp", "pool"):
            self.rings[q] = []
            for i in range(self.RING):
                key = "d_%s_%d" % (q, i)
                self.sem[key] = stack.enter_context(nc.semaphore(key))
                self.cnt[key] = 0
                self.rings[q].append(key)
            self.ringpos[q] = 0

    def _wait(self, e, marks):
        need = {}
        for (k, v) in marks:
            if need.get(k, 0) < v:
                need[k] = v
        wd = self.waited[e]
        for k, v in need.items():
            if wd.get(k, 0) < v:
                self.eng[e].wait_ge(self.sem[k], v)
                wd[k] = v

    def _deps(self, e, reads, writes):
        marks = []
        for t in reads:
            for m in t.w.items():
                if m[0] == e and e == "pe":
                    continue
                marks.append(m)
        for t in writes:
            for m in t.w.items():
                if m[0] == e:
                    continue
                marks.append(m)
            for m in t.r.items():
                if m[0] == e:
                    continue
                marks.append(m)
        return marks

    def op(self, e, fn, reads=(), writes=(), wadd=()):
        self._wait(e, self._deps(e, reads, tuple(writes) + tuple(wadd)))
        ins = fn(self.eng[e])
        self.cnt[e] += 1
        ins.then_inc(self.sem[e], 1)
        v = self.cnt[e]
        for t in reads:
            t.r[e] = v
        for t in writes:
            t.w = {e: v}
            t.r = {}
        for t in wadd:
            t.w[e] = v
        return ins

    def dma(self, q, out, in_, reads=(), writes=(), wadd=(), **kw):
        key = self.rings[q][self.ringpos[q] % self.RING]
        self.ringpos[q] += 1
        marks = self._deps("dma", reads, tuple(writes) + tuple(wadd))
        if self.cnt[key] > 0:
            marks.append((key, self.cnt[key]))
        self._wait(q, marks)
        ins = self.eng[q].dma_start(out=out, in_=in_, **kw)
        self.cnt[key] += 16
        ins.then_inc(self.sem[key], 16)
        v = self.cnt[key]
        for t in reads:
            t.r[key] = v
        for t in writes:
            t.w = {key: v}
            t.r = {}
        for t in wadd:
            t.w[key] = v

    def all_marks(self):
        marks = []
        for k, v in self.cnt.items():
            if v > 0:
                marks.append((k, v))
        return marks

    def barrier(self):
        marks = self.all_marks()
        for e in ("pe", "act", "dve", "pool", "sp"):
            self._wait(e, [m for m in marks if m[0] != e])

    def finish(self):
        self._wait("sp", self.all_marks())


def _bucket_table():
    n = np.arange(0, 256)
    max_exact = 16
    nf = np.maximum(n, 1).astype(np.float32)
    large = max_exact + (np.log(nf / np.float32(max_exact)) / np.float32(math.log(128 / max_exact))
                         * np.float32(32 - max_exact)).astype(np.int32)
    large = np.minimum(large, 31)
    return np.where(n < max_exact, n, large)


def _host_consts():
    bk = _bucket_table()
    grev = np.zeros((33, 384), np.float32)
    for m in range(384):
        dist = 255 - m
        if dist >= 0:
            grev[bk[dist], m] = 1.0
        else:
            grev[32, m] = 1.0
    ident = np.eye(128, dtype=np.float32)
    irep = np.concatenate([ident] * 4, axis=1)
    t = np.arange(128)[:, None]
    s = np.arange(128)[None, :]
    causalneg = np.where(s <= t, 0.0, -1e30).astype(np.float32)
    eall = np.zeros((16, 16, 128), np.float32)
    for n in range(16):
        eall[n, n, :] = 1.0
    negrow = np.full((1, 16), NEG * 8.0, np.float32)
    return {"c_grev": grev, "c_ident": ident, "c_irep": irep, "c_causal": causalneg,
            "c_eall": eall.reshape(16, 2048), "c_negrow": negrow}


def build_program():
    nc = bass.Bass("TRN2", target_bir_lowering=False)
    din = lambda n, s, d=F32: nc.dram_tensor(n, s, d, kind="ExternalInput").ap()
    x = din("x", [S, D])
    norm_g = din("norm_g", [DEPTH, D])
    w_in = din("w_in", [DEPTH, D, DIN])
    qn_a = din("q_norm_a", [DEPTH, 64])
    kn_a = din("k_norm_a", [DEPTH, 64])
    qn_b = din("q_norm_b", [DEPTH, 64])
    kn_b = din("k_norm_b", [DEPTH, 64])
    w_bra = din("w_branch_a", [DEPTH, 512, D])
    w_brb = din("w_branch_b", [DEPTH, 512, D])
    w_out = din("w_out", [DEPTH, D, D])
    rel_bias = din("rel_bias", [32, 16])
    c_grev = din("c_grev", [33, 384])
    c_ident = din("c_ident", [128, 128])
    c_irep = din("c_irep", [128, 512])
    c_causal = din("c_causal", [128, 128])
    c_eall = din("c_eall", [16, 2048])
    c_negrow = din("c_negrow", [1, 16])
    out = nc.dram_tensor("out", [S, D], F32, kind="ExternalOutput").ap()
    scr = lambda n, s, d: nc.dram_tensor(n, s, d, kind="Internal").ap()
    qs = scr("qs", [S, QS_W], BF16)
    wis = scr("wis", [S, 4], F32)
    kaT_s = scr("kaT_s", [4, 128, S], BF16)
    x1 = scr("x1", [S, D], F32)

    top = ExitStack()
    with top:
        c = Ctx(nc, top)

        uid = [0]

        def sbt(st, name, shape, dt):
            uid[0] += 1
            return st.enter_context(nc.sbuf_tensor("%s_%d" % (name, uid[0]), shape, dt))

        def pst(st, name, shape, dt):
            uid[0] += 1
            return st.enter_context(nc.psum_tensor("%s_%d" % (name, uid[0]), shape, dt))

        ident_bf = sbt(top, "ident_bf", [128, 128], BF16); t_ident = T()
        irep_bf = sbt(top, "irep_bf", [128, 512], BF16); t_irep = T()
        causal = sbt(top, "causal", [128, 128], F32); t_causal = T()
        eall_bf = sbt(top, "eall_bf", [128, 2048], BF16); t_eall = T()
        bimg_hi = sbt(top, "bimg_hi", [128, 16, 256], BF16)
        t_bimg = T()
        v1a = sbt(top, "v1a", [128, NT, 8, 65], BF16); t_v1a = [T() for _ in range(NT)]
        v1b = sbt(top, "v1b", [128, NT, 65], BF16); t_v1b = [T() for _ in range(NT)]
        kbT = sbt(top, "kbT", [128, S], BF16); t_kbT = [T() for _ in range(NT)]
        kiT = sbt(top, "kiT", [128, S], BF16); t_kiT = [T() for _ in range(NT)]
        kmacc = sbt(top, "kmacc", [128, 4, 16], F32); t_kmacc = T()

        c.dma("pool", ident_bf[:], c_ident, writes=[t_ident])
        c.dma("pool", irep_bf[:], c_irep, writes=[t_irep])
        c.op("dve", lambda e: e.memset(eall_bf[:, :], 0.0), writes=[t_eall])
        c.dma("pool", eall_bf[0:16, :], c_eall, writes=[t_eall])
        t_kz = T()
        c.op("dve", lambda e: e.memset(kbT[64:128, :], 0.0), writes=[t_kz])
        c.op("dve", lambda e: e.memset(kiT[64:128, :], 0.0), writes=[t_kz])
        c.dma("sp", causal[:], c_causal, writes=[t_causal])
        t_ones = T()
        c.op("dve", lambda e: e.memset(v1a[:, :, :, 64:65], 1.0), writes=[t_ones])
        c.op("dve", lambda e: e.memset(v1b[:, :, 64:65], 1.0), writes=[t_ones])

        with ExitStack() as st:
            grev = sbt(st, "grev", [33, 384], F32); t_grev = T()
            reltab = sbt(st, "reltab", [33, 16], F32); t_rel = T()
            r31 = sbt(st, "r31", [32, 16], F32); t_r31 = T()
            bps = pst(st, "bps", [128, 2048], F32); t_bps = T()
            c.dma("sp", grev[:], c_grev, writes=[t_grev])
            c.dma("sp", reltab[0:32, :], rel_bias, writes=[t_rel])
            c.dma("sp", r31[:], rel_bias[31:32, :].to_broadcast([32, 16]), writes=[t_r31])
            c.op("dve", lambda e: e.tensor_tensor(out=reltab[0:32, :], in0=reltab[0:32, :], in1=r31[:], op=ALU.subtract),
                 reads=[t_r31, t_rel], writes=[t_rel])
            c.op("dve", lambda e: e.tensor_scalar(out=reltab[0:32, :], in0=reltab[0:32, :], scalar1=8.0, scalar2=None, op0=ALU.mult),
                 reads=[t_rel], writes=[t_rel])
            c.dma("sp", reltab[32:33, :], c_negrow, reads=[t_rel], writes=[t_rel])
            for half in range(2):
                for el in range(128):
                    e_ = half * 128 + el
                    c.op("pe", lambda e, el=el, e_=e_: e.matmul(bps[:, el * 16:(el + 1) * 16], lhsT=grev[0:33, 255 - e_:255 - e_ + 128],
                                                                 rhs=reltab[0:33, :], start=True, stop=True),
                         reads=[t_grev, t_rel], writes=[t_bps])
                src = bps[:, :].rearrange("p (e h) -> p h e", h=16)
                hi_v = bimg_hi[:, :, half * 128:(half + 1) * 128]
                c.op("dve", lambda e: e.tensor_copy(out=hi_v, in_=src), reads=[t_bps], wadd=[t_bimg])
            c.barrier()

        for l in range(DEPTH):
            x_src = x if l == 0 else x1
            o_dst = x1 if l == 0 else out
            with ExitStack() as st:
                w_bf = sbt(st, "w_bf", [128, 8, DIN], BF16); t_w = [T() for _ in range(8)]
                gbc = sbt(st, "gbc", [128, D], F32); t_gbc = T()
                gq_a = sbt(st, "gq_a", [128, 64], F32)
                gk_a = sbt(st, "gk_a", [128, 64], F32)
                gq_b = sbt(st, "gq_b", [128, 64], F32)
                gk_b = sbt(st, "gk_b", [128, 64], F32)
                t_gs = T()
                xb = [sbt(st, "xb%d" % i, [128, D], F32) for i in range(2)]; t_xb = [T(), T()]
                sqj = sbt(st, "sqj", [128, D], BF16); t_sqj = T()
                stat = sbt(st, "stat", [128, 8], F32); t_stat = T()
                hb = sbt(st, "hb", [128, D], BF16); t_hb = T()
                hT = [sbt(st, "hT%d" % i, [128, 8, 128], BF16) for i in range(2)]; t_hT = [T(), T()]
                qt = [sbt(st, "qt%d" % i, [128, QS_W], BF16) for i in range(2)]; t_qt = [T(), T()]
                sqf = sbt(st, "sqf", [128, 512], F32); t_sqf = T()
                hst = sbt(st, "hst", [128, 32], F32); t_hst = T()
                tmpf = sbt(st, "tmpf", [128, 512], F32); t_tmpf = T()
                kan = sbt(st, "kan", [128, 512], BF16); t_kan = T()
                kTt = [sbt(st, "kTt%d" % i, [128, 4, 128], BF16) for i in range(2)]; t_kTt = [T(), T()]
                kred = sbt(st, "kred", [128, 4], F32); t_kred = T()
                sm = sbt(st, "sm", [128, 128], BF16); t_sm = T()
                wit = [sbt(st, "witp%d" % i, [128, 4], F32) for i in range(2)]; t_wit = [T(), T()]
                tp = pst(st, "tp", [128, 1024], BF16); t_tp = T()
                tp2 = pst(st, "tp2", [128, 1024], BF16); t_tp2 = T()
                pj = [pst(st, "pj%d" % i, [128, 512], F32) for i in range(6)]; t_pj = [T() for _ in range(6)]

                for kc in range(8):
                    for (c0, c1) in ((0, 2048), (2048, 4096), (4096, DIN)):
                        c.dma("pool", w_bf[:, kc, c0:c1], w_in[l, kc * 128:(kc + 1) * 128, c0:c1], wadd=[t_w[kc]])
                c.dma("sp", gbc[:], norm_g[l:l + 1, :].to_broadcast([128, D]), writes=[t_gbc])
                for (g_sb, g_dr) in ((gq_a, qn_a), (gk_a, kn_a), (gq_b, qn_b), (gk_b, kn_b)):
                    c.dma("sp", g_sb[:], g_dr[l:l + 1, :].to_broadcast([128, 64]), wadd=[t_gs])
                c.op("dve", lambda e: e.memset(kmacc[:], 0.0), writes=[t_kmacc])

                pjn = [0]

                def project(i, col0, ncols):
                    b = pjn[0] % 6
                    pjn[0] += 1
                    for kc in range(8):
                        c.op("pe", lambda e, kc=kc: e.matmul(pj[b][:, 0:ncols], lhsT=hT[i % 2][:, kc, :],
                                                              rhs=w_bf[:, kc, col0:col0 + ncols],
                                                              start=(kc == 0), stop=(kc == 7)),
                             reads=[t_hT[i % 2], t_w[kc]], writes=[t_pj[b]])
                    return pj[b], t_pj[b]

                def rsqrt_small(ap_in, ap_out, n, inv_n):
                    c.op("dve", lambda e: e.tensor_scalar(out=ap_out, in0=ap_in, scalar1=inv_n, scalar2=EPS,
                                                          op0=ALU.mult, op1=ALU.add), reads=[t_hst], writes=[t_hst])
                    c.op("act", lambda e: e.activation(out=ap_out, in_=ap_out, func=AF.Ln), reads=[t_hst], writes=[t_hst])
                    c.op("act", lambda e: e.activation(out=ap_out, in_=ap_out, func=AF.Exp, scale=-0.5),
                         reads=[t_hst], writes=[t_hst])

                def headnorm(ps_ap, t_ps, H, g_sb, out_ap, t_out, wadd_out=False):
                    W = H * 64
                    c.op("act", lambda e: e.activation(out=sqf[:, 0:W], in_=ps_ap, func=AF.Square),
                         reads=[t_ps], writes=[t_sqf])
                    c.op("dve", lambda e: e.tensor_reduce(out=hst[:, 0:H], in_=sqf[:, 0:W].rearrange("p (h d) -> p h d", d=64),
                                                          axis=AX.X, op=ALU.add), reads=[t_sqf], writes=[t_hst])
                    rsqrt_small(hst[:, 0:H], hst[:, 0:H], H, 1.0 / 64.0)
                    c.op("dve", lambda e: e.tensor_tensor(out=tmpf[:, 0:W].rearrange("p (h d) -> p h d", d=64),
                                                          in0=ps_ap.rearrange("p (h d) -> p h d", d=64),
                                                          in1=hst[:, 0:H].unsqueeze(2).to_broadcast([128, H, 64]), op=ALU.mult),
                         reads=[t_ps, t_hst], writes=[t_tmpf])
                    kw = dict(wadd=[t_out]) if wadd_out else dict(writes=[t_out])
                    c.op("dve", lambda e: e.tensor_tensor(out=out_ap.rearrange("p (h d) -> p h d", d=64),
                                                          in0=tmpf[:, 0:W].rearrange("p (h d) -> p h d", d=64),
                                                          in1=g_sb[:, :].unsqueeze(1).to_broadcast([128, H, 64]), op=ALU.mult),
                         reads=[t_tmpf, t_gs], **kw)

                def norm_stage(i):
                    rows = slice(i * 128, (i + 1) * 128)
                    xt, t_xt = xb[i % 2], t_xb[i % 2]
                    c.dma("sp", xt[:], x_src[rows, :], writes=[t_xt])
                    c.op("act", lambda e: e.activation(out=sqj[:], in_=xt[:], func=AF.Square, accum_out=stat[:, 0:1]),
                         reads=[t_xt], writes=[t_sqj, t_stat])
                    c.op("dve", lambda e: e.tensor_scalar(out=stat[:, 1:2], in0=stat[:, 0:1], scalar1=1.0 / D, scalar2=EPS,
                                                          op0=ALU.mult, op1=ALU.add), reads=[t_stat], writes=[t_stat])
                    c.op("act", lambda e: e.activation(out=stat[:, 2:3], in_=stat[:, 1:2], func=AF.Ln), reads=[t_stat], writes=[t_stat])
                    c.op("act", lambda e: e.activation(out=stat[:, 3:4], in_=stat[:, 2:3], func=AF.Exp, scale=-0.5),
                         reads=[t_stat], writes=[t_stat])
                    c.op("dve", lambda e: e.scalar_tensor_tensor(out=hb[:], in0=xt[:], scalar=stat[:, 3:4], in1=gbc[:],
                                                                 op0=ALU.mult, op1=ALU.mult),
                         reads=[t_xt, t_stat, t_gbc], writes=[t_hb])
                    for kc in range(8):
                        c.op("pe", lambda e, kc=kc: e.transpose(tp[:, kc * 128:(kc + 1) * 128], hb[:, kc * 128:(kc + 1) * 128], ident_bf[:]),
                             reads=[t_hb, t_ident], writes=[t_tp])
                    c.op("act", lambda e: e.activation(out=hT[i % 2][:, :, :], in_=tp[:, :].rearrange("p (k t) -> p k t", t=128), func=AF.Copy),
                         reads=[t_tp], writes=[t_hT[i % 2]])

                norm_stage(0)
                for i in range(NT):
                    rows = slice(i * 128, (i + 1) * 128)
                    q, t_q = qt[i % 2], t_qt[i % 2]
                    if i + 1 < NT:
                        norm_stage(i + 1)
                    ps, tps = project(i, C_QA, 512)
                    headnorm(ps[:, 0:512], tps, 8, gq_a, q[:, 0:512], t_q)
                    ps, tps = project(i, C_KA, 512)
                    headnorm(ps[:, 0:512], tps, 8, gk_a, kan[:, :], t_kan)
                    kt, t_kt = kTt[i % 2], t_kTt[i % 2]
                    for p4 in range(4):
                        c.op("pe", lambda e, p4=p4: e.transpose(tp2[:, p4 * 128:(p4 + 1) * 128], kan[:, p4 * 128:(p4 + 1) * 128], ident_bf[:]),
                             reads=[t_kan, t_ident], writes=[t_tp2])
                    c.op("act", lambda e: e.activation(out=kt[:, :, :], in_=tp2[:, 0:512].rearrange("p (k t) -> p k t", t=128), func=AF.Copy),
                         reads=[t_tp2], writes=[t_kt])
                    for p4 in range(4):
                        c.dma("pool", kaT_s[p4, :, i * 128:(i + 1) * 128], kt[:, p4, :], reads=[t_kt])
                    c.op("dve", lambda e: e.tensor_reduce(out=kred[:, :], in_=kt[:, :, :], axis=AX.X, op=ALU.add),
                         reads=[t_kt], writes=[t_kred])
                    nblk = i // 2
                    c.op("dve", lambda e: e.tensor_tensor(out=kmacc[:, :, nblk], in0=kmacc[:, :, nblk], in1=kred[:, :], op=ALU.add),
                         reads=[t_kred, t_kmacc], writes=[t_kmacc])
                    ps, tps = project(i, C_QB, 512)
                    headnorm(ps[:, 0:512], tps, 8, gq_b, q[:, 512:1024], t_q, wadd_out=True)
                    ps, tps = project(i, C_KB, 128)
                    headnorm(ps[:, 0:64], tps, 1, gk_b, sm[:, 0:64], t_sm)
                    c.op("dve", lambda e: e.tensor_copy(out=v1b[:, i, 0:64], in_=ps[:, 64:128]), reads=[tps], writes=[t_v1b[i]])
                    ps2, tps2 = project(i, C_QI, 324)
                    c.op("dve", lambda e: e.tensor_copy(out=q[:, 4096:4352], in_=ps2[:, 0:256]), reads=[tps2], wadd=[t_q])
                    c.op("dve", lambda e: e.tensor_copy(out=sm[:, 64:128], in_=ps2[:, 256:320]), reads=[tps2], wadd=[t_sm])
                    c.op("dve", lambda e: e.tensor_copy(out=wit[i % 2][:, :], in_=ps2[:, 320:324]), reads=[tps2], writes=[t_wit[i % 2]])
                    c.dma("pool", wis[rows, :], wit[i % 2][:, :], reads=[t_wit[i % 2]])
                    c.op("pe", lambda e: e.transpose(tp2[0:64, 512:640], sm[:, 0:64], ident_bf[:]), reads=[t_sm, t_ident], writes=[t_tp2])
                    c.op("pe", lambda e: e.transpose(tp2[0:64, 640:768], sm[:, 64:128], ident_bf[:]), reads=[t_sm, t_ident], writes=[t_tp2])
                    c.op("act", lambda e: e.activation(out=kbT[0:64, i * 128:(i + 1) * 128], in_=tp2[0:64, 512:640], func=AF.Copy),
                         reads=[t_tp2], writes=[t_kbT[i]])
                    c.op("act", lambda e: e.activation(out=kiT[0:64, i * 128:(i + 1) * 128], in_=tp2[0:64, 640:768], func=AF.Copy),
                         reads=[t_tp2], writes=[t_kiT[i]])
                    ps, tps = project(i, C_VA, 512)
                    c.op("act", lambda e: e.activation(out=v1a[:, i, :, 0:64], in_=ps[:, 0:512].rearrange("p (h d) -> p h d", d=64), func=AF.Copy),
                         reads=[tps], writes=[t_v1a[i]])
                    ps, tps = project(i, C_GA, 512)
                    c.op("act", lambda e: e.activation(out=q[:, 1024:1536], in_=ps[:, 0:512], func=AF.Silu), reads=[tps], wadd=[t_q])
                    ps, tps = project(i, C_GB, 512)
                    c.op("act", lambda e: e.activation(out=q[:, 1536:2048], in_=ps[:, 0:512], func=AF.Silu), reads=[tps], wadd=[t_q])
                    for gi, cm in enumerate((C_MA, C_MA + 512, C_MB, C_MB + 512)):
                        ps, tps = project(i, cm, 512)
                        c.op("act", lambda e, gi=gi: e.activation(out=q[:, 2048 + gi * 512:2048 + (gi + 1) * 512], in_=ps[:, 0:512], func=AF.Sigmoid),
                             reads=[tps], wadd=[t_q])
                    c.dma("pool", qs[rows, :], q[:, :], reads=[t_q])
                c.barrier()

            with ExitStack() as st:
                kaT2 = sbt(st, "kaT2", [128, 4, S], BF16); t_kaT2 = T()
                km_bf = sbt(st, "km_bf", [128, 4, 16], BF16); t_km = T()
                wbra = sbt(st, "wbra", [128, 4, D], BF16); t_wbra = T()
                wbrb = sbt(st, "wbrb", [128, 4, D], BF16); t_wbrb = T()
                wo = sbt(st, "wo", [128, 8, D], BF16); t_wo = T()
                qm = sbt(st, "qm", [128, 1024], BF16); t_qm = T()
                qmi = sbt(st, "qmi", [128, 256], BF16); t_qmi = T()
                gt = sbt(st, "gt", [128, 3072], BF16); t_gt = T()
                wit2 = [sbt(st, "wit2%d" % k, [128, 4], F32) for k in range(2)]; t_wit2 = [T(), T()]
                xt = sbt(st, "xt", [128, D], F32); t_xt = T()
                qpad = sbt(st, "qpad", [128, 4, 2, 128], BF16); t_qpad = T()
                qbT = sbt(st, "qbT", [128, 8, 128], BF16); t_qbT = T()
                qiT = [sbt(st, "qiT%d" % k, [128, 4, 128], BF16) for k in range(2)]; t_qiT = [T(), T()]
                score = sbt(st, "score", [128, S], F32); t_score = T()
                mbias = [sbt(st, "mbias%d" % k, [128, S], BF16) for k in range(2)]; t_mbias = [T(), T()]
                rl = sbt(st, "rl", [128, 512], F32); t_rl = T()
                bs = sbt(st, "bs", [128, 8], F32)
                t_cnt = T(); t_tmp = T(); t_mid = T(); t_thr = T()
                sg = sbt(st, "sg", [128, 2], F32); t_sg = T()
                gatem = sbt(st, "gatem", [128, 8, 16], F32); t_gatem = T()
                top8 = sbt(st, "top8", [128, 8, 8], F32); t_top8 = T()
                thr3 = sbt(st, "thr3", [128, 8], F32); t_thr3 = T()
                mbf = sbt(st, "mbf", [128, 8, 16], F32); t_mbf = T()
                mb16 = sbt(st, "mb16", [128, 8, 16], BF16); t_mb16 = T()
                mbT = sbt(st, "mbT", [128, 8, 128], BF16); t_mbT = T()
                pT = [sbt(st, "pT%d" % k, [128, 512], BF16) for k in range(3)]; t_pT = [T(), T(), T()]
                accs = [sbt(st, "accs%d" % k, [128, 520], F32) for k in range(2)]; t_accs = [T(), T()]
                rec = sbt(st, "rec", [128, 8], F32); t_rec = T()
                ytmp = sbt(st, "ytmp", [128, 512], F32); t_ytmp = T()
                yag = sbt(st, "yag", [128, 512], BF16); t_yag = T()
                ybg = sbt(st, "ybg", [128, 512], BF16); t_ybg = T()
                yaT = sbt(st, "yaT", [128, 4, 128], BF16); t_yaT = T()
                ybT = sbt(st, "ybT", [128, 4, 128], BF16); t_ybT = T()
                mtmp2 = sbt(st, "mtmp2", [128, 512], F32); t_mtmp2 = T()
                mrg = sbt(st, "mrg", [128, D], BF16); t_mrg = T()
                mT = sbt(st, "mT", [128, 8, 128], BF16); t_mT = T()
                stp = [pst(st, "stp%d" % k, [128, 512], F32) for k in range(3)]; t_stp = [T(), T(), T()]
                acc = [pst(st, "acc%d" % k, [128, 512], F32) for k in range(2)]; t_acc = [T(), T()]
                m0 = pst(st, "m0", [128, 512], F32); t_m0 = T()
                m1 = pst(st, "m1", [128, 1024], BF16); t_m1 = T()
                mo = pst(st, "mo", [128, 512], F32); t_mo = T()

                for p4 in range(4):
                    c.dma("sp", kaT2[:, p4, :], kaT_s[p4, :, :], wadd=[t_kaT2])
                for kc in range(4):
                    c.dma("pool", wbra[:, kc, :], w_bra[l, kc * 128:(kc + 1) * 128, :], wadd=[t_wbra])
                    c.dma("pool", wbrb[:, kc, :], w_brb[l, kc * 128:(kc + 1) * 128, :], wadd=[t_wbrb])
                for kc in range(8):
                    c.dma("pool", wo[:, kc, :], w_out[l, kc * 128:(kc + 1) * 128, :], wadd=[t_wo])
                c.op("dve", lambda e: e.tensor_scalar(out=km_bf[:, :, :], in0=kmacc[:, :, :], scalar1=1.0 / 256.0, scalar2=None, op0=ALU.mult),
                     reads=[t_kmacc], writes=[t_km])
                c.op("dve", lambda e: e.memset(qpad[:, :, :, :], 0.0), writes=[t_qpad])
                c.op("dve", lambda e: e.memset(qbT[:, :, :], 0.0), writes=[t_qbT])
                c.op("dve", lambda e: e.memset(mbT[:, :, :], 0.0), writes=[t_mbT])
                for k in range(2):
                    c.op("dve", lambda e, k=k: e.memset(qiT[k][:, :, :], 0.0), writes=[t_qiT[k]])

                def rows_of(i):
                    return slice(i * 128, (i + 1) * 128)

                def gen_A(i):
                    rows = rows_of(i)
                    nk = 128 * (i + 1)
                    b = i % 2
                    c.dma("sp", qmi[:, :], qs[rows, 4096:4352], writes=[t_qmi])
                    c.dma("sp", wit2[b][:, :], wis[rows, :], writes=[t_wit2[b]])
                    for h in range(4):
                        c.op("pe", lambda e, h=h: e.transpose(m1[0:64, h * 128:(h + 1) * 128], qmi[:, h * 64:(h + 1) * 64], ident_bf[:]),
                             reads=[t_qmi, t_ident], writes=[t_m1])
                    c.op("act", lambda e: e.activation(out=qiT[b][0:64, :, :], in_=m1[0:64, 0:512].rearrange("p (k t) -> p k t", t=128), func=AF.Copy),
                         reads=[t_m1], wadd=[t_qiT[b]])
                    yield
                    for c0 in range(0, nk, 512):
                        n = min(512, nk - c0)
                        kdeps = [t_kiT[jj] for jj in range(c0 // 128, (c0 + n) // 128)]
                        for hh in range(4):
                            c.op("pe", lambda e, hh=hh: e.matmul(m0[:, 0:n], lhsT=qiT[b][:, hh, :], rhs=kiT[:, c0:c0 + n], start=True, stop=True),
                                 reads=[t_qiT[b]] + kdeps, writes=[t_m0])
                            c.op("act", lambda e: e.activation(out=rl[:, 0:n], in_=m0[:, 0:n], func=AF.Relu),
                                 reads=[t_m0], writes=[t_rl])
                            if hh == 0:
                                c.op("dve", lambda e: e.tensor_scalar(out=score[:, c0:c0 + n], in0=rl[:, 0:n], scalar1=wit2[b][:, 0:1],
                                                                      scalar2=None, op0=ALU.mult),
                                     reads=[t_rl, t_wit2[b]], writes=[t_score])
                            else:
                                c.op("dve", lambda e, hh=hh: e.scalar_tensor_tensor(out=score[:, c0:c0 + n], in0=rl[:, 0:n],
                                                                                   scalar=wit2[b][:, hh:hh + 1], in1=score[:, c0:c0 + n],
                                                                                   op0=ALU.mult, op1=ALU.add),
                                     reads=[t_rl, t_wit2[b], t_score], writes=[t_score])
                            yield
                    c.op("dve", lambda e: e.tensor_tensor(out=score[:, i * 128:(i + 1) * 128], in0=score[:, i * 128:(i + 1) * 128],
                                                          in1=causal[:, :], op=ALU.add),
                         reads=[t_score, t_causal], writes=[t_score])
                    if i >= 2:
                        n_act = 128 * (((i + 1) * ACT_SHARE_NUM) // ACT_SHARE_DEN)
                        n1 = nk - n_act
                        c.op("dve", lambda e: e.memset(bs[:, 2:3], MID0), writes=[t_mid])
                        w = W0
                        for k in range(NBIS):
                            c.op("dve", lambda e: e.tensor_scalar(out=mbias[b][:, 0:n1], in0=score[:, 0:n1], scalar1=bs[:, 2:3], scalar2=None,
                                                                  op0=ALU.is_ge, op1=ALU.add, accum_out=bs[:, 0:1]),
                                 reads=[t_score, t_mid], writes=[t_cnt], wadd=[t_mbias[b]])
                            if n_act > 0:
                                c.op("act", lambda e: e.activation(out=mbias[b][:, n1:nk], in_=score[:, n1:nk], func=AF.Sign,
                                                                   bias=bs[:, 2:3], scale=-1.0, accum_out=sg[:, 0:1]),
                                     reads=[t_score, t_mid], writes=[t_sg], wadd=[t_mbias[b]])
                                c.op("dve", lambda e: e.scalar_tensor_tensor(out=bs[:, 4:5], in0=bs[:, 0:1], scalar=2.0, in1=sg[:, 0:1],
                                                                             op0=ALU.mult, op1=ALU.subtract),
                                     reads=[t_cnt, t_sg], writes=[t_tmp])
                                c.op("dve", lambda e: e.tensor_scalar(out=bs[:, 1:2], in0=bs[:, 4:5], scalar1=2.0 * TOPK - n_act, scalar2=0.5,
                                                                      op0=ALU.is_ge, op1=ALU.subtract), reads=[t_tmp], writes=[t_tmp])
                            else:
                                c.op("dve", lambda e: e.tensor_scalar(out=bs[:, 1:2], in0=bs[:, 0:1], scalar1=TOPK, scalar2=0.5,
                                                                      op0=ALU.is_ge, op1=ALU.subtract), reads=[t_cnt], writes=[t_tmp])
                            c.op("dve", lambda e, w=w: e.scalar_tensor_tensor(out=bs[:, 2:3], in0=bs[:, 1:2], scalar=w, in1=bs[:, 2:3],
                                                                              op0=ALU.mult, op1=ALU.add), reads=[t_tmp, t_mid], writes=[t_mid])
                            w = w / 2.0
                            yield
                        c.op("dve", lambda e, w=w: e.tensor_scalar(out=bs[:, 3:4], in0=bs[:, 2:3], scalar1=-w, scalar2=None, op0=ALU.add),
                             reads=[t_mid], writes=[t_thr])
                    else:
                        c.op("dve", lambda e: e.memset(bs[:, 3:4], -1e29), writes=[t_thr])
                    c.op("dve", lambda e: e.tensor_scalar(out=mbias[b][:, 0:nk], in0=score[:, 0:nk], scalar1=bs[:, 3:4], scalar2=NEG,
                                                          op0=ALU.is_lt, op1=ALU.mult), reads=[t_score, t_thr], writes=[t_mbias[b]])
                    yield

                def prepB(i):
                    rows = rows_of(i)
                    qblk = i // 2
                    c.dma("sp", qm[:, :], qs[rows, 0:1024], writes=[t_qm])
                    for p4 in range(4):
                        c.op("pe", lambda e, p4=p4: e.transpose(m1[:, p4 * 128:(p4 + 1) * 128], qm[:, p4 * 128:(p4 + 1) * 128], ident_bf[:]),
                             reads=[t_qm, t_ident], writes=[t_m1])
                    c.op("act", lambda e: e.activation(out=qpad[0:64, :, 0, :], in_=m1[0:64, 0:512].rearrange("p (k t) -> p k t", t=128), func=AF.Copy),
                         reads=[t_m1], wadd=[t_qpad])
                    c.op("act", lambda e: e.activation(out=qpad[64:128, :, 1, :], in_=m1[64:128, 0:512].rearrange("p (k t) -> p k t", t=128), func=AF.Copy),
                         reads=[t_m1], wadd=[t_qpad])
                    for h in range(8):
                        c.op("pe", lambda e, h=h: e.transpose(m1[0:64, h * 128:(h + 1) * 128], qm[:, 512 + h * 64:512 + (h + 1) * 64], ident_bf[:]),
                             reads=[t_qm, t_ident], writes=[t_m1])
                    c.op("act", lambda e: e.activation(out=qbT[0:64, :, :], in_=m1[0:64, :].rearrange("p (k t) -> p k t", t=128), func=AF.Copy),
                         reads=[t_m1], wadd=[t_qbT])
                    for h in range(8):
                        c.op("pe", lambda e, h=h: e.matmul(m0[:, h * 16:(h + 1) * 16], lhsT=qpad[:, h // 2, h % 2, :], rhs=km_bf[:, h // 2, :],
                                                           start=True, stop=True),
                             reads=[t_qpad, t_km], writes=[t_m0])
                    c.op("dve", lambda e: e.tensor_copy(out=gatem[:, :, :], in_=m0[:, 0:128].rearrange("p (h n) -> p h n", n=16)),
                         reads=[t_m0], writes=[t_gatem])
                    c.op("dve", lambda e: e.memset(gatem[:, :, qblk:16], -1e30), reads=[t_gatem], writes=[t_gatem])
                    for h in range(8):
                        c.op("dve", lambda e, h=h: e.max(out=top8[:, h, :], in_=gatem[:, h, :]), reads=[t_gatem], wadd=[t_top8])
                    c.op("dve", lambda e: e.tensor_scalar(out=thr3[:, :].unsqueeze(2), in0=top8[:, :, 2:3], scalar1=-1e29, scalar2=None, op0=ALU.max),
                         reads=[t_top8], writes=[t_thr3])
                    c.op("dve", lambda e: e.tensor_tensor(out=mbf[:, :, :], in0=gatem[:, :, :],
                                                          in1=thr3[:, :].unsqueeze(2).to_broadcast([128, 8, 16]), op=ALU.is_lt),
                         reads=[t_gatem, t_thr3, t_top8], writes=[t_mbf])
                    c.op("dve", lambda e: e.tensor_scalar(out=mb16[:, :, :], in0=mbf[:, :, :], scalar1=NEG, scalar2=None, op0=ALU.mult),
                         reads=[t_mbf], writes=[t_mb16])
                    c.op("dve", lambda e: e.memset(mb16[:, :, qblk:qblk + 1], 0.0), reads=[t_mb16], writes=[t_mb16])
                    for h in range(8):
                        c.op("pe", lambda e, h=h: e.transpose(m1[0:16, h * 128:(h + 1) * 128], mb16[:, h, :], ident_bf[:]),
                             reads=[t_mb16, t_ident], writes=[t_m1])
                    c.op("act", lambda e: e.activation(out=mbT[0:16, :, :], in_=m1[0:16, :].rearrange("p (k t) -> p k t", t=128), func=AF.Copy),
                         reads=[t_m1], wadd=[t_mbT])

                def gen_C(i):
                    rows = rows_of(i)
                    c.dma("sp", gt[:, :], qs[rows, 1024:4096], writes=[t_gt])
                    c.dma("sp", xt[:, :], x_src[rows, :], writes=[t_xt])
                    for (br, g0, y_out, t_y) in ((0, 0, yag, t_yag), (1, 512, ybg, t_ybg)):
                        av = accs[br][:, :].rearrange("p (h d) -> p h d", d=65)
                        c.op("dve", lambda e: e.reciprocal(out=rec[:, :].unsqueeze(2), in_=av[:, :, 64:65]),
                             reads=[t_accs[br]], writes=[t_rec])
                        c.op("dve", lambda e: e.tensor_tensor(out=ytmp[:, :].rearrange("p (h d) -> p h d", d=64), in0=av[:, :, 0:64],
                                                              in1=rec[:, :].unsqueeze(2).to_broadcast([128, 8, 64]), op=ALU.mult),
                             reads=[t_accs[br], t_rec], writes=[t_ytmp])
                        c.op("dve", lambda e: e.tensor_tensor(out=y_out[:, :], in0=ytmp[:, :], in1=gt[:, g0:g0 + 512], op=ALU.mult),
                             reads=[t_ytmp, t_gt], writes=[t_y])
                        yield
                    for (ysrc, t_ys, yT, t_yT) in ((yag, t_yag, yaT, t_yaT), (ybg, t_ybg, ybT, t_ybT)):
                        for kc in range(4):
                            c.op("pe", lambda e, kc=kc, ysrc=ysrc: e.transpose(m1[:, kc * 128:(kc + 1) * 128], ysrc[:, kc * 128:(kc + 1) * 128], ident_bf[:]),
                                 reads=[t_ys, t_ident], writes=[t_m1])
                        c.op("act", lambda e, yT=yT: e.activation(out=yT[:, :, :], in_=m1[:, 0:512].rearrange("p (k t) -> p k t", t=128), func=AF.Copy),
                             reads=[t_m1], writes=[t_yT])
                        yield
                    for cc in range(2):
                        cs = slice(cc * 512, (cc + 1) * 512)
                        for kc in range(4):
                            c.op("pe", lambda e, kc=kc: e.matmul(mo[:, :], lhsT=yaT[:, kc, :], rhs=wbra[:, kc, cs], start=(kc == 0), stop=(kc == 3)),
                                 reads=[t_yaT, t_wbra], writes=[t_mo])
                        c.op("dve", lambda e: e.tensor_tensor(out=ytmp[:, :], in0=mo[:, :], in1=gt[:, 1024 + cc * 512:1024 + (cc + 1) * 512], op=ALU.mult),
                             reads=[t_mo, t_gt], writes=[t_ytmp])
                        yield
                        for kc in range(4):
                            c.op("pe", lambda e, kc=kc: e.matmul(mo[:, :], lhsT=ybT[:, kc, :], rhs=wbrb[:, kc, cs], start=(kc == 0), stop=(kc == 3)),
                                 reads=[t_ybT, t_wbrb], writes=[t_mo])
                        c.op("dve", lambda e: e.tensor_tensor(out=mtmp2[:, :], in0=mo[:, :], in1=gt[:, 2048 + cc * 512:2048 + (cc + 1) * 512], op=ALU.mult),
                             reads=[t_mo, t_gt], writes=[t_mtmp2])
                        c.op("dve", lambda e: e.tensor_tensor(out=mrg[:, cs], in0=ytmp[:, :], in1=mtmp2[:, :], op=ALU.add),
                             reads=[t_ytmp, t_mtmp2], wadd=[t_mrg])
                        yield
                    for kc in range(8):
                        c.op("pe", lambda e, kc=kc: e.transpose(m1[:, kc * 128:(kc + 1) * 128], mrg[:, kc * 128:(kc + 1) * 128], ident_bf[:]),
                             reads=[t_mrg, t_ident], writes=[t_m1])
                    c.op("act", lambda e: e.activation(out=mT[:, :, :], in_=m1[:, :].rearrange("p (k t) -> p k t", t=128), func=AF.Copy),
                         reads=[t_m1], writes=[t_mT])
                    yield
                    for cc in range(2):
                        cs = slice(cc * 512, (cc + 1) * 512)
                        for kc in range(8):
                            c.op("pe", lambda e, kc=kc: e.matmul(mo[:, :], lhsT=mT[:, kc, :], rhs=wo[:, kc, cs], start=(kc == 0), stop=(kc == 7)),
                                 reads=[t_mT, t_wo], writes=[t_mo])
                        c.op("dve", lambda e: e.tensor_tensor(out=xt[:, cs], in0=mo[:, :], in1=xt[:, cs], op=ALU.add),
                             reads=[t_mo], wadd=[t_xt])
                        yield
                    c.dma("pool", o_dst[rows, :], xt[:, :], reads=[t_xt])
                    yield

                ucnt = [0]

                def make_units(i):
                    units = []
                    for br in range(2):
                        for j in range(i + 1):
                            for hf in range(2):
                                units.append((br, j, hf))
                    return units

                def emit_scores(i, u, slot):
                    br, j, hf = u
                    near = j >= i - 1
                    e0 = 128 * (i - j)
                    o = stp[slot][:, :]
                    ts = t_stp[slot]
                    ks = slice(j * 128, (j + 1) * 128)
                    if br == 0:
                        c.op("pe", lambda e: e.matmul(o, lhsT=eall_bf[:, (j // 2) * 128:(j // 2 + 1) * 128],
                                                      rhs=mbT[:, hf * 4:(hf + 1) * 4, :].rearrange("p k t -> p (k t)"),
                                                      start=True, stop=False, skip_group_check=True),
                             reads=[t_eall, t_mbT], writes=[ts])
                        if near:
                            c.op("pe", lambda e: e.matmul(o, lhsT=ident_bf[:, :], rhs=bimg_hi[:, hf * 4:(hf + 1) * 4, e0:e0 + 128],
                                                          start=False, stop=False, skip_group_check=True),
                                 reads=[t_ident, t_bimg], writes=[ts])
                        for hq in range(4):
                            h = hf * 4 + hq
                            c.op("pe", lambda e, h=h, hq=hq: e.matmul(stp[slot][:, hq * 128:(hq + 1) * 128], lhsT=kaT2[:, h // 2, ks],
                                                                      rhs=qpad[:, h // 2, h % 2, :], start=False, stop=(hq == 3),
                                                                      skip_group_check=True),
                                 reads=[t_kaT2, t_qpad], writes=[ts])
                    else:
                        c.op("pe", lambda e: e.matmul(o, lhsT=kbT[:, ks], rhs=qbT[:, hf * 4:(hf + 1) * 4, :].rearrange("p k t -> p (k t)"),
                                                      start=True, stop=False, skip_group_check=True),
                             reads=[t_kbT[j], t_qbT], writes=[ts])
                        if near:
                            c.op("pe", lambda e: e.matmul(o, lhsT=ident_bf[:, :], rhs=bimg_hi[:, 8 + hf * 4:8 + (hf + 1) * 4, e0:e0 + 128],
                                                          start=False, stop=False, skip_group_check=True),
                                 reads=[t_ident, t_bimg], writes=[ts])
                        c.op("pe", lambda e: e.matmul(o, lhsT=mbias[i % 2][:, ks], rhs=irep_bf[:, :], start=False, stop=True,
                                                      skip_group_check=True),
                             reads=[t_mbias[i % 2], t_irep], writes=[ts])

                def emit_exp(slot):
                    c.op("act", lambda e: e.activation(out=pT[slot][:, :], in_=stp[slot][:, :], func=AF.Exp, scale=0.125),
                         reads=[t_stp[slot]], writes=[t_pT[slot]])

                def emit_pv(i, u, slot):
                    br, j, hf = u
                    for hq in range(4):
                        h = hf * 4 + hq
                        rhs = v1a[:, j, h, :] if br == 0 else v1b[:, j, :]
                        tv = t_v1a[j] if br == 0 else t_v1b[j]
                        c.op("pe", lambda e, hq=hq, rhs=rhs: e.matmul(acc[hf][:, hq * 65:hq * 65 + 65], lhsT=pT[slot][:, hq * 128:(hq + 1) * 128],
                                                                      rhs=rhs, start=(j == 0 and hq == 0), stop=(j == i and hq == 3),
                                                                      skip_group_check=True),
                             reads=[t_pT[slot], tv, t_ones], writes=[t_acc[hf]])
                    if j == i:
                        c.op("act", lambda e: e.activation(out=accs[br][:, hf * 260:(hf + 1) * 260], in_=acc[hf][:, 0:260], func=AF.Copy),
                             reads=[t_acc[hf]], wadd=[t_accs[br]])

                def interleave(gens):
                    gens = list(gens)
                    while gens:
                        for g in list(gens):
                            try:
                                next(g)
                                yield
                            except StopIteration:
                                gens.remove(g)

                def bg_schedule(gA, gC, nidx):
                    if gC is not None:
                        for _ in range(2):
                            next(gC)
                            yield
                    if gA is not None:
                        for _ in range(nidx):
                            try:
                                next(gA)
                                yield
                            except StopIteration:
                                break
                    yield from interleave([g for g in (gA, gC) if g is not None])

                def count_A(i):
                    nk = 128 * (i + 1)
                    return 3 + 4 * ((nk + 511) // 512) + (NBIS if i >= 2 else 0)

                for _ in gen_A(0):
                    pass
                prepB(0)
                for i in range(NT):
                    nbg = 0
                    gC = gA = None
                    if i - 1 >= 0:
                        gC = gen_C(i - 1)
                        nbg += 12
                    if i + 1 < NT:
                        gA = gen_A(i + 1)
                        nbg += count_A(i + 1)
                    bg = bg_schedule(gA, gC, 1 + 4 * ((128 * (i + 2) + 511) // 512))
                    units = make_units(i)
                    nU = len(units)
                    quota = -(-nbg // nU) if nU else nbg
                    slots = [(ucnt[0] + k) % 3 for k in range(nU)]
                    ucnt[0] += nU
                    emit_scores(i, units[0], slots[0])
                    for k in range(nU):
                        if k + 1 < nU:
                            emit_scores(i, units[k + 1], slots[k + 1])
                            if k + 2 == nU and i + 1 < NT:
                                prepB(i + 1)
                        emit_exp(slots[k])
                        emit_pv(i, units[k], slots[k])
                        for _ in range(quota):
                            try:
                                next(bg)
                            except StopIteration:
                                break
                    for _ in bg:
                        pass
                for _ in gen_C(NT - 1):
                    pass
                c.barrier()
        c.finish()
    return nc


_CACHE = {}


def kernel(x, norm_g, w_in, q_norm_a, k_norm_a, q_norm_b, k_norm_b, w_branch_a, w_branch_b, w_out, rel_bias):
    if "nc" not in _CACHE:
        _CACHE["nc"] = build_program()
    nc = _CACHE["nc"]
    f = lambda a: np.ascontiguousarray(np.asarray(a, dtype=np.float32))
    shared = {"norm_g": f(norm_g), "w_in": f(w_in), "q_norm_a": f(q_norm_a), "k_norm_a": f(k_norm_a),
              "q_norm_b": f(q_norm_b), "k_norm_b": f(k_norm_b), "w_branch_a": f(w_branch_a),
              "w_branch_b": f(w_branch_b), "w_out": f(w_out), "rel_bias": f(rel_bias)}
    shared.update(_host_consts())
    xs = f(x)
    in_maps = []
    for b in range(8):
        m = dict(shared)
        m["x"] = xs[b]
        in_maps.append(m)
    res = run_bass_kernel_spmd(nc, in_maps, core_ids=list(range(8)))
    return np.stack([np.asarray(r["out"]) for r in res.results], axis=0).astype(np.float32)
```

```python
import math
from contextlib import ExitStack

import numpy as np
import concourse.bass as bass
import concourse.mybir as mybir
from concourse.bass_utils import run_bass_kernel_spmd

F32 = mybir.dt.float32
BF16 = mybir.dt.bfloat16
ALU = mybir.AluOpType
AF = mybir.ActivationFunctionType
AX = mybir.AxisListType

S = 4096
D = 1024
NT = S // 128
DIN = 5572
DEPTH = 2
EPS = 1e-6
NEG = -30000.0
QS_W = 4352
NBIS = 22
W0 = 1024.0
TOPK = 256.0
MID0 = 2.0 ** -13
ACT_SHARE_NUM, ACT_SHARE_DEN = 0, 8

C_QA, C_KA, C_VA, C_GA = 0, 512, 1024, 1536
C_QB, C_KB, C_VB, C_GB = 2048, 2560, 2624, 2688
C_QI, C_KI, C_WI, C_MA, C_MB = 3200, 3456, 3520, 3524, 4548


class T:
    __slots__ = ("w", "r")

    def __init__(self):
        self.w = {}
        self.r = {}


class Ctx:
    RING = 8

    def __init__(self, nc, stack):
        self.nc = nc
        self.eng = {"pe": nc.tensor, "act": nc.scalar, "dve": nc.vector, "pool": nc.gpsimd, "sp": nc.sync}
        self.sem = {}
        self.cnt = {}
        for k in ("pe", "act", "dve", "pool"):
            self.sem[k] = stack.enter_context(nc.semaphore("s_" + k))
            self.cnt[k] = 0
        self.waited = {k: {} for k in self.eng}
        self.rings = {}
        self.ringpos = {}
        for q in ("sp", "pool"):
            self.rings[q] = []
            for i in range(self.RING):
                key = "d_%s_%d" % (q, i)
                self.sem[key] = stack.enter_context(nc.semaphore(key))
                self.cnt[key] = 0
                self.rings[q].append(key)
            self.ringpos[q] = 0

    def _wait(self, e, marks):
        need = {}
        for (k, v) in marks:
            if need.get(k, 0) < v:
                need[k] = v
        wd = self.waited[e]
        for k, v in need.items():
            if wd.get(k, 0) < v:
                self.eng[e].wait_ge(self.sem[k], v)
                wd[k] = v

    def _deps(self, e, reads, writes):
        marks = []
        for t in reads:
            for m in t.w.items():
                if m[0] == e and e == "pe":
                    continue
                marks.append(m)
        for t in writes:
            for m in t.w.items():
                if m[0] == e:
                    continue
                marks.append(m)
            for m in t.r.items():
                if m[0] == e:
                    continue
                marks.append(m)
        return marks

    def op(self, e, fn, reads=(), writes=(), wadd=()):
        self._wait(e, self._deps(e, reads, tuple(writes) + tuple(wadd)))
        ins = fn(self.eng[e])
        self.cnt[e] += 1
        ins.then_inc(self.sem[e], 1)
        v = self.cnt[e]
        for t in reads:
            t.r[e] = v
        for t in writes:
            t.w = {e: v}
            t.r = {}
        for t in wadd:
            t.w[e] = v
        return ins

    def dma(self, q, out, in_, reads=(), writes=(), wadd=(), **kw):
        key = self.rings[q][self.ringpos[q] % self.RING]
        self.ringpos[q] += 1
        marks = self._deps("dma", reads, tuple(writes) + tuple(wadd))
        if self.cnt[key] > 0:
            marks.append((key, self.cnt[key]))
        self._wait(q, marks)
        ins = self.eng[q].dma_start(out=out, in_=in_, **kw)
        self.cnt[key] += 16
        ins.then_inc(self.sem[key], 16)
        v = self.cnt[key]
        for t in reads:
            t.r[key] = v
        for t in writes:
            t.w = {key: v}
            t.r = {}
        for t in wadd:
            t.w[key] = v

    def all_marks(self):
        marks = []
        for k, v in self.cnt.items():
            if v > 0:
                marks.append((k, v))
        return marks

    def barrier(self):
        marks = self.all_marks()
        for e in ("pe", "act", "dve", "pool", "sp"):
            self._wait(e, [m for m in marks if m[0] != e])

    def finish(self):
        self._wait("sp", self.all_marks())


def _bucket_table():
    n = np.arange(0, 256)
    max_exact = 16
    nf = np.maximum(n, 1).astype(np.float32)
    large = max_exact + (np.log(nf / np.float32(max_exact)) / np.float32(math.log(128 / max_exact))
                         * np.float32(32 - max_exact)).astype(np.int32)
    large = np.minimum(large, 31)
    return np.where(n < max_exact, n, large)


def _host_consts():
    bk = _bucket_table()
    grev = np.zeros((33, 384), np.float32)
    for m in range(384):
        dist = 255 - m
        if dist >= 0:
            grev[bk[dist], m] = 1.0
        else:
            grev[32, m] = 1.0
    ident = np.eye(128, dtype=np.float32)
    irep = np.concatenate([ident] * 4, axis=1)
    t = np.arange(128)[:, None]
    s = np.arange(128)[None, :]
    causalneg = np.where(s <= t, 0.0, -1e30).astype(np.float32)
    eall = np.zeros((16, 16, 128), np.float32)
    for n in range(16):
        eall[n, n, :] = 1.0
    negrow = np.full((1, 16), NEG * 8.0, np.float32)
    return {"c_grev": grev, "c_ident": ident, "c_irep": irep, "c_causal": causalneg,
            "c_eall": eall.reshape(16, 2048), "c_negrow": negrow}


def build_program():
    nc = bass.Bass("TRN2", target_bir_lowering=False)
    din = lambda n, s, d=F32: nc.dram_tensor(n, s, d, kind="ExternalInput").ap()
    x = din("x", [S, D])
    norm_g = din("norm_g", [DEPTH, D])
    w_in = din("w_in", [DEPTH, D, DIN])
    qn_a = din("q_norm_a", [DEPTH, 64])
    kn_a = din("k_norm_a", [DEPTH, 64])
    qn_b = din("q_norm_b", [DEPTH, 64])
    kn_b = din("k_norm_b", [DEPTH, 64])
    w_bra = din("w_branch_a", [DEPTH, 512, D])
    w_brb = din("w_branch_b", [DEPTH, 512, D])
    w_out = din("w_out", [DEPTH, D, D])
    rel_bias = din("rel_bias", [32, 16])
    c_grev = din("c_grev", [33, 384])
    c_ident = din("c_ident", [128, 128])
    c_irep = din("c_irep", [128, 512])
    c_causal = din("c_causal", [128, 128])
    c_eall = din("c_eall", [16, 2048])
    c_negrow = din("c_negrow", [1, 16])
    out = nc.dram_tensor("out", [S, D], F32, kind="ExternalOutput").ap()
    scr = lambda n, s, d: nc.dram_tensor(n, s, d, kind="Internal").ap()
    qs = scr("qs", [S, QS_W], BF16)
    wis = scr("wis", [S, 4], F32)
    kaT_s = scr("kaT_s", [4, 128, S], BF16)
    x1 = scr("x1", [S, D], F32)

    top = ExitStack()
    with top:
        c = Ctx(nc, top)

        uid = [0]

        def sbt(st, name, shape, dt):
            uid[0] += 1
            return st.enter_context(nc.sbuf_tensor("%s_%d" % (name, uid[0]), shape, dt))

        def pst(st, name, shape, dt):
            uid[0] += 1
            return st.enter_context(nc.psum_tensor("%s_%d" % (name, uid[0]), shape, dt))

        ident_bf = sbt(top, "ident_bf", [128, 128], BF16); t_ident = T()
        irep_bf = sbt(top, "irep_bf", [128, 512], BF16); t_irep = T()
        causal = sbt(top, "causal", [128, 128], F32); t_causal = T()
        eall_bf = sbt(top, "eall_bf", [128, 2048], BF16); t_eall = T()
        bimg_hi = sbt(top, "bimg_hi", [128, 16, 256], BF16)
        t_bimg = T()
        v1a = sbt(top, "v1a", [128, NT, 8, 65], BF16); t_v1a = [T() for _ in range(NT)]
        v1b = sbt(top, "v1b", [128, NT, 65], BF16); t_v1b = [T() for _ in range(NT)]
        kbT = sbt(top, "kbT", [128, S], BF16); t_kbT = [T() for _ in range(NT)]
        kiT = sbt(top, "kiT", [128, S], BF16); t_kiT = [T() for _ in range(NT)]
        kmacc = sbt(top, "kmacc", [128, 4, 16], F32); t_kmacc = T()

        c.dma("pool", ident_bf[:], c_ident, writes=[t_ident])
        c.dma("pool", irep_bf[:], c_irep, writes=[t_irep])
        c.op("dve", lambda e: e.memset(eall_bf[:, :], 0.0), writes=[t_eall])
        c.dma("pool", eall_bf[0:16, :], c_eall, writes=[t_eall])
        t_kz = T()
        c.op("dve", lambda e: e.memset(kbT[64:128, :], 0.0), writes=[t_kz])
        c.op("dve", lambda e: e.memset(kiT[64:128, :], 0.0), writes=[t_kz])
        c.dma("sp", causal[:], c_causal, writes=[t_causal])
        t_ones = T()
        c.op("dve", lambda e: e.memset(v1a[:, :, :, 64:65], 1.0), writes=[t_ones])
        c.op("dve", lambda e: e.memset(v1b[:, :, 64:65], 1.0), writes=[t_ones])

        with ExitStack() as st:
            grev = sbt(st, "grev", [33, 384], F32); t_grev = T()
            reltab = sbt(st, "reltab", [33, 16], F32); t_rel = T()
            r31 = sbt(st, "r31", [32, 16], F32); t_r31 = T()
            bps = pst(st, "bps", [128, 2048], F32); t_bps = T()
            c.dma("sp", grev[:], c_grev, writes=[t_grev])
            c.dma("sp", reltab[0:32, :], rel_bias, writes=[t_rel])
            c.dma("sp", r31[:], rel_bias[31:32, :].to_broadcast([32, 16]), writes=[t_r31])
            c.op("dve", lambda e: e.tensor_tensor(out=reltab[0:32, :], in0=reltab[0:32, :], in1=r31[:], op=ALU.subtract),
                 reads=[t_r31, t_rel], writes=[t_rel])
            c.op("dve", lambda e: e.tensor_scalar(out=reltab[0:32, :], in0=reltab[0:32, :], scalar1=8.0, scalar2=None, op0=ALU.mult),
                 reads=[t_rel], writes=[t_rel])
            c.dma("sp", reltab[32:33, :], c_negrow, reads=[t_rel], writes=[t_rel])
            for half in range(2):
                for el in range(128):
                    e_ = half * 128 + el
                    c.op("pe", lambda e, el=el, e_=e_: e.matmul(bps[:, el * 16:(el + 1) * 16], lhsT=grev[0:33, 255 - e_:255 - e_ + 128],
                                                                 rhs=reltab[0:33, :], start=True, stop=True),
                         reads=[t_grev, t_rel], writes=[t_bps])
                src = bps[:, :].rearrange("p (e h) -> p h e", h=16)
                hi_v = bimg_hi[:, :, half * 128:(half + 1) * 128]
                c.op("dve", lambda e: e.tensor_copy(out=hi_v, in_=src), reads=[t_bps], wadd=[t_bimg])
            c.barrier()

        for l in range(DEPTH):
            x_src = x if l == 0 else x1
            o_dst = x1 if l == 0 else out
            with ExitStack() as st:
                w_bf = sbt(st, "w_bf", [128, 8, DIN], BF16); t_w = [T() for _ in range(8)]
                gbc = sbt(st, "gbc", [128, D], F32); t_gbc = T()
                gq_a = sbt(st, "gq_a", [128, 64], F32)
                gk_a = sbt(st, "gk_a", [128, 64], F32)
                gq_b = sbt(st, "gq_b", [128, 64], F32)
                gk_b = sbt(st, "gk_b", [128, 64], F32)
                t_gs = T()
                xb = [sbt(st, "xb%d" % i, [128, D], F32) for i in range(2)]; t_xb = [T(), T()]
                sqj = sbt(st, "sqj", [128, D], BF16); t_sqj = T()
                stat = sbt(st, "stat", [128, 8], F32); t_stat = T()
                hb = sbt(st, "hb", [128, D], BF16); t_hb = T()
                hT = [sbt(st, "hT%d" % i, [128, 8, 128], BF16) for i in range(2)]; t_hT = [T(), T()]
                qt = [sbt(st, "qt%d" % i, [128, QS_W], BF16) for i in range(2)]; t_qt = [T(), T()]
                sqf = sbt(st, "sqf", [128, 512], F32); t_sqf = T()
                hst = sbt(st, "hst", [128, 32], F32); t_hst = T()
                tmpf = sbt(st, "tmpf", [128, 512], F32); t_tmpf = T()
                kan = sbt(st, "kan", [128, 512], BF16); t_kan = T()
                kTt = [sbt(st, "kTt%d" % i, [128, 4, 128], BF16) for i in range(2)]; t_kTt = [T(), T()]
                kred = sbt(st, "kred", [128, 4], F32); t_kred = T()
                sm = sbt(st, "sm", [128, 128], BF16); t_sm = T()
                wit = [sbt(st, "witp%d" % i, [128, 4], F32) for i in range(2)]; t_wit = [T(), T()]
                tp = pst(st, "tp", [128, 1024], BF16); t_tp = T()
                tp2 = pst(st, "tp2", [128, 1024], BF16); t_tp2 = T()
                pj = [pst(st, "pj%d" % i, [128, 512], F32) for i in range(6)]; t_pj = [T() for _ in range(6)]

                for kc in range(8):
                    for (c0, c1) in ((0, 2048), (2048, 4096), (4096, DIN)):
                        c.dma("pool", w_bf[:, kc, c0:c1], w_in[l, kc * 128:(kc + 1) * 128, c0:c1], wadd=[t_w[kc]])
                c.dma("sp", gbc[:], norm_g[l:l + 1, :].to_broadcast([128, D]), writes=[t_gbc])
                for (g_sb, g_dr) in ((gq_a, qn_a), (gk_a, kn_a), (gq_b, qn_b), (gk_b, kn_b)):
                    c.dma("sp", g_sb[:], g_dr[l:l + 1, :].to_broadcast([128, 64]), wadd=[t_gs])
                c.op("dve", lambda e: e.memset(kmacc[:], 0.0), writes=[t_kmacc])

                pjn = [0]

                def project(i, col0, ncols):
                    b = pjn[0] % 6
                    pjn[0] += 1
                    for kc in range(8):
                        c.op("pe", lambda e, kc=kc: e.matmul(pj[b][:, 0:ncols], lhsT=hT[i % 2][:, kc, :],
                                                              rhs=w_bf[:, kc, col0:col0 + ncols],
                                                              start=(kc == 0), stop=(kc == 7)),
                             reads=[t_hT[i % 2], t_w[kc]], writes=[t_pj[b]])
                    return pj[b], t_pj[b]

                def rsqrt_small(ap_in, ap_out, n, inv_n):
                    c.op("dve", lambda e: e.tensor_scalar(out=ap_out, in0=ap_in, scalar1=inv_n, scalar2=EPS,
                                                          op0=ALU.mult, op1=ALU.add), reads=[t_hst], writes=[t_hst])
                    c.op("act", lambda e: e.activation(out=ap_out, in_=ap_out, func=AF.Ln), reads=[t_hst], writes=[t_hst])
                    c.op("act", lambda e: e.activation(out=ap_out, in_=ap_out, func=AF.Exp, scale=-0.5),
                         reads=[t_hst], writes=[t_hst])

                def headnorm(ps_ap, t_ps, H, g_sb, out_ap, t_out, wadd_out=False):
                    W = H * 64
                    c.op("act", lambda e: e.activation(out=sqf[:, 0:W], in_=ps_ap, func=AF.Square),
                         reads=[t_ps], writes=[t_sqf])
                    c.op("dve", lambda e: e.tensor_reduce(out=hst[:, 0:H], in_=sqf[:, 0:W].rearrange("p (h d) -> p h d", d=64),
                                                          axis=AX.X, op=ALU.add), reads=[t_sqf], writes=[t_hst])
                    rsqrt_small(hst[:, 0:H], hst[:, 0:H], H, 1.0 / 64.0)
                    c.op("dve", lambda e: e.tensor_tensor(out=tmpf[:, 0:W].rearrange("p (h d) -> p h d", d=64),
                                                          in0=ps_ap.rearrange("p (h d) -> p h d", d=64),
                                                          in1=hst[:, 0:H].unsqueeze(2).to_broadcast([128, H, 64]), op=ALU.mult),
                         reads=[t_ps, t_hst], writes=[t_tmpf])
                    kw = dict(wadd=[t_out]) if wadd_out else dict(writes=[t_out])
                    c.op("dve", lambda e: e.tensor_tensor(out=out_ap.rearrange("p (h d) -> p h d", d=64),
                                                          in0=tmpf[:, 0:W].rearrange("p (h d) -> p h d", d=64),
                                                          in1=g_sb[:, :].unsqueeze(1).to_broadcast([128, H, 64]), op=ALU.mult),
                         reads=[t_tmpf, t_gs], **kw)

                def norm_act(i):
                    rows = slice(i * 128, (i + 1) * 128)
                    xt, t_xt = xb[i % 2], t_xb[i % 2]
                    c.dma("sp", xt[:], x_src[rows, :], writes=[t_xt])
                    c.op("act", lambda e: e.activation(out=sqj[:], in_=xt[:], func=AF.Square, accum_out=stat[:, 0:1]),
                         reads=[t_xt], writes=[t_sqj, t_stat])
                    c.op("dve", lambda e: e.tensor_scalar(out=stat[:, 1:2], in0=stat[:, 0:1], scalar1=1.0 / D, scalar2=EPS,
                                                          op0=ALU.mult, op1=ALU.add), reads=[t_stat], writes=[t_stat])
                    c.op("act", lambda e: e.activation(out=stat[:, 2:3], in_=stat[:, 1:2], func=AF.Ln), reads=[t_stat], writes=[t_stat])
                    c.op("act", lambda e: e.activation(out=stat[:, 3:4], in_=stat[:, 2:3], func=AF.Exp, scale=-0.5),
                         reads=[t_stat], writes=[t_stat])
                    c.op("dve", lambda e: e.scalar_tensor_tensor(out=hb[:], in0=xt[:], scalar=stat[:, 3:4], in1=gbc[:],
                                                                 op0=ALU.mult, op1=ALU.mult),
                         reads=[t_xt, t_stat, t_gbc], writes=[t_hb])

                def norm_pe(i):
                    for kc in range(8):
                        c.op("pe", lambda e, kc=kc: e.transpose(tp[:, kc * 128:(kc + 1) * 128], hb[:, kc * 128:(kc + 1) * 128], ident_bf[:]),
                             reads=[t_hb, t_ident], writes=[t_tp])
                    c.op("act", lambda e: e.activation(out=hT[i % 2][:, :, :], in_=tp[:, :].rearrange("p (k t) -> p k t", t=128), func=AF.Copy),
                         reads=[t_tp], writes=[t_hT[i % 2]])

                norm_act(0)
                norm_pe(0)
                for i in range(NT):
                    rows = slice(i * 128, (i + 1) * 128)
                    q, t_q = qt[i % 2], t_qt[i % 2]
                    kt, t_kt = kTt[i % 2], t_kTt[i % 2]
                    if i + 1 < NT:
                        norm_act(i + 1)
                    ps, tps = project(i, C_QA, 512)
                    headnorm(ps[:, 0:512], tps, 8, gq_a, q[:, 0:512], t_q)
                    ps, tps = project(i, C_KA, 512)
                    headnorm(ps[:, 0:512], tps, 8, gk_a, kan[:, :], t_kan)
                    ps, tps = project(i, C_QB, 512)
                    headnorm(ps[:, 0:512], tps, 8, gq_b, q[:, 512:1024], t_q, wadd_out=True)
                    ps, tps = project(i, C_KB, 128)
                    headnorm(ps[:, 0:64], tps, 1, gk_b, sm[:, 0:64], t_sm)
                    c.op("dve", lambda e: e.tensor_copy(out=v1b[:, i, 0:64], in_=ps[:, 64:128]), reads=[tps], writes=[t_v1b[i]])
                    if i + 1 < NT:
                        norm_pe(i + 1)
                    ps2, tps2 = project(i, C_QI, 324)
                    c.op("dve", lambda e: e.tensor_copy(out=q[:, 4096:4352], in_=ps2[:, 0:256]), reads=[tps2], wadd=[t_q])
                    c.op("dve", lambda e: e.tensor_copy(out=sm[:, 64:128], in_=ps2[:, 256:320]), reads=[tps2], wadd=[t_sm])
                    c.op("dve", lambda e: e.tensor_copy(out=wit[i % 2][:, :], in_=ps2[:, 320:324]), reads=[tps2], writes=[t_wit[i % 2]])
                    c.dma("pool", wis[rows, :], wit[i % 2][:, :], reads=[t_wit[i % 2]])
                    for p4 in range(4):
                        c.op("pe", lambda e, p4=p4: e.transpose(tp2[:, p4 * 128:(p4 + 1) * 128], kan[:, p4 * 128:(p4 + 1) * 128], ident_bf[:]),
                             reads=[t_kan, t_ident], writes=[t_tp2])
                    c.op("act", lambda e: e.activation(out=kt[:, :, :], in_=tp2[:, 0:512].rearrange("p (k t) -> p k t", t=128), func=AF.Copy),
                         reads=[t_tp2], writes=[t_kt])
                    for p4 in range(4):
                        c.dma("pool", kaT_s[p4, :, i * 128:(i + 1) * 128], kt[:, p4, :], reads=[t_kt])
                    c.op("dve", lambda e: e.tensor_reduce(out=kred[:, :], in_=kt[:, :, :], axis=AX.X, op=ALU.add),
                         reads=[t_kt], writes=[t_kred])
                    nblk = i // 2
                    c.op("dve", lambda e: e.tensor_tensor(out=kmacc[:, :, nblk], in0=kmacc[:, :, nblk], in1=kred[:, :], op=ALU.add),
                         reads=[t_kred, t_kmacc], writes=[t_kmacc])
                    ps, tps = project(i, C_VA, 512)
                    c.op("act", lambda e: e.activation(out=v1a[:, i, :, 0:64], in_=ps[:, 0:512].rearrange("p (h d) -> p h d", d=64), func=AF.Copy),
                         reads=[tps], writes=[t_v1a[i]])
                    c.op("pe", lambda e: e.transpose(tp2[0:64, 512:640], sm[:, 0:64], ident_bf[:]), reads=[t_sm, t_ident], writes=[t_tp2])
                    c.op("pe", lambda e: e.transpose(tp2[0:64, 640:768], sm[:, 64:128], ident_bf[:]), reads=[t_sm, t_ident], writes=[t_tp2])
                    c.op("act", lambda e: e.activation(out=kbT[0:64, i * 128:(i + 1) * 128], in_=tp2[0:64, 512:640], func=AF.Copy),
                         reads=[t_tp2], writes=[t_kbT[i]])
                    c.op("act", lambda e: e.activation(out=kiT[0:64, i * 128:(i + 1) * 128], in_=tp2[0:64, 640:768], func=AF.Copy),
                         reads=[t_tp2], writes=[t_kiT[i]])
                    ps, tps = project(i, C_GA, 512)
                    c.op("act", lambda e: e.activation(out=q[:, 1024:1536], in_=ps[:, 0:512], func=AF.Silu), reads=[tps], wadd=[t_q])
                    ps, tps = project(i, C_GB, 512)
                    c.op("act", lambda e: e.activation(out=q[:, 1536:2048], in_=ps[:, 0:512], func=AF.Silu), reads=[tps], wadd=[t_q])
                    for gi, cm in enumerate((C_MA, C_MA + 512, C_MB, C_MB + 512)):
                        ps, tps = project(i, cm, 512)
                        c.op("act", lambda e, gi=gi: e.activation(out=q[:, 2048 + gi * 512:2048 + (gi + 1) * 512], in_=ps[:, 0:512], func=AF.Sigmoid),
                             reads=[tps], wadd=[t_q])
                    c.dma("pool", qs[rows, :], q[:, :], reads=[t_q])
                c.barrier()

            with ExitStack() as st:
                kaT2 = sbt(st, "kaT2", [128, 4, S], BF16); t_kaT2 = T()
                km_bf = sbt(st, "km_bf", [128, 4, 16], BF16); t_km = T()
                wbra = sbt(st, "wbra", [128, 4, D], BF16); t_wbra = T()
                wbrb = sbt(st, "wbrb", [128, 4, D], BF16); t_wbrb = T()
                wo = sbt(st, "wo", [128, 8, D], BF16); t_wo = T()
                qm = sbt(st, "qm", [128, 1024], BF16); t_qm = T()
                qmi = sbt(st, "qmi", [128, 256], BF16); t_qmi = T()
                gt = sbt(st, "gt", [128, 3072], BF16); t_gt = T()
                wit2 = [sbt(st, "wit2%d" % k, [128, 4], F32) for k in range(2)]; t_wit2 = [T(), T()]
                xt = sbt(st, "xt", [128, D], F32); t_xt = T()
                qpad = sbt(st, "qpad", [128, 4, 2, 128], BF16); t_qpad = T()
                qbT = sbt(st, "qbT", [128, 8, 128], BF16); t_qbT = T()
                qiT = [sbt(st, "qiT%d" % k, [128, 4, 128], BF16) for k in range(2)]; t_qiT = [T(), T()]
                score = sbt(st, "score", [128, S], F32); t_score = T()
                mbias = [sbt(st, "mbias%d" % k, [128, S], BF16) for k in range(2)]; t_mbias = [T(), T()]
                rl = sbt(st, "rl", [128, 512], F32); t_rl = T()
                bs = sbt(st, "bs", [128, 8], F32)
                t_cnt = T(); t_tmp = T(); t_mid = T(); t_thr = T()
                sg = sbt(st, "sg", [128, 2], F32); t_sg = T()
                gatem = sbt(st, "gatem", [128, 8, 16], F32); t_gatem = T()
                top8 = sbt(st, "top8", [128, 8, 8], F32); t_top8 = T()
                thr3 = sbt(st, "thr3", [128, 8], F32); t_thr3 = T()
                mbf = sbt(st, "mbf", [128, 8, 16], F32); t_mbf = T()
                mb16 = sbt(st, "mb16", [128, 8, 16], BF16); t_mb16 = T()
                mbT = sbt(st, "mbT", [128, 8, 128], BF16); t_mbT = T()
                pT = [sbt(st, "pT%d" % k, [128, 512], BF16) for k in range(3)]; t_pT = [T(), T(), T()]
                accs = [sbt(st, "accs%d" % k, [128, 520], F32) for k in range(2)]; t_accs = [T(), T()]
                rec = sbt(st, "rec", [128, 8], F32); t_rec = T()
                ytmp = sbt(st, "ytmp", [128, 512], F32); t_ytmp = T()
                yag = sbt(st, "yag", [128, 512], BF16); t_yag = T()
                ybg = sbt(st, "ybg", [128, 512], BF16); t_ybg = T()
                yaT = sbt(st, "yaT", [128, 4, 128], BF16); t_yaT = T()
                ybT = sbt(st, "ybT", [128, 4, 128], BF16); t_ybT = T()
                mtmp2 = sbt(st, "mtmp2", [128, 512], F32); t_mtmp2 = T()
                mrg = sbt(st, "mrg", [128, D], BF16); t_mrg = T()
                mT = sbt(st, "mT", [128, 8, 128], BF16); t_mT = T()
                stp = [pst(st, "stp%d" % k, [128, 512], F32) for k in range(3)]; t_stp = [T(), T(), T()]
                acc = [pst(st, "acc%d" % k, [128, 512], F32) for k in range(2)]; t_acc = [T(), T()]
                m0 = pst(st, "m0", [128, 512], F32); t_m0 = T()
                m1 = pst(st, "m1", [128, 1024], BF16); t_m1 = T()
                mo = pst(st, "mo", [128, 512], F32); t_mo = T()

                for p4 in range(4):
                    c.dma("sp", kaT2[:, p4, :], kaT_s[p4, :, :], wadd=[t_kaT2])
                for kc in range(4):
                    c.dma("pool", wbra[:, kc, :], w_bra[l, kc * 128:(kc + 1) * 128, :], wadd=[t_wbra])
                    c.dma("pool", wbrb[:, kc, :], w_brb[l, kc * 128:(kc + 1) * 128, :], wadd=[t_wbrb])
                for kc in range(8):
                    c.dma("pool", wo[:, kc, :], w_out[l, kc * 128:(kc + 1) * 128, :], wadd=[t_wo])
                c.op("dve", lambda e: e.tensor_scalar(out=km_bf[:, :, :], in0=kmacc[:, :, :], scalar1=1.0 / 256.0, scalar2=None, op0=ALU.mult),
                     reads=[t_kmacc], writes=[t_km])
                c.op("dve", lambda e: e.memset(qpad[:, :, :, :], 0.0), writes=[t_qpad])
                c.op("dve", lambda e: e.memset(qbT[:, :, :], 0.0), writes=[t_qbT])
                c.op("dve", lambda e: e.memset(mbT[:, :, :], 0.0), writes=[t_mbT])
                for k in range(2):
                    c.op("dve", lambda e, k=k: e.memset(qiT[k][:, :, :], 0.0), writes=[t_qiT[k]])

                def rows_of(i):
                    return slice(i * 128, (i + 1) * 128)

                def gen_A(i):
                    rows = rows_of(i)
                    nk = 128 * (i + 1)
                    b = i % 2
                    c.dma("sp", qmi[:, :], qs[rows, 4096:4352], writes=[t_qmi])
                    c.dma("sp", wit2[b][:, :], wis[rows, :], writes=[t_wit2[b]])
                    for h in range(4):
                        c.op("pe", lambda e, h=h: e.transpose(m1[0:64, h * 128:(h + 1) * 128], qmi[:, h * 64:(h + 1) * 64], ident_bf[:]),
                             reads=[t_qmi, t_ident], writes=[t_m1])
                    c.op("act", lambda e: e.activation(out=qiT[b][0:64, :, :], in_=m1[0:64, 0:512].rearrange("p (k t) -> p k t", t=128), func=AF.Copy),
                         reads=[t_m1], wadd=[t_qiT[b]])
                    yield
                    for c0 in range(0, nk, 512):
                        n = min(512, nk - c0)
                        kdeps = [t_kiT[jj] for jj in range(c0 // 128, (c0 + n) // 128)]
                        for hh in range(4):
                            c.op("pe", lambda e, hh=hh: e.matmul(m0[:, 0:n], lhsT=qiT[b][:, hh, :], rhs=kiT[:, c0:c0 + n], start=True, stop=True),
                                 reads=[t_qiT[b]] + kdeps, writes=[t_m0])
                            c.op("act", lambda e: e.activation(out=rl[:, 0:n], in_=m0[:, 0:n], func=AF.Relu),
                                 reads=[t_m0], writes=[t_rl])
                            if hh == 0:
                                c.op("dve", lambda e: e.tensor_scalar(out=score[:, c0:c0 + n], in0=rl[:, 0:n], scalar1=wit2[b][:, 0:1],
                                                                      scalar2=None, op0=ALU.mult),
                                     reads=[t_rl, t_wit2[b]], writes=[t_score])
                            else:
                                c.op("dve", lambda e, hh=hh: e.scalar_tensor_tensor(out=score[:, c0:c0 + n], in0=rl[:, 0:n],
                                                                                   scalar=wit2[b][:, hh:hh + 1], in1=score[:, c0:c0 + n],
                                                                                   op0=ALU.mult, op1=ALU.add),
                                     reads=[t_rl, t_wit2[b], t_score], writes=[t_score])
                            yield
                    c.op("dve", lambda e: e.tensor_tensor(out=score[:, i * 128:(i + 1) * 128], in0=score[:, i * 128:(i + 1) * 128],
                                                          in1=causal[:, :], op=ALU.add),
                         reads=[t_score, t_causal], writes=[t_score])
                    if i >= 2:
                        n_act = 128 * (((i + 1) * ACT_SHARE_NUM) // ACT_SHARE_DEN)
                        n1 = nk - n_act
                        c.op("dve", lambda e: e.memset(bs[:, 2:3], MID0), writes=[t_mid])
                        w = W0
                        for k in range(NBIS):
                            c.op("dve", lambda e: e.tensor_scalar(out=mbias[b][:, 0:n1], in0=score[:, 0:n1], scalar1=bs[:, 2:3], scalar2=None,
                                                                  op0=ALU.is_ge, op1=ALU.add, accum_out=bs[:, 0:1]),
                                 reads=[t_score, t_mid], writes=[t_cnt], wadd=[t_mbias[b]])
                            if n_act > 0:
                                c.op("act", lambda e: e.activation(out=mbias[b][:, n1:nk], in_=score[:, n1:nk], func=AF.Sign,
                                                                   bias=bs[:, 2:3], scale=-1.0, accum_out=sg[:, 0:1]),
                                     reads=[t_score, t_mid], writes=[t_sg], wadd=[t_mbias[b]])
                                c.op("dve", lambda e: e.scalar_tensor_tensor(out=bs[:, 4:5], in0=bs[:, 0:1], scalar=2.0, in1=sg[:, 0:1],
                                                                             op0=ALU.mult, op1=ALU.subtract),
                                     reads=[t_cnt, t_sg], writes=[t_tmp])
                                c.op("dve", lambda e: e.tensor_scalar(out=bs[:, 1:2], in0=bs[:, 4:5], scalar1=2.0 * TOPK - n_act, scalar2=0.5,
                                                                      op0=ALU.is_ge, op1=ALU.subtract), reads=[t_tmp], writes=[t_tmp])
                            else:
                                c.op("dve", lambda e: e.tensor_scalar(out=bs[:, 1:2], in0=bs[:, 0:1], scalar1=TOPK, scalar2=0.5,
                                                                      op0=ALU.is_ge, op1=ALU.subtract), reads=[t_cnt], writes=[t_tmp])
                            c.op("dve", lambda e, w=w: e.scalar_tensor_tensor(out=bs[:, 2:3], in0=bs[:, 1:2], scalar=w, in1=bs[:, 2:3],
                                                                              op0=ALU.mult, op1=ALU.add), reads=[t_tmp, t_mid], writes=[t_mid])
                            w = w / 2.0
                            yield
                        c.op("dve", lambda e, w=w: e.tensor_scalar(out=bs[:, 3:4], in0=bs[:, 2:3], scalar1=-w, scalar2=None, op0=ALU.add),
                             reads=[t_mid], writes=[t_thr])
                    else:
                        c.op("dve", lambda e: e.memset(bs[:, 3:4], -1e29), writes=[t_thr])
                    c.op("dve", lambda e: e.tensor_scalar(out=mbias[b][:, 0:nk], in0=score[:, 0:nk], scalar1=bs[:, 3:4], scalar2=NEG,
                                                          op0=ALU.is_lt, op1=ALU.mult), reads=[t_score, t_thr], writes=[t_mbias[b]])
                    yield

                def prepB(i):
                    rows = rows_of(i)
                    qblk = i // 2
                    c.dma("sp", qm[:, :], qs[rows, 0:1024], writes=[t_qm])
                    for p4 in range(4):
                        c.op("pe", lambda e, p4=p4: e.transpose(m1[:, p4 * 128:(p4 + 1) * 128], qm[:, p4 * 128:(p4 + 1) * 128], ident_bf[:]),
                             reads=[t_qm, t_ident], writes=[t_m1])
                    c.op("act", lambda e: e.activation(out=qpad[0:64, :, 0, :], in_=m1[0:64, 0:512].rearrange("p (k t) -> p k t", t=128), func=AF.Copy),
                         reads=[t_m1], wadd=[t_qpad])
                    c.op("act", lambda e: e.activation(out=qpad[64:128, :, 1, :], in_=m1[64:128, 0:512].rearrange("p (k t) -> p k t", t=128), func=AF.Copy),
                         reads=[t_m1], wadd=[t_qpad])
                    for h in range(8):
                        c.op("pe", lambda e, h=h: e.transpose(m1[0:64, h * 128:(h + 1) * 128], qm[:, 512 + h * 64:512 + (h + 1) * 64], ident_bf[:]),
                             reads=[t_qm, t_ident], writes=[t_m1])
                    c.op("act", lambda e: e.activation(out=qbT[0:64, :, :], in_=m1[0:64, :].rearrange("p (k t) -> p k t", t=128), func=AF.Copy),
                         reads=[t_m1], wadd=[t_qbT])
                    for h in range(8):
                        c.op("pe", lambda e, h=h: e.matmul(m0[:, h * 16:(h + 1) * 16], lhsT=qpad[:, h // 2, h % 2, :], rhs=km_bf[:, h // 2, :],
                                                           start=True, stop=True),
                             reads=[t_qpad, t_km], writes=[t_m0])
                    c.op("dve", lambda e: e.tensor_copy(out=gatem[:, :, :], in_=m0[:, 0:128].rearrange("p (h n) -> p h n", n=16)),
                         reads=[t_m0], writes=[t_gatem])
                    c.op("dve", lambda e: e.memset(gatem[:, :, qblk:16], -1e30), reads=[t_gatem], writes=[t_gatem])
                    for h in range(8):
                        c.op("dve", lambda e, h=h: e.max(out=top8[:, h, :], in_=gatem[:, h, :]), reads=[t_gatem], wadd=[t_top8])
                    c.op("dve", lambda e: e.tensor_scalar(out=thr3[:, :].unsqueeze(2), in0=top8[:, :, 2:3], scalar1=-1e29, scalar2=None, op0=ALU.max),
                         reads=[t_top8], writes=[t_thr3])
                    c.op("dve", lambda e: e.tensor_tensor(out=mbf[:, :, :], in0=gatem[:, :, :],
                                                          in1=thr3[:, :].unsqueeze(2).to_broadcast([128, 8, 16]), op=ALU.is_lt),
                         reads=[t_gatem, t_thr3, t_top8], writes=[t_mbf])
                    c.op("dve", lambda e: e.tensor_scalar(out=mb16[:, :, :], in0=mbf[:, :, :], scalar1=NEG, scalar2=None, op0=ALU.mult),
                         reads=[t_mbf], writes=[t_mb16])
                    c.op("dve", lambda e: e.memset(mb16[:, :, qblk:qblk + 1], 0.0), reads=[t_mb16], writes=[t_mb16])
                    for h in range(8):
                        c.op("pe", lambda e, h=h: e.transpose(m1[0:16, h * 128:(h + 1) * 128], mb16[:, h, :], ident_bf[:]),
                             reads=[t_mb16, t_ident], writes=[t_m1])
                    c.op("act", lambda e: e.activation(out=mbT[0:16, :, :], in_=m1[0:16, :].rearrange("p (k t) -> p k t", t=128), func=AF.Copy),
                         reads=[t_m1], wadd=[t_mbT])

                def gen_C(i):
                    rows = rows_of(i)
                    c.dma("sp", gt[:, :], qs[rows, 1024:4096], writes=[t_gt])
                    c.dma("sp", xt[:, :], x_src[rows, :], writes=[t_xt])
                    for (br, g0, y_out, t_y) in ((0, 0, yag, t_yag), (1, 512, ybg, t_ybg)):
                        av = accs[br][:, :].rearrange("p (h d) -> p h d", d=65)
                        c.op("dve", lambda e: e.reciprocal(out=rec[:, :].unsqueeze(2), in_=av[:, :, 64:65]),
                             reads=[t_accs[br]], writes=[t_rec])
                        c.op("dve", lambda e: e.tensor_tensor(out=ytmp[:, :].rearrange("p (h d) -> p h d", d=64), in0=av[:, :, 0:64],
                                                              in1=rec[:, :].unsqueeze(2).to_broadcast([128, 8, 64]), op=ALU.mult),
                             reads=[t_accs[br], t_rec], writes=[t_ytmp])
                        c.op("dve", lambda e: e.tensor_tensor(out=y_out[:, :], in0=ytmp[:, :], in1=gt[:, g0:g0 + 512], op=ALU.mult),
                             reads=[t_ytmp, t_gt], writes=[t_y])
                        yield
                    for (ysrc, t_ys, yT, t_yT) in ((yag, t_yag, yaT, t_yaT), (ybg, t_ybg, ybT, t_ybT)):
                        for kc in range(4):
                            c.op("pe", lambda e, kc=kc, ysrc=ysrc: e.transpose(m1[:, kc * 128:(kc + 1) * 128], ysrc[:, kc * 128:(kc + 1) * 128], ident_bf[:]),
                                 reads=[t_ys, t_ident], writes=[t_m1])
                        c.op("act", lambda e, yT=yT: e.activation(out=yT[:, :, :], in_=m1[:, 0:512].rearrange("p (k t) -> p k t", t=128), func=AF.Copy),
                             reads=[t_m1], writes=[t_yT])
                        yield
                    for cc in range(2):
                        cs = slice(cc * 512, (cc + 1) * 512)
                        for kc in range(4):
                            c.op("pe", lambda e, kc=kc: e.matmul(mo[:, :], lhsT=yaT[:, kc, :], rhs=wbra[:, kc, cs], start=(kc == 0), stop=(kc == 3)),
                                 reads=[t_yaT, t_wbra], writes=[t_mo])
                        c.op("dve", lambda e: e.tensor_tensor(out=ytmp[:, :], in0=mo[:, :], in1=gt[:, 1024 + cc * 512:1024 + (cc + 1) * 512], op=ALU.mult),
                             reads=[t_mo, t_gt], writes=[t_ytmp])
                        yield
                        for kc in range(4):
                            c.op("pe", lambda e, kc=kc: e.matmul(mo[:, :], lhsT=ybT[:, kc, :], rhs=wbrb[:, kc, cs], start=(kc == 0), stop=(kc == 3)),
                                 reads=[t_ybT, t_wbrb], writes=[t_mo])
                        c.op("dve", lambda e: e.tensor_tensor(out=mtmp2[:, :], in0=mo[:, :], in1=gt[:, 2048 + cc * 512:2048 + (cc + 1) * 512], op=ALU.mult),
                             reads=[t_mo, t_gt], writes=[t_mtmp2])
                        c.op("dve", lambda e: e.tensor_tensor(out=mrg[:, cs], in0=ytmp[:, :], in1=mtmp2[:, :], op=ALU.add),
                             reads=[t_ytmp, t_mtmp2], wadd=[t_mrg])
                        yield
                    for kc in range(8):
                        c.op("pe", lambda e, kc=kc: e.transpose(m1[:, kc * 128:(kc + 1) * 128], mrg[:, kc * 128:(kc + 1) * 128], ident_bf[:]),
                             reads=[t_mrg, t_ident], writes=[t_m1])
                    c.op("act", lambda e: e.activation(out=mT[:, :, :], in_=m1[:, :].rearrange("p (k t) -> p k t", t=128), func=AF.Copy),
                         reads=[t_m1], writes=[t_mT])
                    yield
                    for cc in range(2):
                        cs = slice(cc * 512, (cc + 1) * 512)
                        for kc in range(8):
                            c.op("pe", lambda e, kc=kc: e.matmul(mo[:, :], lhsT=mT[:, kc, :], rhs=wo[:, kc, cs], start=(kc == 0), stop=(kc == 7)),
                                 reads=[t_mT, t_wo], writes=[t_mo])
                        c.op("dve", lambda e: e.tensor_tensor(out=xt[:, cs], in0=mo[:, :], in1=xt[:, cs], op=ALU.add),
                             reads=[t_mo], wadd=[t_xt])
                        yield
                    c.dma("pool", o_dst[rows, :], xt[:, :], reads=[t_xt])
                    yield

                ucnt = [0]

                def make_units(i):
                    units = []
                    for br in range(2):
                        for j in range(i + 1):
                            for hf in range(2):
                                units.append((br, j, hf))
                    return units

                def emit_scores(i, u, slot):
                    br, j, hf = u
                    near = j >= i - 1
                    e0 = 128 * (i - j)
                    o = stp[slot][:, :]
                    ts = t_stp[slot]
                    ks = slice(j * 128, (j + 1) * 128)
                    if br == 0:
                        c.op("pe", lambda e: e.matmul(o, lhsT=eall_bf[:, (j // 2) * 128:(j // 2 + 1) * 128],
                                                      rhs=mbT[:, hf * 4:(hf + 1) * 4, :].rearrange("p k t -> p (k t)"),
                                                      start=True, stop=False, skip_group_check=True),
                             reads=[t_eall, t_mbT], writes=[ts])
                        if near:
                            c.op("pe", lambda e: e.matmul(o, lhsT=ident_bf[:, :], rhs=bimg_hi[:, hf * 4:(hf + 1) * 4, e0:e0 + 128],
                                                          start=False, stop=False, skip_group_check=True),
                                 reads=[t_ident, t_bimg], writes=[ts])
                        for hq in range(4):
                            h = hf * 4 + hq
                            c.op("pe", lambda e, h=h, hq=hq: e.matmul(stp[slot][:, hq * 128:(hq + 1) * 128], lhsT=kaT2[:, h // 2, ks],
                                                                      rhs=qpad[:, h // 2, h % 2, :], start=False, stop=(hq == 3),
                                                                      skip_group_check=True),
                                 reads=[t_kaT2, t_qpad], writes=[ts])
                    else:
                        c.op("pe", lambda e: e.matmul(o, lhsT=kbT[:, ks], rhs=qbT[:, hf * 4:(hf + 1) * 4, :].rearrange("p k t -> p (k t)"),
                                                      start=True, stop=False, skip_group_check=True),
                             reads=[t_kbT[j], t_qbT], writes=[ts])
                        if near:
                            c.op("pe", lambda e: e.matmul(o, lhsT=ident_bf[:, :], rhs=bimg_hi[:, 8 + hf * 4:8 + (hf + 1) * 4, e0:e0 + 128],
                                                          start=False, stop=False, skip_group_check=True),
                                 reads=[t_ident, t_bimg], writes=[ts])
                        c.op("pe", lambda e: e.matmul(o, lhsT=mbias[i % 2][:, ks], rhs=irep_bf[:, :], start=False, stop=True,
                                                      skip_group_check=True),
                             reads=[t_mbias[i % 2], t_irep], writes=[ts])

                def emit_exp(slot):
                    c.op("act", lambda e: e.activation(out=pT[slot][:, :], in_=stp[slot][:, :], func=AF.Exp, scale=0.125),
                         reads=[t_stp[slot]], writes=[t_pT[slot]])

                def emit_pv(i, u, slot):
                    br, j, hf = u
                    for hq in range(4):
                        h = hf * 4 + hq
                        rhs = v1a[:, j, h, :] if br == 0 else v1b[:, j, :]
                        tv = t_v1a[j] if br == 0 else t_v1b[j]
                        c.op("pe", lambda e, hq=hq, rhs=rhs: e.matmul(acc[hf][:, hq * 65:hq * 65 + 65], lhsT=pT[slot][:, hq * 128:(hq + 1) * 128],
                                                                      rhs=rhs, start=(j == 0 and hq == 0), stop=(j == i and hq == 3),
                                                                      skip_group_check=True),
                             reads=[t_pT[slot], tv, t_ones], writes=[t_acc[hf]])
                    if j == i:
                        c.op("act", lambda e: e.activation(out=accs[br][:, hf * 260:(hf + 1) * 260], in_=acc[hf][:, 0:260], func=AF.Copy),
                             reads=[t_acc[hf]], wadd=[t_accs[br]])

                def interleave(gens):
                    gens = list(gens)
                    while gens:
                        for g in list(gens):
                            try:
                                next(g)
                                yield
                            except StopIteration:
                                gens.remove(g)

                def bg_schedule(gA, gC, nidx):
                    if gC is not None:
                        for _ in range(2):
                            next(gC)
                            yield
                    if gA is not None:
                        for _ in range(nidx):
                            try:
                                next(gA)
                                yield
                            except StopIteration:
                                break
                    yield from interleave([g for g in (gA, gC) if g is not None])

                def count_A(i):
                    nk = 128 * (i + 1)
                    return 3 + 4 * ((nk + 511) // 512) + (NBIS if i >= 2 else 0)

                for _ in gen_A(0):
                    pass
                prepB(0)
                for i in range(NT):
                    nbg = 0
                    gC = gA = None
                    if i - 1 >= 0:
                        gC = gen_C(i - 1)
                        nbg += 12
                    if i + 1 < NT:
                        gA = gen_A(i + 1)
                        nbg += count_A(i + 1)
                    bg = bg_schedule(gA, gC, 1 + 4 * ((128 * (i + 2) + 511) // 512))
                    units = make_units(i)
                    nU = len(units)
                    quota = -(-nbg // nU) if nU else nbg
                    slots = [(ucnt[0] + k) % 3 for k in range(nU)]
                    ucnt[0] += nU
                    emit_scores(i, units[0], slots[0])
                    for k in range(nU):
                        if k + 1 < nU:
                            emit_scores(i, units[k + 1], slots[k + 1])
                            if k + 2 == nU and i + 1 < NT:
                                prepB(i + 1)
                        emit_exp(slots[k])
                        emit_pv(i, units[k], slots[k])
                        for _ in range(quota):
                            try:
                                next(bg)
                            except StopIteration:
                                break
                    for _ in bg:
                        pass
                for _ in gen_C(NT - 1):
                    pass
                c.barrier()
        c.finish()
    return nc


_CACHE = {}


def kernel(x, norm_g, w_in, q_norm_a, k_norm_a, q_norm_b, k_norm_b, w_branch_a, w_branch_b, w_out, rel_bias):
    if "nc" not in _CACHE:
        _CACHE["nc"] = build_program()
    nc = _CACHE["nc"]
    f = lambda a: np.ascontiguousarray(np.asarray(a, dtype=np.float32))
    shared = {"norm_g": f(norm_g), "w_in": f(w_in), "q_norm_a": f(q_norm_a), "k_norm_a": f(k_norm_a),
              "q_norm_b": f(q_norm_b), "k_norm_b": f(k_norm_b), "w_branch_a": f(w_branch_a),
              "w_branch_b": f(w_branch_b), "w_out": f(w_out), "rel_bias": f(rel_bias)}
    shared.update(_host_consts())
    xs = f(x)
    in_maps = []
    for b in range(8):
        m = dict(shared)
        m["x"] = xs[b]
        in_maps.append(m)
    res = run_bass_kernel_spmd(nc, in_maps, core_ids=list(range(8)))
    return np.stack([np.asarray(r["out"]) for r in res.results], axis=0).astype(np.float32)
```

```python
import math
from contextlib import ExitStack

import numpy as np
import concourse.bass as bass
import concourse.mybir as mybir
from concourse.bass_utils import run_bass_kernel_spmd

F32 = mybir.dt.float32
BF16 = mybir.dt.bfloat16
ALU = mybir.AluOpType
AF = mybir.ActivationFunctionType
AX = mybir.AxisListType

S = 4096
D = 1024
NT = S // 128
DIN = 5572
DEPTH = 2
EPS = 1e-6
NEG = -30000.0
QS_W = 4352
NBIS = 22
W0 = 1024.0
TOPK = 256.0
MID0 = 2.0 ** -13
ACT_SHARE_NUM, ACT_SHARE_DEN = 0, 8

C_QA, C_KA, C_VA, C_GA = 0, 512, 1024, 1536
C_QB, C_KB, C_VB, C_GB = 2048, 2560, 2624, 2688
C_QI, C_KI, C_WI, C_MA, C_MB = 3200, 3456, 3520, 3524, 4548


class T:
    __slots__ = ("w", "r")

    def __init__(self):
        self.w = {}
        self.r = {}


class Ctx:
    RING = 8

    def __init__(self, nc, stack):
        self.nc = nc
        self.eng = {"pe": nc.tensor, "act": nc.scalar, "dve": nc.vector, "pool": nc.gpsimd, "sp": nc.sync}
        self.sem = {}
        self.cnt = {}
        for k in ("pe", "act", "dve", "pool"):
            self.sem[k] = stack.enter_context(nc.semaphore("s_" + k))
            self.cnt[k] = 0
        self.waited = {k: {} for k in self.eng}
        self.rings = {}
        self.ringpos = {}
        for q in ("sp", "pool"):
            self.rings[q] = []
            for i in range(self.RING):
                key = "d_%s_%d" % (q, i)
                self.sem[key] = stack.enter_context(nc.semaphore(key))
                self.cnt[key] = 0
                self.rings[q].append(key)
            self.ringpos[q] = 0

    def _wait(self, e, marks):
        need = {}
        for (k, v) in marks:
            if need.get(k, 0) < v:
                need[k] = v
        wd = self.waited[e]
        for k, v in need.items():
            if wd.get(k, 0) < v:
                self.eng[e].wait_ge(self.sem[k], v)
                wd[k] = v

    def _deps(self, e, reads, writes):
        marks = []
        for t in reads:
            for m in t.w.items():
                if m[0] == e and e == "pe":
                    continue
                marks.append(m)
        for t in writes:
            for m in t.w.items():
                if m[0] == e:
                    continue
                marks.append(m)
            for m in t.r.items():
                if m[0] == e:
                    continue
                marks.append(m)
        return marks

    def op(self, e, fn, reads=(), writes=(), wadd=()):
        self._wait(e, self._deps(e, reads, tuple(writes) + tuple(wadd)))
        ins = fn(self.eng[e])
        self.cnt[e] += 1
        ins.then_inc(self.sem[e], 1)
        v = self.cnt[e]
        for t in reads:
            t.r[e] = v
        for t in writes:
            t.w = {e: v}
            t.r = {}
        for t in wadd:
            t.w[e] = v
        return ins

    def dma(self, q, out, in_, reads=(), writes=(), wadd=(), **kw):
        key = self.rings[q][self.ringpos[q] % self.RING]
        self.ringpos[q] += 1
        marks = self._deps("dma", reads, tuple(writes) + tuple(wadd))
        if self.cnt[key] > 0:
            marks.append((key, self.cnt[key]))
        self._wait(q, marks)
        ins = self.eng[q].dma_start(out=out, in_=in_, **kw)
        self.cnt[key] += 16
        ins.then_inc(self.sem[key], 16)
        v = self.cnt[key]
        for t in reads:
            t.r[key] = v
        for t in writes:
            t.w = {key: v}
            t.r = {}
        for t in wadd:
            t.w[key] = v

    def all_marks(self):
        marks = []
        for k, v in self.cnt.items():
            if v > 0:
                marks.append((k, v))
        return marks

    def barrier(self):
        marks = self.all_marks()
        for e in ("pe", "act", "dve", "pool", "sp"):
            self._wait(e, [m for m in marks if m[0] != e])

    def finish(self):
        self._wait("sp", self.all_marks())


def _bucket_table():
    n = np.arange(0, 256)
    max_exact = 16
    nf = np.maximum(n, 1).astype(np.float32)
    large = max_exact + (np.log(nf / np.float32(max_exact)) / np.float32(math.log(128 / max_exact))
                         * np.float32(32 - max_exact)).astype(np.int32)
    large = np.minimum(large, 31)
    return np.where(n < max_exact, n, large)


def _host_consts():
    bk = _bucket_table()
    grev = np.zeros((33, 384), np.float32)
    for m in range(384):
        dist = 255 - m
        if dist >= 0:
            grev[bk[dist], m] = 1.0
        else:
            grev[32, m] = 1.0
    ident = np.eye(128, dtype=np.float32)
    irep = np.concatenate([ident] * 4, axis=1)
    t = np.arange(128)[:, None]
    s = np.arange(128)[None, :]
    causalneg = np.where(s <= t, 0.0, -1e30).astype(np.float32)
    eall = np.zeros((16, 16, 128), np.float32)
    for n in range(16):
        eall[n, n, :] = 1.0
    negrow = np.full((1, 16), NEG * 8.0, np.float32)
    return {"c_grev": grev, "c_ident": ident, "c_irep": irep, "c_causal": causalneg,
            "c_eall": eall.reshape(16, 2048), "c_negrow": negrow}


def build_program():
    nc = bass.Bass("TRN2", target_bir_lowering=False)
    din = lambda n, s, d=F32: nc.dram_tensor(n, s, d, kind="ExternalInput").ap()
    x = din("x", [S, D])
    norm_g = din("norm_g", [DEPTH, D])
    w_in = din("w_in", [DEPTH, D, DIN])
    qn_a = din("q_norm_a", [DEPTH, 64])
    kn_a = din("k_norm_a", [DEPTH, 64])
    qn_b = din("q_norm_b", [DEPTH, 64])
    kn_b = din("k_norm_b", [DEPTH, 64])
    w_bra = din("w_branch_a", [DEPTH, 512, D])
    w_brb = din("w_branch_b", [DEPTH, 512, D])
    w_out = din("w_out", [DEPTH, D, D])
    rel_bias = din("rel_bias", [32, 16])
    c_grev = din("c_grev", [33, 384])
    c_ident = din("c_ident", [128, 128])
    c_irep = din("c_irep", [128, 512])
    c_causal = din("c_causal", [128, 128])
    c_eall = din("c_eall", [16, 2048])
    c_negrow = din("c_negrow", [1, 16])
    out = nc.dram_tensor("out", [S, D], F32, kind="ExternalOutput").ap()
    scr = lambda n, s, d: nc.dram_tensor(n, s, d, kind="Internal").ap()
    qs = scr("qs", [S, QS_W], BF16)
    wis = scr("wis", [S, 4], F32)
    kaT_s = scr("kaT_s", [4, 128, S], BF16)
    x1 = scr("x1", [S, D], F32)

    top = ExitStack()
    with top:
        c = Ctx(nc, top)

        uid = [0]

        def sbt(st, name, shape, dt):
            uid[0] += 1
            return st.enter_context(nc.sbuf_tensor("%s_%d" % (name, uid[0]), shape, dt))

        def pst(st, name, shape, dt):
            uid[0] += 1
            return st.enter_context(nc.psum_tensor("%s_%d" % (name, uid[0]), shape, dt))

        ident_bf = sbt(top, "ident_bf", [128, 128], BF16); t_ident = T()
        irep_bf = sbt(top, "irep_bf", [128, 512], BF16); t_irep = T()
        causal = sbt(top, "causal", [128, 128], F32); t_causal = T()
        eall_bf = sbt(top, "eall_bf", [128, 2048], BF16); t_eall = T()
        bimg_hi = sbt(top, "bimg_hi", [128, 16, 256], BF16)
        t_bimg = T()
        v1a = sbt(top, "v1a", [128, NT, 8, 65], BF16); t_v1a = [T() for _ in range(NT)]
        v1b = sbt(top, "v1b", [128, NT, 65], BF16); t_v1b = [T() for _ in range(NT)]
        kbT = sbt(top, "kbT", [128, S], BF16); t_kbT = [T() for _ in range(NT)]
        kiT = sbt(top, "kiT", [128, S], BF16); t_kiT = [T() for _ in range(NT)]
        kmacc = sbt(top, "kmacc", [128, 4, 16], F32); t_kmacc = T()

        c.dma("pool", ident_bf[:], c_ident, writes=[t_ident])
        c.dma("pool", irep_bf[:], c_irep, writes=[t_irep])
        c.op("dve", lambda e: e.memset(eall_bf[:, :], 0.0), writes=[t_eall])
        c.dma("pool", eall_bf[0:16, :], c_eall, writes=[t_eall])
        t_kz = T()
        c.op("dve", lambda e: e.memset(kbT[64:128, :], 0.0), writes=[t_kz])
        c.op("dve", lambda e: e.memset(kiT[64:128, :], 0.0), writes=[t_kz])
        c.dma("sp", causal[:], c_causal, writes=[t_causal])
        t_ones = T()
        c.op("dve", lambda e: e.memset(v1a[:, :, :, 64:65], 1.0), writes=[t_ones])
        c.op("dve", lambda e: e.memset(v1b[:, :, 64:65], 1.0), writes=[t_ones])

        with ExitStack() as st:
            grev = sbt(st, "grev", [33, 384], F32); t_grev = T()
            reltab = sbt(st, "reltab", [33, 16], F32); t_rel = T()
            r31 = sbt(st, "r31", [32, 16], F32); t_r31 = T()
            bps = pst(st, "bps", [128, 2048], F32); t_bps = T()
            c.dma("sp", grev[:], c_grev, writes=[t_grev])
            c.dma("sp", reltab[0:32, :], rel_bias, writes=[t_rel])
            c.dma("sp", r31[:], rel_bias[31:32, :].to_broadcast([32, 16]), writes=[t_r31])
            c.op("dve", lambda e: e.tensor_tensor(out=reltab[0:32, :], in0=reltab[0:32, :], in1=r31[:], op=ALU.subtract),
                 reads=[t_r31, t_rel], writes=[t_rel])
            c.op("dve", lambda e: e.tensor_scalar(out=reltab[0:32, :], in0=reltab[0:32, :], scalar1=8.0, scalar2=None, op0=ALU.mult),
                 reads=[t_rel], writes=[t_rel])
            c.dma("sp", reltab[32:33, :], c_negrow, reads=[t_rel], writes=[t_rel])
            for half in range(2):
                for el in range(128):
                    e_ = half * 128 + el
                    c.op("pe", lambda e, el=el, e_=e_: e.matmul(bps[:, el * 16:(el + 1) * 16], lhsT=grev[0:33, 255 - e_:255 - e_ + 128],
                                                                 rhs=reltab[0:33, :], start=True, stop=True),
                         reads=[t_grev, t_rel], writes=[t_bps])
                src = bps[:, :].rearrange("p (e h) -> p h e", h=16)
                hi_v = bimg_hi[:, :, half * 128:(half + 1) * 128]
                c.op("dve", lambda e: e.tensor_copy(out=hi_v, in_=src), reads=[t_bps], wadd=[t_bimg])
            c.barrier()

        for l in range(DEPTH):
            x_src = x if l == 0 else x1
            o_dst = x1 if l == 0 else out
            with ExitStack() as st:
                w_bf = sbt(st, "w_bf", [128, 8, DIN], BF16); t_w = [T() for _ in range(8)]
                gbc = sbt(st, "gbc", [128, D], F32); t_gbc = T()
                gq_a = sbt(st, "gq_a", [128, 64], F32)
                gk_a = sbt(st, "gk_a", [128, 64], F32)
                gq_b = sbt(st, "gq_b", [128, 64], F32)
                gk_b = sbt(st, "gk_b", [128, 64], F32)
                t_gs = T()
                xb = [sbt(st, "xb%d" % i, [128, D], F32) for i in range(2)]; t_xb = [T(), T()]
                sqj = sbt(st, "sqj", [128, D], BF16); t_sqj = T()
                stat = sbt(st, "stat", [128, 8], F32); t_stat = T()
                hb = sbt(st, "hb", [128, D], BF16); t_hb = T()
                hT = [sbt(st, "hT%d" % i, [128, 8, 128], BF16) for i in range(2)]; t_hT = [T(), T()]
                qt = [sbt(st, "qt%d" % i, [128, QS_W], BF16) for i in range(2)]; t_qt = [T(), T()]
                sqf = sbt(st, "sqf", [128, 512], F32); t_sqf = T()
                hst = sbt(st, "hst", [128, 32], F32); t_hst = T()
                tmpf = sbt(st, "tmpf", [128, 512], F32); t_tmpf = T()
                kan = sbt(st, "kan", [128, 512], BF16); t_kan = T()
                kTt = [sbt(st, "kTt%d" % i, [128, 4, 128], BF16) for i in range(2)]; t_kTt = [T(), T()]
                kred = sbt(st, "kred", [128, 4], F32); t_kred = T()
                sm = sbt(st, "sm", [128, 128], BF16); t_sm = T()
                wit = [sbt(st, "witp%d" % i, [128, 4], F32) for i in range(2)]; t_wit = [T(), T()]
                tp = pst(st, "tp", [128, 1024], BF16); t_tp = T()
                tp2 = pst(st, "tp2", [128, 1024], BF16); t_tp2 = T()
                pj = [pst(st, "pj%d" % i, [128, 512], F32) for i in range(6)]; t_pj = [T() for _ in range(6)]

                for kc in range(8):
                    for (c0, c1) in ((0, 2048), (2048, 4096), (4096, DIN)):
                        c.dma("pool", w_bf[:, kc, c0:c1], w_in[l, kc * 128:(kc + 1) * 128, c0:c1], wadd=[t_w[kc]])
                c.dma("sp", gbc[:], norm_g[l:l + 1, :].to_broadcast([128, D]), writes=[t_gbc])
                for (g_sb, g_dr) in ((gq_a, qn_a), (gk_a, kn_a), (gq_b, qn_b), (gk_b, kn_b)):
                    c.dma("sp", g_sb[:], g_dr[l:l + 1, :].to_broadcast([128, 64]), wadd=[t_gs])
                c.op("dve", lambda e: e.memset(kmacc[:], 0.0), writes=[t_kmacc])

                pjn = [0]

                def project(i, col0, ncols):
                    b = pjn[0] % 6
                    pjn[0] += 1
                    for kc in range(8):
                        c.op("pe", lambda e, kc=kc: e.matmul(pj[b][:, 0:ncols], lhsT=hT[i % 2][:, kc, :],
                                                              rhs=w_bf[:, kc, col0:col0 + ncols],
                                                              start=(kc == 0), stop=(kc == 7)),
                             reads=[t_hT[i % 2], t_w[kc]], writes=[t_pj[b]])
                    return pj[b], t_pj[b]

                def rsqrt_small(ap_in, ap_out, n, inv_n):
                    c.op("dve", lambda e: e.tensor_scalar(out=ap_out, in0=ap_in, scalar1=inv_n, scalar2=EPS,
                                                          op0=ALU.mult, op1=ALU.add), reads=[t_hst], writes=[t_hst])
                    c.op("act", lambda e: e.activation(out=ap_out, in_=ap_out, func=AF.Ln), reads=[t_hst], writes=[t_hst])
                    c.op("act", lambda e: e.activation(out=ap_out, in_=ap_out, func=AF.Exp, scale=-0.5),
                         reads=[t_hst], writes=[t_hst])

                def headnorm(ps_ap, t_ps, H, g_sb, out_ap, t_out, wadd_out=False):
                    W = H * 64
                    c.op("act", lambda e: e.activation(out=sqf[:, 0:W], in_=ps_ap, func=AF.Square),
                         reads=[t_ps], writes=[t_sqf])
                    c.op("dve", lambda e: e.tensor_reduce(out=hst[:, 0:H], in_=sqf[:, 0:W].rearrange("p (h d) -> p h d", d=64),
                                                          axis=AX.X, op=ALU.add), reads=[t_sqf], writes=[t_hst])
                    rsqrt_small(hst[:, 0:H], hst[:, 0:H], H, 1.0 / 64.0)
                    c.op("dve", lambda e: e.tensor_tensor(out=tmpf[:, 0:W].rearrange("p (h d) -> p h d", d=64),
                                                          in0=ps_ap.rearrange("p (h d) -> p h d", d=64),
                                                          in1=hst[:, 0:H].unsqueeze(2).to_broadcast([128, H, 64]), op=ALU.mult),
                         reads=[t_ps, t_hst], writes=[t_tmpf])
                    kw = dict(wadd=[t_out]) if wadd_out else dict(writes=[t_out])
                    c.op("dve", lambda e: e.tensor_tensor(out=out_ap.rearrange("p (h d) -> p h d", d=64),
                                                          in0=tmpf[:, 0:W].rearrange("p (h d) -> p h d", d=64),
                                                          in1=g_sb[:, :].unsqueeze(1).to_broadcast([128, H, 64]), op=ALU.mult),
                         reads=[t_tmpf, t_gs], **kw)

                def norm_act(i):
                    rows = slice(i * 128, (i + 1) * 128)
                    xt, t_xt = xb[i % 2], t_xb[i % 2]
                    c.dma("sp", xt[:], x_src[rows, :], writes=[t_xt])
                    c.op("act", lambda e: e.activation(out=sqj[:], in_=xt[:], func=AF.Square, accum_out=stat[:, 0:1]),
                         reads=[t_xt], writes=[t_sqj, t_stat])
                    c.op("dve", lambda e: e.tensor_scalar(out=stat[:, 1:2], in0=stat[:, 0:1], scalar1=1.0 / D, scalar2=EPS,
                                                          op0=ALU.mult, op1=ALU.add), reads=[t_stat], writes=[t_stat])
                    c.op("act", lambda e: e.activation(out=stat[:, 2:3], in_=stat[:, 1:2], func=AF.Ln), reads=[t_stat], writes=[t_stat])
                    c.op("act", lambda e: e.activation(out=stat[:, 3:4], in_=stat[:, 2:3], func=AF.Exp, scale=-0.5),
                         reads=[t_stat], writes=[t_stat])
                    c.op("dve", lambda e: e.scalar_tensor_tensor(out=hb[:], in0=xt[:], scalar=stat[:, 3:4], in1=gbc[:],
                                                                 op0=ALU.mult, op1=ALU.mult),
                         reads=[t_xt, t_stat, t_gbc], writes=[t_hb])

                def norm_pe(i):
                    for kc in range(8):
                        c.op("pe", lambda e, kc=kc: e.transpose(tp[:, kc * 128:(kc + 1) * 128], hb[:, kc * 128:(kc + 1) * 128], ident_bf[:]),
                             reads=[t_hb, t_ident], writes=[t_tp])
                    c.op("act", lambda e: e.activation(out=hT[i % 2][:, :, :], in_=tp[:, :].rearrange("p (k t) -> p k t", t=128), func=AF.Copy),
                         reads=[t_tp], writes=[t_hT[i % 2]])

                norm_act(0)
                norm_pe(0)
                for i in range(NT):
                    rows = slice(i * 128, (i + 1) * 128)
                    q, t_q = qt[i % 2], t_qt[i % 2]
                    kt, t_kt = kTt[i % 2], t_kTt[i % 2]
                    if i + 1 < NT:
                        norm_act(i + 1)
                    ps, tps = project(i, C_QA, 512)
                    headnorm(ps[:, 0:512], tps, 8, gq_a, q[:, 0:512], t_q)
                    ps, tps = project(i, C_KA, 512)
                    headnorm(ps[:, 0:512], tps, 8, gk_a, kan[:, :], t_kan)
                    ps, tps = project(i, C_QB, 512)
                    headnorm(ps[:, 0:512], tps, 8, gq_b, q[:, 512:1024], t_q, wadd_out=True)
                    ps, tps = project(i, C_KB, 128)
                    headnorm(ps[:, 0:64], tps, 1, gk_b, sm[:, 0:64], t_sm)
                    c.op("dve", lambda e: e.tensor_copy(out=v1b[:, i, 0:64], in_=ps[:, 64:128]), reads=[tps], writes=[t_v1b[i]])
                    if i + 1 < NT:
                        norm_pe(i + 1)
                    ps2, tps2 = project(i, C_QI, 324)
                    c.op("dve", lambda e: e.tensor_copy(out=q[:, 4096:4352], in_=ps2[:, 0:256]), reads=[tps2], wadd=[t_q])
                    c.op("dve", lambda e: e.tensor_copy(out=sm[:, 64:128], in_=ps2[:, 256:320]), reads=[tps2], wadd=[t_sm])
                    c.op("dve", lambda e: e.tensor_copy(out=wit[i % 2][:, :], in_=ps2[:, 320:324]), reads=[tps2], writes=[t_wit[i % 2]])
                    c.dma("pool", wis[rows, :], wit[i % 2][:, :], reads=[t_wit[i % 2]])
                    for p4 in range(4):
                        c.op("pe", lambda e, p4=p4: e.transpose(tp2[:, p4 * 128:(p4 + 1) * 128], kan[:, p4 * 128:(p4 + 1) * 128], ident_bf[:]),
                             reads=[t_kan, t_ident], writes=[t_tp2])
                    c.op("act", lambda e: e.activation(out=kt[:, :, :], in_=tp2[:, 0:512].rearrange("p (k t) -> p k t", t=128), func=AF.Copy),
                         reads=[t_tp2], writes=[t_kt])
                    for p4 in range(4):
                        c.dma("pool", kaT_s[p4, :, i * 128:(i + 1) * 128], kt[:, p4, :], reads=[t_kt])
                    c.op("dve", lambda e: e.tensor_reduce(out=kred[:, :], in_=kt[:, :, :], axis=AX.X, op=ALU.add),
                         reads=[t_kt], writes=[t_kred])
                    nblk = i // 2
                    c.op("dve", lambda e: e.tensor_tensor(out=kmacc[:, :, nblk], in0=kmacc[:, :, nblk], in1=kred[:, :], op=ALU.add),
                         reads=[t_kred, t_kmacc], writes=[t_kmacc])
                    ps, tps = project(i, C_VA, 512)
                    c.op("act", lambda e: e.activation(out=v1a[:, i, :, 0:64], in_=ps[:, 0:512].rearrange("p (h d) -> p h d", d=64), func=AF.Copy),
                         reads=[tps], writes=[t_v1a[i]])
                    c.op("pe", lambda e: e.transpose(tp2[0:64, 512:640], sm[:, 0:64], ident_bf[:]), reads=[t_sm, t_ident], writes=[t_tp2])
                    c.op("pe", lambda e: e.transpose(tp2[0:64, 640:768], sm[:, 64:128], ident_bf[:]), reads=[t_sm, t_ident], writes=[t_tp2])
                    c.op("act", lambda e: e.activation(out=kbT[0:64, i * 128:(i + 1) * 128], in_=tp2[0:64, 512:640], func=AF.Copy),
                         reads=[t_tp2], writes=[t_kbT[i]])
                    c.op("act", lambda e: e.activation(out=kiT[0:64, i * 128:(i + 1) * 128], in_=tp2[0:64, 640:768], func=AF.Copy),
                         reads=[t_tp2], writes=[t_kiT[i]])
                    ps, tps = project(i, C_GA, 512)
                    c.op("act", lambda e: e.activation(out=q[:, 1024:1536], in_=ps[:, 0:512], func=AF.Silu), reads=[tps], wadd=[t_q])
                    ps, tps = project(i, C_GB, 512)
                    c.op("act", lambda e: e.activation(out=q[:, 1536:2048], in_=ps[:, 0:512], func=AF.Silu), reads=[tps], wadd=[t_q])
                    for gi, cm in enumerate((C_MA, C_MA + 512, C_MB, C_MB + 512)):
                        ps, tps = project(i, cm, 512)
                        c.op("act", lambda e, gi=gi: e.activation(out=q[:, 2048 + gi * 512:2048 + (gi + 1) * 512], in_=ps[:, 0:512], func=AF.Sigmoid),
                             reads=[tps], wadd=[t_q])
                    c.dma("pool", qs[rows, :], q[:, :], reads=[t_q])
                c.barrier()

            with ExitStack() as st:
                kaT2 = sbt(st, "kaT2", [128, 4, S], BF16); t_kaT2 = T()
                km_bf = sbt(st, "km_bf", [128, 4, 16], BF16); t_km = T()
                wbra = sbt(st, "wbra", [128, 4, D], BF16); t_wbra = T()
                wbrb = sbt(st, "wbrb", [128, 4, D], BF16); t_wbrb = T()
                wo = sbt(st, "wo", [128, 8, D], BF16); t_wo = T()
                qm = sbt(st, "qm", [128, 1024], BF16); t_qm = T()
                qmi = sbt(st, "qmi", [128, 256], BF16); t_qmi = T()
                gt = sbt(st, "gt", [128, 3072], BF16); t_gt = T()
                wit2 = [sbt(st, "wit2%d" % k, [128, 4], F32) for k in range(2)]; t_wit2 = [T(), T()]
                xt = sbt(st, "xt", [128, D], F32); t_xt = T()
                qpad = sbt(st, "qpad", [128, 4, 2, 128], BF16); t_qpad = T()
                qbT = sbt(st, "qbT", [128, 8, 128], BF16); t_qbT = T()
                qiT = [sbt(st, "qiT%d" % k, [128, 4, 128], BF16) for k in range(2)]; t_qiT = [T(), T()]
                score = sbt(st, "score", [128, S], F32); t_score = T()
                mbias = [sbt(st, "mbias%d" % k, [128, S], BF16) for k in range(2)]; t_mbias = [T(), T()]
                rl2 = [sbt(st, "rl%d" % k, [128, 512], F32) for k in range(2)]; t_rl2 = [T(), T()]
                bs = sbt(st, "bs", [128, 8], F32)
                t_cnt = T(); t_tmp = T(); t_mid = T(); t_thr = T()
                sg = sbt(st, "sg", [128, 2], F32); t_sg = T()
                gatem = sbt(st, "gatem", [128, 8, 16], F32); t_gatem = T()
                top8 = sbt(st, "top8", [128, 8, 8], F32); t_top8 = T()
                thr3 = sbt(st, "thr3", [128, 8], F32); t_thr3 = T()
                mbf = sbt(st, "mbf", [128, 8, 16], F32); t_mbf = T()
                mb16 = sbt(st, "mb16", [128, 8, 16], BF16); t_mb16 = T()
                mbT = sbt(st, "mbT", [128, 8, 128], BF16); t_mbT = T()
                pT = [sbt(st, "pT%d" % k, [128, 512], BF16) for k in range(2)]; t_pT = [T(), T()]
                accs = [sbt(st, "accs%d" % k, [128, 520], F32) for k in range(2)]; t_accs = [T(), T()]
                rec = sbt(st, "rec", [128, 8], F32); t_rec = T()
                ytmp = sbt(st, "ytmp", [128, 512], F32); t_ytmp = T()
                yag = sbt(st, "yag", [128, 512], BF16); t_yag = T()
                ybg = sbt(st, "ybg", [128, 512], BF16); t_ybg = T()
                yaT = sbt(st, "yaT", [128, 4, 128], BF16); t_yaT = T()
                ybT = sbt(st, "ybT", [128, 4, 128], BF16); t_ybT = T()
                mtmp2 = rl2[1]; t_mtmp2 = t_rl2[1]
                mrg = sbt(st, "mrg", [128, D], BF16); t_mrg = T()
                mT = sbt(st, "mT", [128, 8, 128], BF16); t_mT = T()
                stp = [pst(st, "stp%d" % k, [128, 512], F32) for k in range(3)]; t_stp = [T(), T(), T()]
                acc = [pst(st, "acc%d" % k, [128, 512], F32) for k in range(2)]; t_acc = [T(), T()]
                m0 = pst(st, "m0", [128, 512], F32); t_m0 = T()
                m1 = pst(st, "m1", [128, 1024], BF16); t_m1 = T()
                mo = pst(st, "mo", [128, 512], F32); t_mo = T()

                for p4 in range(4):
                    c.dma("sp", kaT2[:, p4, :], kaT_s[p4, :, :], wadd=[t_kaT2])
                for kc in range(4):
                    c.dma("pool", wbra[:, kc, :], w_bra[l, kc * 128:(kc + 1) * 128, :], wadd=[t_wbra])
                    c.dma("pool", wbrb[:, kc, :], w_brb[l, kc * 128:(kc + 1) * 128, :], wadd=[t_wbrb])
                for kc in range(8):
                    c.dma("pool", wo[:, kc, :], w_out[l, kc * 128:(kc + 1) * 128, :], wadd=[t_wo])
                c.op("dve", lambda e: e.tensor_scalar(out=km_bf[:, :, :], in0=kmacc[:, :, :], scalar1=1.0 / 256.0, scalar2=None, op0=ALU.mult),
                     reads=[t_kmacc], writes=[t_km])
                c.op("dve", lambda e: e.memset(qpad[:, :, :, :], 0.0), writes=[t_qpad])
                c.op("dve", lambda e: e.memset(qbT[:, :, :], 0.0), writes=[t_qbT])
                c.op("dve", lambda e: e.memset(mbT[:, :, :], 0.0), writes=[t_mbT])
                for k in range(2):
                    c.op("dve", lambda e, k=k: e.memset(qiT[k][:, :, :], 0.0), writes=[t_qiT[k]])

                def rows_of(i):
                    return slice(i * 128, (i + 1) * 128)

                idxn = [0]

                def gen_A(i):
                    rows = rows_of(i)
                    nk = 128 * (i + 1)
                    b = i % 2
                    c.dma("sp", qmi[:, :], qs[rows, 4096:4352], writes=[t_qmi])
                    c.dma("sp", wit2[b][:, :], wis[rows, :], writes=[t_wit2[b]])
                    for h in range(4):
                        c.op("pe", lambda e, h=h: e.transpose(m1[0:64, h * 128:(h + 1) * 128], qmi[:, h * 64:(h + 1) * 64], ident_bf[:]),
                             reads=[t_qmi, t_ident], writes=[t_m1])
                    c.op("act", lambda e: e.activation(out=qiT[b][0:64, :, :], in_=m1[0:64, 0:512].rearrange("p (k t) -> p k t", t=128), func=AF.Copy),
                         reads=[t_m1], wadd=[t_qiT[b]])
                    yield
                    for c0 in range(0, nk, 512):
                        n = min(512, nk - c0)
                        kdeps = [t_kiT[jj] for jj in range(c0 // 128, (c0 + n) // 128)]
                        for hh in range(4):
                            ib = idxn[0] % 2
                            idxn[0] += 1
                            pm, t_pm = (m0, t_m0) if ib == 0 else (mo, t_mo)
                            rl, t_rl = rl2[ib], t_rl2[ib]
                            c.op("pe", lambda e, hh=hh, pm=pm: e.matmul(pm[:, 0:n], lhsT=qiT[b][:, hh, :], rhs=kiT[:, c0:c0 + n], start=True, stop=True),
                                 reads=[t_qiT[b]] + kdeps, writes=[t_pm])
                            c.op("act", lambda e, pm=pm, rl=rl: e.activation(out=rl[:, 0:n], in_=pm[:, 0:n], func=AF.Relu),
                                 reads=[t_pm], writes=[t_rl])
                            if hh == 0:
                                c.op("dve", lambda e, rl=rl: e.tensor_scalar(out=score[:, c0:c0 + n], in0=rl[:, 0:n], scalar1=wit2[b][:, 0:1],
                                                                             scalar2=None, op0=ALU.mult),
                                     reads=[t_rl, t_wit2[b]], writes=[t_score])
                            else:
                                c.op("dve", lambda e, hh=hh, rl=rl: e.scalar_tensor_tensor(out=score[:, c0:c0 + n], in0=rl[:, 0:n],
                                                                                          scalar=wit2[b][:, hh:hh + 1], in1=score[:, c0:c0 + n],
                                                                                          op0=ALU.mult, op1=ALU.add),
                                     reads=[t_rl, t_wit2[b], t_score], writes=[t_score])
                            yield
                    c.op("dve", lambda e: e.tensor_tensor(out=score[:, i * 128:(i + 1) * 128], in0=score[:, i * 128:(i + 1) * 128],
                                                          in1=causal[:, :], op=ALU.add),
                         reads=[t_score, t_causal], writes=[t_score])
                    if i >= 2:
                        n_act = 128 * (((i + 1) * ACT_SHARE_NUM) // ACT_SHARE_DEN)
                        n1 = nk - n_act
                        c.op("dve", lambda e: e.memset(bs[:, 2:3], MID0), writes=[t_mid])
                        w = W0
                        for k in range(NBIS):
                            c.op("dve", lambda e: e.tensor_scalar(out=mbias[b][:, 0:n1], in0=score[:, 0:n1], scalar1=bs[:, 2:3], scalar2=None,
                                                                  op0=ALU.is_ge, op1=ALU.add, accum_out=bs[:, 0:1]),
                                 reads=[t_score, t_mid], writes=[t_cnt], wadd=[t_mbias[b]])
                            if n_act > 0:
                                c.op("act", lambda e: e.activation(out=mbias[b][:, n1:nk], in_=score[:, n1:nk], func=AF.Sign,
                                                                   bias=bs[:, 2:3], scale=-1.0, accum_out=sg[:, 0:1]),
                                     reads=[t_score, t_mid], writes=[t_sg], wadd=[t_mbias[b]])
                                c.op("dve", lambda e: e.scalar_tensor_tensor(out=bs[:, 4:5], in0=bs[:, 0:1], scalar=2.0, in1=sg[:, 0:1],
                                                                             op0=ALU.mult, op1=ALU.subtract),
                                     reads=[t_cnt, t_sg], writes=[t_tmp])
                                c.op("dve", lambda e: e.tensor_scalar(out=bs[:, 1:2], in0=bs[:, 4:5], scalar1=2.0 * TOPK - n_act, scalar2=0.5,
                                                                      op0=ALU.is_ge, op1=ALU.subtract), reads=[t_tmp], writes=[t_tmp])
                            else:
                                c.op("dve", lambda e: e.tensor_scalar(out=bs[:, 1:2], in0=bs[:, 0:1], scalar1=TOPK, scalar2=0.5,
                                                                      op0=ALU.is_ge, op1=ALU.subtract), reads=[t_cnt], writes=[t_tmp])
                            c.op("dve", lambda e, w=w: e.scalar_tensor_tensor(out=bs[:, 2:3], in0=bs[:, 1:2], scalar=w, in1=bs[:, 2:3],
                                                                              op0=ALU.mult, op1=ALU.add), reads=[t_tmp, t_mid], writes=[t_mid])
                            w = w / 2.0
                            yield
                        c.op("dve", lambda e, w=w: e.tensor_scalar(out=bs[:, 3:4], in0=bs[:, 2:3], scalar1=-w, scalar2=None, op0=ALU.add),
                             reads=[t_mid], writes=[t_thr])
                    else:
                        c.op("dve", lambda e: e.memset(bs[:, 3:4], -1e29), writes=[t_thr])
                    c.op("dve", lambda e: e.tensor_scalar(out=mbias[b][:, 0:nk], in0=score[:, 0:nk], scalar1=bs[:, 3:4], scalar2=NEG,
                                                          op0=ALU.is_lt, op1=ALU.mult), reads=[t_score, t_thr], writes=[t_mbias[b]])
                    yield

                def prepB(i):
                    rows = rows_of(i)
                    qblk = i // 2
                    c.dma("sp", qm[:, :], qs[rows, 0:1024], writes=[t_qm])
                    for p4 in range(4):
                        c.op("pe", lambda e, p4=p4: e.transpose(m1[:, p4 * 128:(p4 + 1) * 128], qm[:, p4 * 128:(p4 + 1) * 128], ident_bf[:]),
                             reads=[t_qm, t_ident], writes=[t_m1])
                    c.op("act", lambda e: e.activation(out=qpad[0:64, :, 0, :], in_=m1[0:64, 0:512].rearrange("p (k t) -> p k t", t=128), func=AF.Copy),
                         reads=[t_m1], wadd=[t_qpad])
                    c.op("act", lambda e: e.activation(out=qpad[64:128, :, 1, :], in_=m1[64:128, 0:512].rearrange("p (k t) -> p k t", t=128), func=AF.Copy),
                         reads=[t_m1], wadd=[t_qpad])
                    for h in range(8):
                        c.op("pe", lambda e, h=h: e.transpose(m1[0:64, h * 128:(h + 1) * 128], qm[:, 512 + h * 64:512 + (h + 1) * 64], ident_bf[:]),
                             reads=[t_qm, t_ident], writes=[t_m1])
                    c.op("act", lambda e: e.activation(out=qbT[0:64, :, :], in_=m1[0:64, :].rearrange("p (k t) -> p k t", t=128), func=AF.Copy),
                         reads=[t_m1], wadd=[t_qbT])
                    for h in range(8):
                        c.op("pe", lambda e, h=h: e.matmul(m0[:, h * 16:(h + 1) * 16], lhsT=qpad[:, h // 2, h % 2, :], rhs=km_bf[:, h // 2, :],
                                                           start=True, stop=True),
                             reads=[t_qpad, t_km], writes=[t_m0])
                    c.op("dve", lambda e: e.tensor_copy(out=gatem[:, :, :], in_=m0[:, 0:128].rearrange("p (h n) -> p h n", n=16)),
                         reads=[t_m0], writes=[t_gatem])
                    c.op("dve", lambda e: e.memset(gatem[:, :, qblk:16], -1e30), reads=[t_gatem], writes=[t_gatem])
                    for h in range(8):
                        c.op("dve", lambda e, h=h: e.max(out=top8[:, h, :], in_=gatem[:, h, :]), reads=[t_gatem], wadd=[t_top8])
                    c.op("dve", lambda e: e.tensor_scalar(out=thr3[:, :].unsqueeze(2), in0=top8[:, :, 2:3], scalar1=-1e29, scalar2=None, op0=ALU.max),
                         reads=[t_top8], writes=[t_thr3])
                    c.op("dve", lambda e: e.tensor_tensor(out=mbf[:, :, :], in0=gatem[:, :, :],
                                                          in1=thr3[:, :].unsqueeze(2).to_broadcast([128, 8, 16]), op=ALU.is_lt),
                         reads=[t_gatem, t_thr3, t_top8], writes=[t_mbf])
                    c.op("dve", lambda e: e.tensor_scalar(out=mb16[:, :, :], in0=mbf[:, :, :], scalar1=NEG, scalar2=None, op0=ALU.mult),
                         reads=[t_mbf], writes=[t_mb16])
                    c.op("dve", lambda e: e.memset(mb16[:, :, qblk:qblk + 1], 0.0), reads=[t_mb16], writes=[t_mb16])
                    for h in range(8):
                        c.op("pe", lambda e, h=h: e.transpose(m1[0:16, h * 128:(h + 1) * 128], mb16[:, h, :], ident_bf[:]),
                             reads=[t_mb16, t_ident], writes=[t_m1])
                    c.op("act", lambda e: e.activation(out=mbT[0:16, :, :], in_=m1[0:16, :].rearrange("p (k t) -> p k t", t=128), func=AF.Copy),
                         reads=[t_m1], wadd=[t_mbT])

                def gen_C(i):
                    rows = rows_of(i)
                    c.dma("sp", gt[:, :], qs[rows, 1024:4096], writes=[t_gt])
                    c.dma("sp", xt[:, :], x_src[rows, :], writes=[t_xt])
                    for (br, g0, y_out, t_y) in ((0, 0, yag, t_yag), (1, 512, ybg, t_ybg)):
                        av = accs[br][:, :].rearrange("p (h d) -> p h d", d=65)
                        c.op("dve", lambda e: e.reciprocal(out=rec[:, :].unsqueeze(2), in_=av[:, :, 64:65]),
                             reads=[t_accs[br]], writes=[t_rec])
                        c.op("dve", lambda e: e.tensor_tensor(out=ytmp[:, :].rearrange("p (h d) -> p h d", d=64), in0=av[:, :, 0:64],
                                                              in1=rec[:, :].unsqueeze(2).to_broadcast([128, 8, 64]), op=ALU.mult),
                             reads=[t_accs[br], t_rec], writes=[t_ytmp])
                        c.op("dve", lambda e: e.tensor_tensor(out=y_out[:, :], in0=ytmp[:, :], in1=gt[:, g0:g0 + 512], op=ALU.mult),
                             reads=[t_ytmp, t_gt], writes=[t_y])
                        yield
                    for (ysrc, t_ys, yT, t_yT) in ((yag, t_yag, yaT, t_yaT), (ybg, t_ybg, ybT, t_ybT)):
                        for kc in range(4):
                            c.op("pe", lambda e, kc=kc, ysrc=ysrc: e.transpose(m1[:, kc * 128:(kc + 1) * 128], ysrc[:, kc * 128:(kc + 1) * 128], ident_bf[:]),
                                 reads=[t_ys, t_ident], writes=[t_m1])
                        c.op("act", lambda e, yT=yT: e.activation(out=yT[:, :, :], in_=m1[:, 0:512].rearrange("p (k t) -> p k t", t=128), func=AF.Copy),
                             reads=[t_m1], writes=[t_yT])
                        yield
                    for cc in range(2):
                        cs = slice(cc * 512, (cc + 1) * 512)
                        for kc in range(4):
                            c.op("pe", lambda e, kc=kc: e.matmul(mo[:, :], lhsT=yaT[:, kc, :], rhs=wbra[:, kc, cs], start=(kc == 0), stop=(kc == 3)),
                                 reads=[t_yaT, t_wbra], writes=[t_mo])
                        c.op("dve", lambda e: e.tensor_tensor(out=ytmp[:, :], in0=mo[:, :], in1=gt[:, 1024 + cc * 512:1024 + (cc + 1) * 512], op=ALU.mult),
                             reads=[t_mo, t_gt], writes=[t_ytmp])
                        yield
                        for kc in range(4):
                            c.op("pe", lambda e, kc=kc: e.matmul(mo[:, :], lhsT=ybT[:, kc, :], rhs=wbrb[:, kc, cs], start=(kc == 0), stop=(kc == 3)),
                                 reads=[t_ybT, t_wbrb], writes=[t_mo])
                        c.op("dve", lambda e: e.tensor_tensor(out=mtmp2[:, :], in0=mo[:, :], in1=gt[:, 2048 + cc * 512:2048 + (cc + 1) * 512], op=ALU.mult),
                             reads=[t_mo, t_gt], writes=[t_mtmp2])
                        c.op("dve", lambda e: e.tensor_tensor(out=mrg[:, cs], in0=ytmp[:, :], in1=mtmp2[:, :], op=ALU.add),
                             reads=[t_ytmp, t_mtmp2], wadd=[t_mrg])
                        yield
                    for kc in range(8):
                        c.op("pe", lambda e, kc=kc: e.transpose(m1[:, kc * 128:(kc + 1) * 128], mrg[:, kc * 128:(kc + 1) * 128], ident_bf[:]),
                             reads=[t_mrg, t_ident], writes=[t_m1])
                    c.op("act", lambda e: e.activation(out=mT[:, :, :], in_=m1[:, :].rearrange("p (k t) -> p k t", t=128), func=AF.Copy),
                         reads=[t_m1], writes=[t_mT])
                    yield
                    for cc in range(2):
                        cs = slice(cc * 512, (cc + 1) * 512)
                        for kc in range(8):
                            c.op("pe", lambda e, kc=kc: e.matmul(mo[:, :], lhsT=mT[:, kc, :], rhs=wo[:, kc, cs], start=(kc == 0), stop=(kc == 7)),
                                 reads=[t_mT, t_wo], writes=[t_mo])
                        c.op("dve", lambda e: e.tensor_tensor(out=xt[:, cs], in0=mo[:, :], in1=xt[:, cs], op=ALU.add),
                             reads=[t_mo], wadd=[t_xt])
                        yield
                    c.dma("pool", o_dst[rows, :], xt[:, :], reads=[t_xt])
                    yield

                ucnt = [0]
                pcnt = [0]

                def make_units(i):
                    units = []
                    for br in range(2):
                        for j in range(i + 1):
                            for hf in range(2):
                                units.append((br, j, hf))
                    return units

                def emit_scores(i, u, slot):
                    br, j, hf = u
                    near = j >= i - 1
                    e0 = 128 * (i - j)
                    o = stp[slot][:, :]
                    ts = t_stp[slot]
                    ks = slice(j * 128, (j + 1) * 128)
                    if br == 0:
                        c.op("pe", lambda e: e.matmul(o, lhsT=eall_bf[:, (j // 2) * 128:(j // 2 + 1) * 128],
                                                      rhs=mbT[:, hf * 4:(hf + 1) * 4, :].rearrange("p k t -> p (k t)"),
                                                      start=True, stop=False, skip_group_check=True),
                             reads=[t_eall, t_mbT], writes=[ts])
                        if near:
                            c.op("pe", lambda e: e.matmul(o, lhsT=ident_bf[:, :], rhs=bimg_hi[:, hf * 4:(hf + 1) * 4, e0:e0 + 128],
                                                          start=False, stop=False, skip_group_check=True),
                                 reads=[t_ident, t_bimg], writes=[ts])
                        for hq in range(4):
                            h = hf * 4 + hq
                            c.op("pe", lambda e, h=h, hq=hq: e.matmul(stp[slot][:, hq * 128:(hq + 1) * 128], lhsT=kaT2[:, h // 2, ks],
                                                                      rhs=qpad[:, h // 2, h % 2, :], start=False, stop=(hq == 3),
                                                                      skip_group_check=True),
                                 reads=[t_kaT2, t_qpad], writes=[ts])
                    else:
                        c.op("pe", lambda e: e.matmul(o, lhsT=kbT[:, ks], rhs=qbT[:, hf * 4:(hf + 1) * 4, :].rearrange("p k t -> p (k t)"),
                                                      start=True, stop=False, skip_group_check=True),
                             reads=[t_kbT[j], t_qbT], writes=[ts])
                        if near:
                            c.op("pe", lambda e: e.matmul(o, lhsT=ident_bf[:, :], rhs=bimg_hi[:, 8 + hf * 4:8 + (hf + 1) * 4, e0:e0 + 128],
                                                          start=False, stop=False, skip_group_check=True),
                                 reads=[t_ident, t_bimg], writes=[ts])
                        c.op("pe", lambda e: e.matmul(o, lhsT=mbias[i % 2][:, ks], rhs=irep_bf[:, :], start=False, stop=True,
                                                      skip_group_check=True),
                             reads=[t_mbias[i % 2], t_irep], writes=[ts])

                def emit_exp(slot, ps):
                    c.op("act", lambda e: e.activation(out=pT[ps][:, :], in_=stp[slot][:, :], func=AF.Exp, scale=0.125),
                         reads=[t_stp[slot]], writes=[t_pT[ps]])

                def emit_pv(i, u, slot):
                    br, j, hf = u
                    for hq in range(4):
                        h = hf * 4 + hq
                        rhs = v1a[:, j, h, :] if br == 0 else v1b[:, j, :]
                        tv = t_v1a[j] if br == 0 else t_v1b[j]
                        c.op("pe", lambda e, hq=hq, rhs=rhs: e.matmul(acc[hf][:, hq * 65:hq * 65 + 65], lhsT=pT[slot][:, hq * 128:(hq + 1) * 128],
                                                                      rhs=rhs, start=(j == 0 and hq == 0), stop=(j == i and hq == 3),
                                                                      skip_group_check=True),
                             reads=[t_pT[slot], tv, t_ones], writes=[t_acc[hf]])
                    if j == i:
                        c.op("act", lambda e: e.activation(out=accs[br][:, hf * 260:(hf + 1) * 260], in_=acc[hf][:, 0:260], func=AF.Copy),
                             reads=[t_acc[hf]], wadd=[t_accs[br]])

                def interleave(gens):
                    gens = list(gens)
                    while gens:
                        for g in list(gens):
                            try:
                                next(g)
                                yield
                            except StopIteration:
                                gens.remove(g)

                def bg_schedule(gA, gC, nidx):
                    if gC is not None:
                        for _ in range(2):
                            next(gC)
                            yield
                    if gA is not None:
                        for k in range(nidx):
                            try:
                                next(gA)
                                if k % 4 == 0:
                                    yield
                            except StopIteration:
                                break
                    yield from interleave([g for g in (gA, gC) if g is not None])

                def count_A(i):
                    nk = 128 * (i + 1)
                    return 3 + ((nk + 511) // 512) + (NBIS if i >= 2 else 0)

                for _ in gen_A(0):
                    pass
                prepB(0)
                for i in range(NT):
                    nbg = 0
                    gC = gA = None
                    if i - 1 >= 0:
                        gC = gen_C(i - 1)
                        nbg += 12
                    if i + 1 < NT:
                        gA = gen_A(i + 1)
                        nbg += count_A(i + 1)
                    bg = bg_schedule(gA, gC, 1 + 4 * ((128 * (i + 2) + 511) // 512))
                    units = make_units(i)
                    nU = len(units)
                    quota = -(-nbg // nU) if nU else nbg
                    slots = [(ucnt[0] + k) % 3 for k in range(nU)]
                    ucnt[0] += nU
                    emit_scores(i, units[0], slots[0])
                    for k in range(nU):
                        if k + 1 < nU:
                            emit_scores(i, units[k + 1], slots[k + 1])
                            if k + 2 == nU and i + 1 < NT:
                                prepB(i + 1)
                        emit_exp(slots[k], (pcnt[0] + k) % 2)
                        emit_pv(i, units[k], (pcnt[0] + k) % 2)
                        for _ in range(quota):
                            try:
                                next(bg)
                            except StopIteration:
                                break
                    pcnt[0] += nU
                    for _ in bg:
                        pass
                for _ in gen_C(NT - 1):
                    pass
                c.barrier()
        c.finish()
    return nc


_CACHE = {}


def kernel(x, norm_g, w_in, q_norm_a, k_norm_a, q_norm_b, k_norm_b, w_branch_a, w_branch_b, w_out, rel_bias):
    if "nc" not in _CACHE:
        _CACHE["nc"] = build_program()
    nc = _CACHE["nc"]
    f = lambda a: np.ascontiguousarray(np.asarray(a, dtype=np.float32))
    shared = {"norm_g": f(norm_g), "w_in": f(w_in), "q_norm_a": f(q_norm_a), "k_norm_a": f(k_norm_a),
              "q_norm_b": f(q_norm_b), "k_norm_b": f(k_norm_b), "w_branch_a": f(w_branch_a),
              "w_branch_b": f(w_branch_b), "w_out": f(w_out), "rel_bias": f(rel_bias)}
    shared.update(_host_consts())
    xs = f(x)
    in_maps = []
    for b in range(8):
        m = dict(shared)
        m["x"] = xs[b]
        in_maps.append(m)
    res = run_bass_kernel_spmd(nc, in_maps, core_ids=list(range(8)))
    return np.stack([np.asarray(r["out"]) for r in res.results], axis=0).astype(np.float32)
```

```python
import math
from contextlib import ExitStack

import numpy as np
import concourse.bass as bass
import concourse.mybir as mybir
from concourse.bass_utils import run_bass_kernel_spmd

F32 = mybir.dt.float32
BF16 = mybir.dt.bfloat16
ALU = mybir.AluOpType
AF = mybir.ActivationFunctionType
AX = mybir.AxisListType

S = 4096
D = 1024
NT = S // 128
DIN = 5572
DEPTH = 2
EPS = 1e-6
NEG = -30000.0
QS_W = 4352
NBIS = 18
W0 = 1024.0
TOPK = 256.0
MID0 = 2.0 ** -13
ACT_SHARE_NUM, ACT_SHARE_DEN = 0, 8

C_QA, C_KA, C_VA, C_GA = 0, 512, 1024, 1536
C_QB, C_KB, C_VB, C_GB = 2048, 2560, 2624, 2688
C_QI, C_KI, C_WI, C_MA, C_MB = 3200, 3456, 3520, 3524, 4548


class T:
    __slots__ = ("w", "r")

    def __init__(self):
        self.w = {}
        self.r = {}


class Ctx:
    RING = 8

    def __init__(self, nc, stack):
        self.nc = nc
        self.eng = {"pe": nc.tensor, "act": nc.scalar, "dve": nc.vector, "pool": nc.gpsimd, "sp": nc.sync}
        self.sem = {}
        self.cnt = {}
        for k in ("pe", "act", "dve", "pool"):
            self.sem[k] = stack.enter_context(nc.semaphore("s_" + k))
            self.cnt[k] = 0
        self.waited = {k: {} for k in self.eng}
        self.rings = {}
        self.ringpos = {}
        for q in ("sp", "pool"):
            self.rings[q] = []
            for i in range(self.RING):
                key = "d_%s_%d" % (q, i)
                self.sem[key] = stack.enter_context(nc.semaphore(key))
                self.cnt[key] = 0
                self.rings[q].append(key)
            self.ringpos[q] = 0

    def _wait(self, e, marks):
        need = {}
        for (k, v) in marks:
            if need.get(k, 0) < v:
                need[k] = v
        wd = self.waited[e]
        for k, v in need.items():
            if wd.get(k, 0) < v:
                self.eng[e].wait_ge(self.sem[k], v)
                wd[k] = v

    def _deps(self, e, reads, writes):
        marks = []
        for t in reads:
            for m in t.w.items():
                if m[0] == e and e == "pe":
                    continue
                marks.append(m)
        for t in writes:
            for m in t.w.items():
                if m[0] == e:
                    continue
                marks.append(m)
            for m in t.r.items():
                if m[0] == e:
                    continue
                marks.append(m)
        return marks

    def op(self, e, fn, reads=(), writes=(), wadd=()):
        self._wait(e, self._deps(e, reads, tuple(writes) + tuple(wadd)))
        ins = fn(self.eng[e])
        self.cnt[e] += 1
        ins.then_inc(self.sem[e], 1)
        v = self.cnt[e]
        for t in reads:
            t.r[e] = v
        for t in writes:
            t.w = {e: v}
            t.r = {}
        for t in wadd:
            t.w[e] = v
        return ins

    def dma(self, q, out, in_, reads=(), writes=(), wadd=(), **kw):
        key = self.rings[q][self.ringpos[q] % self.RING]
        self.ringpos[q] += 1
        marks = self._deps("dma", reads, tuple(writes) + tuple(wadd))
        if self.cnt[key] > 0:
            marks.append((key, self.cnt[key]))
        self._wait(q, marks)
        ins = self.eng[q].dma_start(out=out, in_=in_, **kw)
        self.cnt[key] += 16
        ins.then_inc(self.sem[key], 16)
        v = self.cnt[key]
        for t in reads:
            t.r[key] = v
        for t in writes:
            t.w = {key: v}
            t.r = {}
        for t in wadd:
            t.w[key] = v

    def all_marks(self):
        marks = []
        for k, v in self.cnt.items():
            if v > 0:
                marks.append((k, v))
        return marks

    def barrier(self):
        marks = self.all_marks()
        for e in ("pe", "act", "dve", "pool", "sp"):
            self._wait(e, [m for m in marks if m[0] != e])

    def finish(self):
        self._wait("sp", self.all_marks())


def _bucket_table():
    n = np.arange(0, 256)
    max_exact = 16
    nf = np.maximum(n, 1).astype(np.float32)
    large = max_exact + (np.log(nf / np.float32(max_exact)) / np.float32(math.log(128 / max_exact))
                         * np.float32(32 - max_exact)).astype(np.int32)
    large = np.minimum(large, 31)
    return np.where(n < max_exact, n, large)


def _host_consts():
    bk = _bucket_table()
    grev = np.zeros((33, 384), np.float32)
    for m in range(384):
        dist = 255 - m
        if dist >= 0:
            grev[bk[dist], m] = 1.0
        else:
            grev[32, m] = 1.0
    ident = np.eye(128, dtype=np.float32)
    irep = np.concatenate([ident] * 4, axis=1)
    t = np.arange(128)[:, None]
    s = np.arange(128)[None, :]
    causalneg = np.where(s <= t, 0.0, -1e30).astype(np.float32)
    eall = np.zeros((16, 16, 128), np.float32)
    for n in range(16):
        eall[n, n, :] = 1.0
    negrow = np.full((1, 16), NEG * 8.0, np.float32)
    return {"c_grev": grev, "c_ident": ident, "c_irep": irep, "c_causal": causalneg,
            "c_eall": eall.reshape(16, 2048), "c_negrow": negrow}


def build_program():
    nc = bass.Bass("TRN2", target_bir_lowering=False)
    din = lambda n, s, d=F32: nc.dram_tensor(n, s, d, kind="ExternalInput").ap()
    x = din("x", [S, D])
    norm_g = din("norm_g", [DEPTH, D])
    w_in = din("w_in", [DEPTH, D, DIN])
    qn_a = din("q_norm_a", [DEPTH, 64])
    kn_a = din("k_norm_a", [DEPTH, 64])
    qn_b = din("q_norm_b", [DEPTH, 64])
    kn_b = din("k_norm_b", [DEPTH, 64])
    w_bra = din("w_branch_a", [DEPTH, 512, D])
    w_brb = din("w_branch_b", [DEPTH, 512, D])
    w_out = din("w_out", [DEPTH, D, D])
    rel_bias = din("rel_bias", [32, 16])
    c_grev = din("c_grev", [33, 384])
    c_ident = din("c_ident", [128, 128])
    c_irep = din("c_irep", [128, 512])
    c_causal = din("c_causal", [128, 128])
    c_eall = din("c_eall", [16, 2048])
    c_negrow = din("c_negrow", [1, 16])
    out = nc.dram_tensor("out", [S, D], F32, kind="ExternalOutput").ap()
    scr = lambda n, s, d: nc.dram_tensor(n, s, d, kind="Internal").ap()
    qs = scr("qs", [S, QS_W], BF16)
    wis = scr("wis", [S, 4], F32)
    kaT_s = scr("kaT_s", [4, 128, S], BF16)
    x1 = scr("x1", [S, D], F32)

    top = ExitStack()
    with top:
        c = Ctx(nc, top)

        uid = [0]

        def sbt(st, name, shape, dt):
            uid[0] += 1
            return st.enter_context(nc.sbuf_tensor("%s_%d" % (name, uid[0]), shape, dt))

        def pst(st, name, shape, dt):
            uid[0] += 1
            return st.enter_context(nc.psum_tensor("%s_%d" % (name, uid[0]), shape, dt))

        ident_bf = sbt(top, "ident_bf", [128, 128], BF16); t_ident = T()
        irep_bf = sbt(top, "irep_bf", [128, 512], BF16); t_irep = T()
        causal = sbt(top, "causal", [128, 128], F32); t_causal = T()
        eall_bf = sbt(top, "eall_bf", [128, 2048], BF16); t_eall = T()
        bimg_hi = sbt(top, "bimg_hi", [128, 16, 256], BF16)
        t_bimg = T()
        v1a = sbt(top, "v1a", [128, NT, 8, 65], BF16); t_v1a = [T() for _ in range(NT)]
        v1b = sbt(top, "v1b", [128, NT, 65], BF16); t_v1b = [T() for _ in range(NT)]
        kbT = sbt(top, "kbT", [128, S], BF16); t_kbT = [T() for _ in range(NT)]
        kiT = sbt(top, "kiT", [128, S], BF16); t_kiT = [T() for _ in range(NT)]
        kmacc = sbt(top, "kmacc", [128, 4, 16], F32); t_kmacc = T()

        c.dma("pool", ident_bf[:], c_ident, writes=[t_ident])
        c.dma("pool", irep_bf[:], c_irep, writes=[t_irep])
        c.op("dve", lambda e: e.memset(eall_bf[:, :], 0.0), writes=[t_eall])
        c.dma("pool", eall_bf[0:16, :], c_eall, writes=[t_eall])
        t_kz = T()
        c.op("dve", lambda e: e.memset(kbT[64:128, :], 0.0), writes=[t_kz])
        c.op("dve", lambda e: e.memset(kiT[64:128, :], 0.0), writes=[t_kz])
        c.dma("sp", causal[:], c_causal, writes=[t_causal])
        t_ones = T()
        c.op("dve", lambda e: e.memset(v1a[:, :, :, 64:65], 1.0), writes=[t_ones])
        c.op("dve", lambda e: e.memset(v1b[:, :, 64:65], 1.0), writes=[t_ones])

        with ExitStack() as st:
            grev = sbt(st, "grev", [33, 384], F32); t_grev = T()
            reltab = sbt(st, "reltab", [33, 16], F32); t_rel = T()
            r31 = sbt(st, "r31", [32, 16], F32); t_r31 = T()
            bps = pst(st, "bps", [128, 2048], F32); t_bps = T()
            c.dma("sp", grev[:], c_grev, writes=[t_grev])
            c.dma("sp", reltab[0:32, :], rel_bias, writes=[t_rel])
            c.dma("sp", r31[:], rel_bias[31:32, :].to_broadcast([32, 16]), writes=[t_r31])
            c.op("dve", lambda e: e.tensor_tensor(out=reltab[0:32, :], in0=reltab[0:32, :], in1=r31[:], op=ALU.subtract),
                 reads=[t_r31, t_rel], writes=[t_rel])
            c.op("dve", lambda e: e.tensor_scalar(out=reltab[0:32, :], in0=reltab[0:32, :], scalar1=8.0, scalar2=None, op0=ALU.mult),
                 reads=[t_rel], writes=[t_rel])
            c.dma("sp", reltab[32:33, :], c_negrow, reads=[t_rel], writes=[t_rel])
            for half in range(2):
                for el in range(128):
                    e_ = half * 128 + el
                    c.op("pe", lambda e, el=el, e_=e_: e.matmul(bps[:, el * 16:(el + 1) * 16], lhsT=grev[0:33, 255 - e_:255 - e_ + 128],
                                                                 rhs=reltab[0:33, :], start=True, stop=True),
                         reads=[t_grev, t_rel], writes=[t_bps])
                src = bps[:, :].rearrange("p (e h) -> p h e", h=16)
                hi_v = bimg_hi[:, :, half * 128:(half + 1) * 128]
                c.op("dve", lambda e: e.tensor_copy(out=hi_v, in_=src), reads=[t_bps], wadd=[t_bimg])
            c.barrier()

        for l in range(DEPTH):
            x_src = x if l == 0 else x1
            o_dst = x1 if l == 0 else out
            with ExitStack() as st:
                w_bf = sbt(st, "w_bf", [128, 8, DIN], BF16)
                t_wc = {(c0, kc): T() for c0 in (0, 2048, 4096) for kc in range(8)}
                gbc = sbt(st, "gbc", [128, D], F32); t_gbc = T()
                gq_a = sbt(st, "gq_a", [128, 64], F32)
                gk_a = sbt(st, "gk_a", [128, 64], F32)
                gq_b = sbt(st, "gq_b", [128, 64], F32)
                gk_b = sbt(st, "gk_b", [128, 64], F32)
                t_gs = T()
                xb = [sbt(st, "xb%d" % i, [128, D], F32) for i in range(2)]; t_xb = [T(), T()]
                sqj = sbt(st, "sqj", [128, D], BF16); t_sqj = T()
                stat = sbt(st, "stat", [128, 8], F32); t_stat = T()
                hb = sbt(st, "hb", [128, D], BF16); t_hb = T()
                hT = [sbt(st, "hT%d" % i, [128, 8, 128], BF16) for i in range(2)]; t_hT = [T(), T()]
                qt = [sbt(st, "qt%d" % i, [128, QS_W], BF16) for i in range(2)]; t_qt = [T(), T()]
                sqf = sbt(st, "sqf", [128, 512], F32); t_sqf = T()
                hst = sbt(st, "hst", [128, 32], F32); t_hst = T()
                tmpf = sbt(st, "tmpf", [128, 512], F32); t_tmpf = T()
                kan = sbt(st, "kan", [128, 512], BF16); t_kan = T()
                kTt = [sbt(st, "kTt%d" % i, [128, 4, 128], BF16) for i in range(2)]; t_kTt = [T(), T()]
                kred = sbt(st, "kred", [128, 4], F32); t_kred = T()
                sm = sbt(st, "sm", [128, 128], BF16); t_sm = T()
                wit = [sbt(st, "witp%d" % i, [128, 4], F32) for i in range(2)]; t_wit = [T(), T()]
                tp = pst(st, "tp", [128, 1024], BF16); t_tp = T()
                tp2 = pst(st, "tp2", [128, 1024], BF16); t_tp2 = T()
                pj = [pst(st, "pj%d" % i, [128, 512], F32) for i in range(6)]; t_pj = [T() for _ in range(6)]

                for (c0, c1) in ((0, 2048), (2048, 4096), (4096, DIN)):
                    for kc in range(8):
                        c.dma("pool", w_bf[:, kc, c0:c1], w_in[l, kc * 128:(kc + 1) * 128, c0:c1], wadd=[t_wc[(c0, kc)]])
                c.dma("sp", gbc[:], norm_g[l:l + 1, :].to_broadcast([128, D]), writes=[t_gbc])
                for (g_sb, g_dr) in ((gq_a, qn_a), (gk_a, kn_a), (gq_b, qn_b), (gk_b, kn_b)):
                    c.dma("sp", g_sb[:], g_dr[l:l + 1, :].to_broadcast([128, 64]), wadd=[t_gs])
                c.op("dve", lambda e: e.memset(kmacc[:], 0.0), writes=[t_kmacc])

                pjn = [0]

                def project(i, col0, ncols):
                    b = pjn[0] % 6
                    pjn[0] += 1
                    for kc in range(8):
                        c.op("pe", lambda e, kc=kc: e.matmul(pj[b][:, 0:ncols], lhsT=hT[i % 2][:, kc, :],
                                                              rhs=w_bf[:, kc, col0:col0 + ncols],
                                                              start=(kc == 0), stop=(kc == 7)),
                             reads=[t_hT[i % 2]] + [t_wc[(cc, kc)] for cc in (0, 2048, 4096)
                                                      if cc < col0 + ncols and col0 < min(cc + 2048, DIN)],
                             writes=[t_pj[b]])
                    return pj[b], t_pj[b]

                def rsqrt_small(ap_in, ap_out, n, inv_n):
                    c.op("dve", lambda e: e.tensor_scalar(out=ap_out, in0=ap_in, scalar1=inv_n, scalar2=EPS,
                                                          op0=ALU.mult, op1=ALU.add), reads=[t_hst], writes=[t_hst])
                    c.op("act", lambda e: e.activation(out=ap_out, in_=ap_out, func=AF.Ln), reads=[t_hst], writes=[t_hst])
                    c.op("act", lambda e: e.activation(out=ap_out, in_=ap_out, func=AF.Exp, scale=-0.5),
                         reads=[t_hst], writes=[t_hst])

                def headnorm(ps_ap, t_ps, H, g_sb, out_ap, t_out, wadd_out=False):
                    W = H * 64
                    c.op("act", lambda e: e.activation(out=sqf[:, 0:W], in_=ps_ap, func=AF.Square),
                         reads=[t_ps], writes=[t_sqf])
                    c.op("dve", lambda e: e.tensor_reduce(out=hst[:, 0:H], in_=sqf[:, 0:W].rearrange("p (h d) -> p h d", d=64),
                                                          axis=AX.X, op=ALU.add), reads=[t_sqf], writes=[t_hst])
                    rsqrt_small(hst[:, 0:H], hst[:, 0:H], H, 1.0 / 64.0)
                    c.op("dve", lambda e: e.tensor_tensor(out=tmpf[:, 0:W].rearrange("p (h d) -> p h d", d=64),
                                                          in0=ps_ap.rearrange("p (h d) -> p h d", d=64),
                                                          in1=hst[:, 0:H].unsqueeze(2).to_broadcast([128, H, 64]), op=ALU.mult),
                         reads=[t_ps, t_hst], writes=[t_tmpf])
                    kw = dict(wadd=[t_out]) if wadd_out else dict(writes=[t_out])
                    c.op("dve", lambda e: e.tensor_tensor(out=out_ap.rearrange("p (h d) -> p h d", d=64),
                                                          in0=tmpf[:, 0:W].rearrange("p (h d) -> p h d", d=64),
                                                          in1=g_sb[:, :].unsqueeze(1).to_broadcast([128, H, 64]), op=ALU.mult),
                         reads=[t_tmpf, t_gs], **kw)

                def norm_act(i):
                    rows = slice(i * 128, (i + 1) * 128)
                    xt, t_xt = xb[i % 2], t_xb[i % 2]
                    c.dma("sp", xt[:], x_src[rows, :], writes=[t_xt])
                    c.op("act", lambda e: e.activation(out=sqj[:], in_=xt[:], func=AF.Square, accum_out=stat[:, 0:1]),
                         reads=[t_xt], writes=[t_sqj, t_stat])
                    c.op("dve", lambda e: e.tensor_scalar(out=stat[:, 1:2], in0=stat[:, 0:1], scalar1=1.0 / D, scalar2=EPS,
                                                          op0=ALU.mult, op1=ALU.add), reads=[t_stat], writes=[t_stat])
                    c.op("act", lambda e: e.activation(out=stat[:, 2:3], in_=stat[:, 1:2], func=AF.Ln), reads=[t_stat], writes=[t_stat])
                    c.op("act", lambda e: e.activation(out=stat[:, 3:4], in_=stat[:, 2:3], func=AF.Exp, scale=-0.5),
                         reads=[t_stat], writes=[t_stat])
                    c.op("dve", lambda e: e.scalar_tensor_tensor(out=hb[:], in0=xt[:], scalar=stat[:, 3:4], in1=gbc[:],
                                                                 op0=ALU.mult, op1=ALU.mult),
                         reads=[t_xt, t_stat, t_gbc], writes=[t_hb])

                def norm_pe(i):
                    for kc in range(8):
                        c.op("pe", lambda e, kc=kc: e.transpose(tp[:, kc * 128:(kc + 1) * 128], hb[:, kc * 128:(kc + 1) * 128], ident_bf[:]),
                             reads=[t_hb, t_ident], writes=[t_tp])
                    c.op("act", lambda e: e.activation(out=hT[i % 2][:, :, :], in_=tp[:, :].rearrange("p (k t) -> p k t", t=128), func=AF.Copy),
                         reads=[t_tp], writes=[t_hT[i % 2]])

                norm_act(0)
                norm_pe(0)
                for i in range(NT):
                    rows = slice(i * 128, (i + 1) * 128)
                    q, t_q = qt[i % 2], t_qt[i % 2]
                    kt, t_kt = kTt[i % 2], t_kTt[i % 2]
                    if i + 1 < NT:
                        norm_act(i + 1)
                    ps, tps = project(i, C_QA, 512)
                    headnorm(ps[:, 0:512], tps, 8, gq_a, q[:, 0:512], t_q)
                    ps, tps = project(i, C_KA, 512)
                    headnorm(ps[:, 0:512], tps, 8, gk_a, kan[:, :], t_kan)
                    ps, tps = project(i, C_QB, 512)
                    headnorm(ps[:, 0:512], tps, 8, gq_b, q[:, 512:1024], t_q, wadd_out=True)
                    ps, tps = project(i, C_KB, 128)
                    headnorm(ps[:, 0:64], tps, 1, gk_b, sm[:, 0:64], t_sm)
                    c.op("dve", lambda e: e.tensor_copy(out=v1b[:, i, 0:64], in_=ps[:, 64:128]), reads=[tps], writes=[t_v1b[i]])
                    if i + 1 < NT:
                        norm_pe(i + 1)
                    ps2, tps2 = project(i, C_QI, 324)
                    c.op("dve", lambda e: e.tensor_copy(out=q[:, 4096:4352], in_=ps2[:, 0:256]), reads=[tps2], wadd=[t_q])
                    c.op("dve", lambda e: e.tensor_copy(out=sm[:, 64:128], in_=ps2[:, 256:320]), reads=[tps2], wadd=[t_sm])
                    c.op("dve", lambda e: e.tensor_copy(out=wit[i % 2][:, :], in_=ps2[:, 320:324]), reads=[tps2], writes=[t_wit[i % 2]])
                    c.dma("pool", wis[rows, :], wit[i % 2][:, :], reads=[t_wit[i % 2]])
                    for p4 in range(4):
                        c.op("pe", lambda e, p4=p4: e.transpose(tp2[:, p4 * 128:(p4 + 1) * 128], kan[:, p4 * 128:(p4 + 1) * 128], ident_bf[:]),
                             reads=[t_kan, t_ident], writes=[t_tp2])
                    c.op("act", lambda e: e.activation(out=kt[:, :, :], in_=tp2[:, 0:512].rearrange("p (k t) -> p k t", t=128), func=AF.Copy),
                         reads=[t_tp2], writes=[t_kt])
                    for p4 in range(4):
                        c.dma("pool", kaT_s[p4, :, i * 128:(i + 1) * 128], kt[:, p4, :], reads=[t_kt])
                    c.op("dve", lambda e: e.tensor_reduce(out=kred[:, :], in_=kt[:, :, :], axis=AX.X, op=ALU.add),
                         reads=[t_kt], writes=[t_kred])
                    nblk = i // 2
                    c.op("dve", lambda e: e.tensor_tensor(out=kmacc[:, :, nblk], in0=kmacc[:, :, nblk], in1=kred[:, :], op=ALU.add),
                         reads=[t_kred, t_kmacc], writes=[t_kmacc])
                    ps, tps = project(i, C_VA, 512)
                    c.op("act", lambda e: e.activation(out=v1a[:, i, :, 0:64], in_=ps[:, 0:512].rearrange("p (h d) -> p h d", d=64), func=AF.Copy),
                         reads=[tps], writes=[t_v1a[i]])
                    c.op("pe", lambda e: e.transpose(tp2[0:64, 512:640], sm[:, 0:64], ident_bf[:]), reads=[t_sm, t_ident], writes=[t_tp2])
                    c.op("pe", lambda e: e.transpose(tp2[0:64, 640:768], sm[:, 64:128], ident_bf[:]), reads=[t_sm, t_ident], writes=[t_tp2])
                    c.op("act", lambda e: e.activation(out=kbT[0:64, i * 128:(i + 1) * 128], in_=tp2[0:64, 512:640], func=AF.Copy),
                         reads=[t_tp2], writes=[t_kbT[i]])
                    c.op("act", lambda e: e.activation(out=kiT[0:64, i * 128:(i + 1) * 128], in_=tp2[0:64, 640:768], func=AF.Copy),
                         reads=[t_tp2], writes=[t_kiT[i]])
                    ps, tps = project(i, C_GA, 512)
                    c.op("act", lambda e: e.activation(out=q[:, 1024:1536], in_=ps[:, 0:512], func=AF.Silu), reads=[tps], wadd=[t_q])
                    ps, tps = project(i, C_GB, 512)
                    c.op("act", lambda e: e.activation(out=q[:, 1536:2048], in_=ps[:, 0:512], func=AF.Silu), reads=[tps], wadd=[t_q])
                    for gi, cm in enumerate((C_MA, C_MA + 512, C_MB, C_MB + 512)):
                        ps, tps = project(i, cm, 512)
                        c.op("act", lambda e, gi=gi: e.activation(out=q[:, 2048 + gi * 512:2048 + (gi + 1) * 512], in_=ps[:, 0:512], func=AF.Sigmoid),
                             reads=[tps], wadd=[t_q])
                    c.dma("pool", qs[rows, :], q[:, :], reads=[t_q])
                c.barrier()

            with ExitStack() as st:
                kaT2 = sbt(st, "kaT2", [128, 4, S], BF16); t_kaT2 = T()
                km_bf = sbt(st, "km_bf", [128, 4, 16], BF16); t_km = T()
                wbra = sbt(st, "wbra", [128, 4, D], BF16); t_wbra = T()
                wbrb = sbt(st, "wbrb", [128, 4, D], BF16); t_wbrb = T()
                wo = sbt(st, "wo", [128, 8, D], BF16); t_wo = T()
                yg2 = sbt(st, "yg2", [128, 1024], BF16); t_yg2 = T()
                qm = yg2; t_qm = t_yg2
                qmi = sbt(st, "qmi", [128, 256], BF16); t_qmi = T()
                gt = sbt(st, "gt", [128, 3072], BF16); t_gt = T()
                wit2 = [sbt(st, "wit2%d" % k, [128, 4], F32) for k in range(2)]; t_wit2 = [T(), T()]
                xt = sbt(st, "xt", [128, 512], F32); t_xt = T()
                qpad = sbt(st, "qpad", [128, 4, 2, 128], BF16); t_qpad = T()
                qbT = sbt(st, "qbT", [128, 8, 128], BF16); t_qbT = T()
                qiT = [sbt(st, "qiT%d" % k, [128, 4, 128], BF16) for k in range(2)]; t_qiT = [T(), T()]
                score2 = [sbt(st, "score%d" % k, [128, S], F32) for k in range(2)]; t_score2 = [T(), T()]
                mbias1 = sbt(st, "mbias", [128, S], BF16); t_mbias1 = T()
                jk = sbt(st, "jk", [128, 2], BF16); t_jk = T()
                rl2 = [sbt(st, "rl%d" % k, [128, 512], F32) for k in range(2)]; t_rl2 = [T(), T()]
                bs = sbt(st, "bs", [128, 16], F32)
                t_cnt = [T(), T()]; t_tmp = [T(), T()]; t_mid = [T(), T()]; t_thr = [T(), T()]
                sg = sbt(st, "sg", [128, 2], F32); t_sg = T()
                gatem = sbt(st, "gatem", [128, 8, 16], F32); t_gatem = T()
                top8 = sbt(st, "top8", [128, 8, 8], F32); t_top8 = T()
                thr3 = sbt(st, "thr3", [128, 8], F32); t_thr3 = T()
                mbf = sbt(st, "mbf", [128, 8, 16], F32); t_mbf = T()
                mb16 = sbt(st, "mb16", [128, 8, 16], BF16); t_mb16 = T()
                mbT = sbt(st, "mbT", [128, 8, 128], BF16); t_mbT = T()
                pT = [sbt(st, "pT%d" % k, [128, 512], BF16) for k in range(2)]; t_pT = [T(), T()]
                accs = [sbt(st, "accs%d" % k, [128, 520], F32) for k in range(2)]; t_accs = [T(), T()]
                rec = sbt(st, "rec", [128, 8], F32); t_rec = T()
                ytmp = sbt(st, "ytmp", [128, 512], F32); t_ytmp = T()
                yag = yg2[:, 0:512]; t_yag = t_yg2
                ybg = yg2[:, 512:1024]; t_ybg = t_yg2
                yT2 = sbt(st, "yT2", [128, 8, 128], BF16); t_yT2 = T()
                yaT = yT2[:, 0:4, :]; t_yaT = t_yT2
                ybT = yT2[:, 4:8, :]; t_ybT = t_yT2
                mtmp2 = rl2[1]; t_mtmp2 = t_rl2[1]
                mrg = yg2; t_mrg = t_yg2
                mT = yT2; t_mT = t_yT2
                stp = [pst(st, "stp%d" % k, [128, 512], F32) for k in range(3)]; t_stp = [T(), T(), T()]
                acc = [pst(st, "acc%d" % k, [128, 512], F32) for k in range(2)]; t_acc = [T(), T()]
                m0 = pst(st, "m0", [128, 512], F32); t_m0 = T()
                m1 = pst(st, "m1", [128, 1024], BF16); t_m1 = T()
                mo = pst(st, "mo", [128, 512], F32); t_mo = T()

                for p4 in range(4):
                    c.dma("sp", kaT2[:, p4, :], kaT_s[p4, :, :], wadd=[t_kaT2])
                for kc in range(4):
                    c.dma("pool", wbra[:, kc, :], w_bra[l, kc * 128:(kc + 1) * 128, :], wadd=[t_wbra])
                    c.dma("pool", wbrb[:, kc, :], w_brb[l, kc * 128:(kc + 1) * 128, :], wadd=[t_wbrb])
                for kc in range(8):
                    c.dma("pool", wo[:, kc, :], w_out[l, kc * 128:(kc + 1) * 128, :], wadd=[t_wo])
                c.op("dve", lambda e: e.tensor_scalar(out=km_bf[:, :, :], in0=kmacc[:, :, :], scalar1=1.0 / 256.0, scalar2=None, op0=ALU.mult),
                     reads=[t_kmacc], writes=[t_km])
                c.op("dve", lambda e: e.memset(qpad[:, :, :, :], 0.0), writes=[t_qpad])
                c.op("dve", lambda e: e.memset(qbT[:, :, :], 0.0), writes=[t_qbT])
                c.op("dve", lambda e: e.memset(mbT[:, :, :], 0.0), writes=[t_mbT])
                for k in range(2):
                    c.op("dve", lambda e, k=k: e.memset(qiT[k][:, :, :], 0.0), writes=[t_qiT[k]])

                def rows_of(i):
                    return slice(i * 128, (i + 1) * 128)

                idxn = [0]

                def gen_I(i):
                    rows = rows_of(i)
                    nk = 128 * (i + 1)
                    b = i % 2
                    score, t_score = score2[b], t_score2[b]
                    c.dma("sp", qmi[:, :], qs[rows, 4096:4352], writes=[t_qmi])
                    c.dma("sp", wit2[b][:, :], wis[rows, :], writes=[t_wit2[b]])
                    for h in range(4):
                        c.op("pe", lambda e, h=h: e.transpose(m1[0:64, h * 128:(h + 1) * 128], qmi[:, h * 64:(h + 1) * 64], ident_bf[:]),
                             reads=[t_qmi, t_ident], writes=[t_m1])
                    c.op("act", lambda e: e.activation(out=qiT[b][0:64, :, :], in_=m1[0:64, 0:512].rearrange("p (k t) -> p k t", t=128), func=AF.Copy),
                         reads=[t_m1], wadd=[t_qiT[b]])
                    yield
                    for c0 in range(0, nk, 512):
                        n = min(512, nk - c0)
                        kdeps = [t_kiT[jj] for jj in range(c0 // 128, (c0 + n) // 128)]
                        for hh in range(4):
                            ib = idxn[0] % 2
                            idxn[0] += 1
                            pm, t_pm = (m0, t_m0) if ib == 0 else (mo, t_mo)
                            rl, t_rl = rl2[ib], t_rl2[ib]
                            c.op("pe", lambda e, hh=hh, pm=pm: e.matmul(pm[:, 0:n], lhsT=qiT[b][:, hh, :], rhs=kiT[:, c0:c0 + n], start=True, stop=True),
                                 reads=[t_qiT[b]] + kdeps, writes=[t_pm])
                            c.op("act", lambda e, pm=pm, rl=rl: e.activation(out=rl[:, 0:n], in_=pm[:, 0:n], func=AF.Relu),
                                 reads=[t_pm], writes=[t_rl])
                            if hh == 0:
                                c.op("dve", lambda e, rl=rl: e.tensor_scalar(out=score[:, c0:c0 + n], in0=rl[:, 0:n], scalar1=wit2[b][:, 0:1],
                                                                             scalar2=None, op0=ALU.mult),
                                     reads=[t_rl, t_wit2[b]], writes=[t_score])
                            else:
                                c.op("dve", lambda e, hh=hh, rl=rl: e.scalar_tensor_tensor(out=score[:, c0:c0 + n], in0=rl[:, 0:n],
                                                                                          scalar=wit2[b][:, hh:hh + 1], in1=score[:, c0:c0 + n],
                                                                                          op0=ALU.mult, op1=ALU.add),
                                     reads=[t_rl, t_wit2[b], t_score], writes=[t_score])
                            yield
                    c.op("dve", lambda e: e.tensor_tensor(out=score[:, i * 128:(i + 1) * 128], in0=score[:, i * 128:(i + 1) * 128],
                                                          in1=causal[:, :], op=ALU.add),
                         reads=[t_score, t_causal], writes=[t_score])
                    yield

                def gen_B(i):
                    nk = 128 * (i + 1)
                    b = i % 2
                    o = 8 * b
                    score, t_score = score2[b], t_score2[b]
                    if i >= 2:
                        c.op("dve", lambda e: e.memset(bs[:, o + 2:o + 3], MID0), writes=[t_mid[b]])
                        w = W0
                        for k in range(NBIS):
                            c.op("dve", lambda e: e.tensor_scalar(out=jk[:, 0:1].to_broadcast([128, nk]), in0=score[:, 0:nk],
                                                                  scalar1=bs[:, o + 2:o + 3], scalar2=None,
                                                                  op0=ALU.is_ge, op1=ALU.add, accum_out=bs[:, o:o + 1]),
                                 reads=[t_score, t_mid[b]], writes=[t_cnt[b], t_jk])
                            c.op("dve", lambda e: e.tensor_scalar(out=bs[:, o + 1:o + 2], in0=bs[:, o:o + 1], scalar1=TOPK, scalar2=0.5,
                                                                  op0=ALU.is_ge, op1=ALU.subtract), reads=[t_cnt[b]], writes=[t_tmp[b]])
                            c.op("dve", lambda e, w=w: e.scalar_tensor_tensor(out=bs[:, o + 2:o + 3], in0=bs[:, o + 1:o + 2], scalar=w,
                                                                              in1=bs[:, o + 2:o + 3], op0=ALU.mult, op1=ALU.add),
                                 reads=[t_tmp[b], t_mid[b]], writes=[t_mid[b]])
                            w = w / 2.0
                            yield
                        c.op("dve", lambda e, w=w: e.tensor_scalar(out=bs[:, o + 3:o + 4], in0=bs[:, o + 2:o + 3], scalar1=-w, scalar2=None, op0=ALU.add),
                             reads=[t_mid[b]], writes=[t_thr[b]])
                    else:
                        c.op("dve", lambda e: e.memset(bs[:, o + 3:o + 4], -1e29), writes=[t_thr[b]])
                    yield

                def gen_F(i):
                    nk = 128 * (i + 1)
                    b = i % 2
                    o = 8 * b
                    c.op("dve", lambda e: e.tensor_scalar(out=mbias1[:, 0:nk], in0=score2[b][:, 0:nk], scalar1=bs[:, o + 3:o + 4], scalar2=NEG,
                                                          op0=ALU.is_lt, op1=ALU.mult), reads=[t_score2[b], t_thr[b]], writes=[t_mbias1])
                    yield

                def prepB(i):
                    rows = rows_of(i)
                    qblk = i // 2
                    c.dma("sp", qm[:, :], qs[rows, 0:1024], writes=[t_qm])
                    for p4 in range(4):
                        c.op("pe", lambda e, p4=p4: e.transpose(m1[:, p4 * 128:(p4 + 1) * 128], qm[:, p4 * 128:(p4 + 1) * 128], ident_bf[:]),
                             reads=[t_qm, t_ident], writes=[t_m1])
                    c.op("act", lambda e: e.activation(out=qpad[0:64, :, 0, :], in_=m1[0:64, 0:512].rearrange("p (k t) -> p k t", t=128), func=AF.Copy),
                         reads=[t_m1], wadd=[t_qpad])
                    c.op("act", lambda e: e.activation(out=qpad[64:128, :, 1, :], in_=m1[64:128, 0:512].rearrange("p (k t) -> p k t", t=128), func=AF.Copy),
                         reads=[t_m1], wadd=[t_qpad])
                    for h in range(8):
                        c.op("pe", lambda e, h=h: e.transpose(m1[0:64, h * 128:(h + 1) * 128], qm[:, 512 + h * 64:512 + (h + 1) * 64], ident_bf[:]),
                             reads=[t_qm, t_ident], writes=[t_m1])
                    c.op("act", lambda e: e.activation(out=qbT[0:64, :, :], in_=m1[0:64, :].rearrange("p (k t) -> p k t", t=128), func=AF.Copy),
                         reads=[t_m1], wadd=[t_qbT])
                    for h in range(8):
                        c.op("pe", lambda e, h=h: e.matmul(m0[:, h * 16:(h + 1) * 16], lhsT=qpad[:, h // 2, h % 2, :], rhs=km_bf[:, h // 2, :],
                                                           start=True, stop=True),
                             reads=[t_qpad, t_km], writes=[t_m0])
                    c.op("dve", lambda e: e.tensor_copy(out=gatem[:, :, :], in_=m0[:, 0:128].rearrange("p (h n) -> p h n", n=16)),
                         reads=[t_m0], writes=[t_gatem])
                    c.op("dve", lambda e: e.memset(gatem[:, :, qblk:16], -1e30), reads=[t_gatem], writes=[t_gatem])
                    for h in range(8):
                        c.op("dve", lambda e, h=h: e.max(out=top8[:, h, :], in_=gatem[:, h, :]), reads=[t_gatem], wadd=[t_top8])
                    c.op("dve", lambda e: e.tensor_scalar(out=thr3[:, :].unsqueeze(2), in0=top8[:, :, 2:3], scalar1=-1e29, scalar2=None, op0=ALU.max),
                         reads=[t_top8], writes=[t_thr3])
                    c.op("dve", lambda e: e.tensor_tensor(out=mbf[:, :, :], in0=gatem[:, :, :],
                                                          in1=thr3[:, :].unsqueeze(2).to_broadcast([128, 8, 16]), op=ALU.is_lt),
                         reads=[t_gatem, t_thr3, t_top8], writes=[t_mbf])
                    c.op("dve", lambda e: e.tensor_scalar(out=mb16[:, :, :], in0=mbf[:, :, :], scalar1=NEG, scalar2=None, op0=ALU.mult),
                         reads=[t_mbf], writes=[t_mb16])
                    c.op("dve", lambda e: e.memset(mb16[:, :, qblk:qblk + 1], 0.0), reads=[t_mb16], writes=[t_mb16])
                    for h in range(8):
                        c.op("pe", lambda e, h=h: e.transpose(m1[0:16, h * 128:(h + 1) * 128], mb16[:, h, :], ident_bf[:]),
                             reads=[t_mb16, t_ident], writes=[t_m1])
                    c.op("act", lambda e: e.activation(out=mbT[0:16, :, :], in_=m1[0:16, :].rearrange("p (k t) -> p k t", t=128), func=AF.Copy),
                         reads=[t_m1], wadd=[t_mbT])

                def gen_C(i):
                    rows = rows_of(i)
                    c.dma("sp", gt[:, :], qs[rows, 1024:4096], writes=[t_gt])
                    for (br, g0, y_out, t_y) in ((0, 0, yag, t_yag), (1, 512, ybg, t_ybg)):
                        av = accs[br][:, :].rearrange("p (h d) -> p h d", d=65)
                        c.op("dve", lambda e: e.reciprocal(out=rec[:, :].unsqueeze(2), in_=av[:, :, 64:65]),
                             reads=[t_accs[br]], writes=[t_rec])
                        c.op("dve", lambda e: e.tensor_tensor(out=ytmp[:, :].rearrange("p (h d) -> p h d", d=64), in0=av[:, :, 0:64],
                                                              in1=rec[:, :].unsqueeze(2).to_broadcast([128, 8, 64]), op=ALU.mult),
                             reads=[t_accs[br], t_rec], writes=[t_ytmp])
                        c.op("dve", lambda e: e.tensor_tensor(out=y_out[:, :], in0=ytmp[:, :], in1=gt[:, g0:g0 + 512], op=ALU.mult),
                             reads=[t_ytmp, t_gt], writes=[t_y])
                        yield
                    for (ysrc, t_ys, yT, t_yT) in ((yag, t_yag, yaT, t_yaT), (ybg, t_ybg, ybT, t_ybT)):
                        for kc in range(4):
                            c.op("pe", lambda e, kc=kc, ysrc=ysrc: e.transpose(m1[:, kc * 128:(kc + 1) * 128], ysrc[:, kc * 128:(kc + 1) * 128], ident_bf[:]),
                                 reads=[t_ys, t_ident], writes=[t_m1])
                        c.op("act", lambda e, yT=yT: e.activation(out=yT[:, :, :], in_=m1[:, 0:512].rearrange("p (k t) -> p k t", t=128), func=AF.Copy),
                             reads=[t_m1], writes=[t_yT])
                        yield
                    for cc in range(2):
                        cs = slice(cc * 512, (cc + 1) * 512)
                        for kc in range(4):
                            c.op("pe", lambda e, kc=kc: e.matmul(mo[:, :], lhsT=yaT[:, kc, :], rhs=wbra[:, kc, cs], start=(kc == 0), stop=(kc == 3)),
                                 reads=[t_yaT, t_wbra], writes=[t_mo])
                        c.op("dve", lambda e: e.tensor_tensor(out=ytmp[:, :], in0=mo[:, :], in1=gt[:, 1024 + cc * 512:1024 + (cc + 1) * 512], op=ALU.mult),
                             reads=[t_mo, t_gt], writes=[t_ytmp])
                        yield
                        for kc in range(4):
                            c.op("pe", lambda e, kc=kc: e.matmul(mo[:, :], lhsT=ybT[:, kc, :], rhs=wbrb[:, kc, cs], start=(kc == 0), stop=(kc == 3)),
                                 reads=[t_ybT, t_wbrb], writes=[t_mo])
                        c.op("dve", lambda e: e.tensor_tensor(out=mtmp2[:, :], in0=mo[:, :], in1=gt[:, 2048 + cc * 512:2048 + (cc + 1) * 512], op=ALU.mult),
                             reads=[t_mo, t_gt], writes=[t_mtmp2])
                        c.op("dve", lambda e: e.tensor_tensor(out=mrg[:, cs], in0=ytmp[:, :], in1=mtmp2[:, :], op=ALU.add),
                             reads=[t_ytmp, t_mtmp2], wadd=[t_mrg])
                        yield
                    for kc in range(8):
                        c.op("pe", lambda e, kc=kc: e.transpose(m1[:, kc * 128:(kc + 1) * 128], mrg[:, kc * 128:(kc + 1) * 128], ident_bf[:]),
                             reads=[t_mrg, t_ident], writes=[t_m1])
                    c.op("act", lambda e: e.activation(out=mT[:, :, :], in_=m1[:, :].rearrange("p (k t) -> p k t", t=128), func=AF.Copy),
                         reads=[t_m1], writes=[t_mT])
                    yield
                    for cc in range(2):
                        cs = slice(cc * 512, (cc + 1) * 512)
                        for kc in range(8):
                            c.op("pe", lambda e, kc=kc: e.matmul(mo[:, :], lhsT=mT[:, kc, :], rhs=wo[:, kc, cs], start=(kc == 0), stop=(kc == 7)),
                                 reads=[t_mT, t_wo], writes=[t_mo])
                        c.dma("sp", xt[:, :], x_src[rows, cs], writes=[t_xt])
                        c.op("dve", lambda e: e.tensor_tensor(out=xt[:, :], in0=mo[:, :], in1=xt[:, :], op=ALU.add),
                             reads=[t_mo, t_xt], writes=[t_xt])
                        c.dma("pool", o_dst[rows, cs], xt[:, :], reads=[t_xt])
                        yield

                ucnt = [0]
                pcnt = [0]

                def make_units(i):
                    units = []
                    for br in range(2):
                        for j in range(i + 1):
                            for hf in range(2):
                                units.append((br, j, hf))
                    return units

                def emit_scores(i, u, slot):
                    br, j, hf = u
                    near = j >= i - 1
                    e0 = 128 * (i - j)
                    o = stp[slot][:, :]
                    ts = t_stp[slot]
                    ks = slice(j * 128, (j + 1) * 128)
                    if br == 0:
                        c.op("pe", lambda e: e.matmul(o, lhsT=eall_bf[:, (j // 2) * 128:(j // 2 + 1) * 128],
                                                      rhs=mbT[:, hf * 4:(hf + 1) * 4, :].rearrange("p k t -> p (k t)"),
                                                      start=True, stop=False, skip_group_check=True),
                             reads=[t_eall, t_mbT], writes=[ts])
                        if near:
                            c.op("pe", lambda e: e.matmul(o, lhsT=ident_bf[:, :], rhs=bimg_hi[:, hf * 4:(hf + 1) * 4, e0:e0 + 128],
                                                          start=False, stop=False, skip_group_check=True),
                                 reads=[t_ident, t_bimg], writes=[ts])
                        for hq in range(4):
                            h = hf * 4 + hq
                            c.op("pe", lambda e, h=h, hq=hq: e.matmul(stp[slot][:, hq * 128:(hq + 1) * 128], lhsT=kaT2[:, h // 2, ks],
                                                                      rhs=qpad[:, h // 2, h % 2, :], start=False, stop=(hq == 3),
                                                                      skip_group_check=True),
                                 reads=[t_kaT2, t_qpad], writes=[ts])
                    else:
                        c.op("pe", lambda e: e.matmul(o, lhsT=kbT[:, ks], rhs=qbT[:, hf * 4:(hf + 1) * 4, :].rearrange("p k t -> p (k t)"),
                                                      start=True, stop=False, skip_group_check=True),
                             reads=[t_kbT[j], t_qbT], writes=[ts])
                        if near:
                            c.op("pe", lambda e: e.matmul(o, lhsT=ident_bf[:, :], rhs=bimg_hi[:, 8 + hf * 4:8 + (hf + 1) * 4, e0:e0 + 128],
                                                          start=False, stop=False, skip_group_check=True),
                                 reads=[t_ident, t_bimg], writes=[ts])
                        c.op("pe", lambda e: e.matmul(o, lhsT=mbias1[:, ks], rhs=irep_bf[:, :], start=False, stop=True,
                                                      skip_group_check=True),
                             reads=[t_mbias1, t_irep], writes=[ts])

                def emit_exp(slot, ps):
                    c.op("act", lambda e: e.activation(out=pT[ps][:, :], in_=stp[slot][:, :], func=AF.Exp, scale=0.125),
                         reads=[t_stp[slot]], writes=[t_pT[ps]])

                def emit_pv(i, u, slot):
                    br, j, hf = u
                    for hq in range(4):
                        h = hf * 4 + hq
                        rhs = v1a[:, j, h, :] if br == 0 else v1b[:, j, :]
                        tv = t_v1a[j] if br == 0 else t_v1b[j]
                        c.op("pe", lambda e, hq=hq, rhs=rhs: e.matmul(acc[hf][:, hq * 65:hq * 65 + 65], lhsT=pT[slot][:, hq * 128:(hq + 1) * 128],
                                                                      rhs=rhs, start=(j == 0 and hq == 0), stop=(j == i and hq == 3),
                                                                      skip_group_check=True),
                             reads=[t_pT[slot], tv, t_ones], writes=[t_acc[hf]])
                    if j == i:
                        c.op("act", lambda e: e.activation(out=accs[br][:, hf * 260:(hf + 1) * 260], in_=acc[hf][:, 0:260], func=AF.Copy),
                             reads=[t_acc[hf]], wadd=[t_accs[br]])

                def interleave(gens):
                    gens = list(gens)
                    while gens:
                        for g in list(gens):
                            try:
                                next(g)
                                yield
                            except StopIteration:
                                gens.remove(g)

                def run_all(g):
                    for _ in g:
                        pass

                def bg_schedule(gF, gC, gI, gB):
                    if gF is not None:
                        next(gF)
                        yield
                    if gC is not None:
                        for _ in range(2):
                            next(gC)
                            yield
                    yield from interleave([g for g in (gB, gI, gC) if g is not None])

                run_all(gen_I(0))
                run_all(gen_B(0))
                run_all(gen_I(1))
                prepB(0)
                for i in range(NT):
                    nbg = 1
                    gF = gen_F(i)
                    gC = gI = gB = None
                    if i - 1 >= 0:
                        gC = gen_C(i - 1)
                        nbg += 10
                    if i + 2 < NT:
                        gI = gen_I(i + 2)
                        nbg += 2 + 4 * ((128 * (i + 3) + 511) // 512)
                    if i + 1 < NT:
                        gB = gen_B(i + 1)
                        nbg += 1 + (NBIS if i + 1 >= 2 else 0)
                    bg = bg_schedule(gF, gC, gI, gB)
                    units = make_units(i)
                    nU = len(units)
                    quota = -(-nbg // nU) if nU else nbg
                    slots = [(ucnt[0] + k) % 3 for k in range(nU)]
                    ucnt[0] += nU
                    next(bg)
                    emit_scores(i, units[0], slots[0])
                    for k in range(nU):
                        if k + 1 < nU:
                            emit_scores(i, units[k + 1], slots[k + 1])
                            if k + 2 == nU and i + 1 < NT:
                                if gC is not None:
                                    run_all(gC)
                                prepB(i + 1)
                        emit_exp(slots[k], (pcnt[0] + k) % 2)
                        emit_pv(i, units[k], (pcnt[0] + k) % 2)
                        for _ in range(quota):
                            try:
                                next(bg)
                            except StopIteration:
                                break
                    pcnt[0] += nU
                    for _ in bg:
                        pass
                run_all(gen_C(NT - 1))
                c.barrier()
        c.finish()
    return nc


_CACHE = {}


def kernel(x, norm_g, w_in, q_norm_a, k_norm_a, q_norm_b, k_norm_b, w_branch_a, w_branch_b, w_out, rel_bias):
    if "nc" not in _CACHE:
        _CACHE["nc"] = build_program()
    nc = _CACHE["nc"]
    f = lambda a: np.ascontiguousarray(np.asarray(a, dtype=np.float32))
    shared = {"norm_g": f(norm_g), "w_in": f(w_in), "q_norm_a": f(q_norm_a), "k_norm_a": f(k_norm_a),
              "q_norm_b": f(q_norm_b), "k_norm_b": f(k_norm_b), "w_branch_a": f(w_branch_a),
              "w_branch_b": f(w_branch_b), "w_out": f(w_out), "rel_bias": f(rel_bias)}
    shared.update(_host_consts())
    xs = f(x)
    in_maps = []
    for b in range(8):
        m = dict(shared)
        m["x"] = xs[b]
        in_maps.append(m)
    res = run_bass_kernel_spmd(nc, in_maps, core_ids=list(range(8)))
    return np.stack([np.asarray(r["out"]) for r in res.results], axis=0).astype(np.float32)
```

```python
import math
from contextlib import ExitStack

import numpy as np
import concourse.bass as bass
import concourse.mybir as mybir
from concourse.bass_utils import run_bass_kernel_spmd

F32 = mybir.dt.float32
BF16 = mybir.dt.bfloat16
ALU = mybir.AluOpType
AF = mybir.ActivationFunctionType
AX = mybir.AxisListType

S = 4096
D = 1024
NT = S // 128
DIN = 5572
DEPTH = 2
EPS = 1e-6
NEG = -30000.0
QS_W = 4352
NBIS = 18
W0 = 1024.0
TOPK = 256.0
MID0 = 2.0 ** -13
ACT_SHARE_NUM, ACT_SHARE_DEN = 0, 8

C_QA, C_KA, C_VA, C_GA = 0, 512, 1024, 1536
C_QB, C_KB, C_VB, C_GB = 2048, 2560, 2624, 2688
C_QI, C_KI, C_WI, C_MA, C_MB = 3200, 3456, 3520, 3524, 4548


class T:
    __slots__ = ("w", "r")

    def __init__(self):
        self.w = {}
        self.r = {}


class Ctx:
    RING = 8

    def __init__(self, nc, stack):
        self.nc = nc
        self.eng = {"pe": nc.tensor, "act": nc.scalar, "dve": nc.vector, "pool": nc.gpsimd, "sp": nc.sync}
        self.sem = {}
        self.cnt = {}
        for k in ("pe", "act", "dve", "pool"):
            self.sem[k] = stack.enter_context(nc.semaphore("s_" + k))
            self.cnt[k] = 0
        self.waited = {k: {} for k in self.eng}
        self.rings = {}
        self.ringpos = {}
        for q in ("sp", "pool"):
            self.rings[q] = []
            for i in range(self.RING):
                key = "d_%s_%d" % (q, i)
                self.sem[key] = stack.enter_context(nc.semaphore(key))
                self.cnt[key] = 0
                self.rings[q].append(key)
            self.ringpos[q] = 0

    def _wait(self, e, marks):
        need = {}
        for (k, v) in marks:
            if need.get(k, 0) < v:
                need[k] = v
        wd = self.waited[e]
        for k, v in need.items():
            if wd.get(k, 0) < v:
                self.eng[e].wait_ge(self.sem[k], v)
                wd[k] = v

    def _deps(self, e, reads, writes):
        marks = []
        for t in reads:
            for m in t.w.items():
                if m[0] == e and e == "pe":
                    continue
                marks.append(m)
        for t in writes:
            for m in t.w.items():
                if m[0] == e:
                    continue
                marks.append(m)
            for m in t.r.items():
                if m[0] == e:
                    continue
                marks.append(m)
        return marks

    def op(self, e, fn, reads=(), writes=(), wadd=()):
        self._wait(e, self._deps(e, reads, tuple(writes) + tuple(wadd)))
        ins = fn(self.eng[e])
        self.cnt[e] += 1
        ins.then_inc(self.sem[e], 1)
        v = self.cnt[e]
        for t in reads:
            t.r[e] = v
        for t in writes:
            t.w = {e: v}
            t.r = {}
        for t in wadd:
            t.w[e] = v
        return ins

    def dma(self, q, out, in_, reads=(), writes=(), wadd=(), **kw):
        key = self.rings[q][self.ringpos[q] % self.RING]
        self.ringpos[q] += 1
        marks = self._deps("dma", reads, tuple(writes) + tuple(wadd))
        if self.cnt[key] > 0:
            marks.append((key, self.cnt[key]))
        self._wait(q, marks)
        ins = self.eng[q].dma_start(out=out, in_=in_, **kw)
        self.cnt[key] += 16
        ins.then_inc(self.sem[key], 16)
        v = self.cnt[key]
        for t in reads:
            t.r[key] = v
        for t in writes:
            t.w = {key: v}
            t.r = {}
        for t in wadd:
            t.w[key] = v

    def all_marks(self):
        marks = []
        for k, v in self.cnt.items():
            if v > 0:
                marks.append((k, v))
        return marks

    def barrier(self):
        marks = self.all_marks()
        for e in ("pe", "act", "dve", "pool", "sp"):
            self._wait(e, [m for m in marks if m[0] != e])

    def finish(self):
        self._wait("sp", self.all_marks())


def _bucket_table():
    n = np.arange(0, 256)
    max_exact = 16
    nf = np.maximum(n, 1).astype(np.float32)
    large = max_exact + (np.log(nf / np.float32(max_exact)) / np.float32(math.log(128 / max_exact))
                         * np.float32(32 - max_exact)).astype(np.int32)
    large = np.minimum(large, 31)
    return np.where(n < max_exact, n, large)


def _host_consts():
    bk = _bucket_table()
    grev = np.zeros((33, 384), np.float32)
    for m in range(384):
        dist = 255 - m
        if dist >= 0:
            grev[bk[dist], m] = 1.0
        else:
            grev[32, m] = 1.0
    ident = np.eye(128, dtype=np.float32)
    irep = np.concatenate([ident] * 4, axis=1)
    t = np.arange(128)[:, None]
    s = np.arange(128)[None, :]
    causalneg = np.where(s <= t, 0.0, -1e30).astype(np.float32)
    eall = np.zeros((16, 16, 128), np.float32)
    for n in range(16):
        eall[n, n, :] = 1.0
    negrow = np.full((1, 16), NEG * 8.0, np.float32)
    return {"c_grev": grev, "c_ident": ident, "c_irep": irep, "c_causal": causalneg,
            "c_eall": eall.reshape(16, 2048), "c_negrow": negrow}


def build_program():
    nc = bass.Bass("TRN2", target_bir_lowering=False)
    din = lambda n, s, d=F32: nc.dram_tensor(n, s, d, kind="ExternalInput").ap()
    x = din("x", [S, D])
    norm_g = din("norm_g", [DEPTH, D])
    w_in = din("w_in", [DEPTH, D, DIN])
    qn_a = din("q_norm_a", [DEPTH, 64])
    kn_a = din("k_norm_a", [DEPTH, 64])
    qn_b = din("q_norm_b", [DEPTH, 64])
    kn_b = din("k_norm_b", [DEPTH, 64])
    w_bra = din("w_branch_a", [DEPTH, 512, D])
    w_brb = din("w_branch_b", [DEPTH, 512, D])
    w_out = din("w_out", [DEPTH, D, D])
    rel_bias = din("rel_bias", [32, 16])
    c_grev = din("c_grev", [33, 384])
    c_ident = din("c_ident", [128, 128])
    c_irep = din("c_irep", [128, 512])
    c_causal = din("c_causal", [128, 128])
    c_eall = din("c_eall", [16, 2048])
    c_negrow = din("c_negrow", [1, 16])
    out = nc.dram_tensor("out", [S, D], F32, kind="ExternalOutput").ap()
    scr = lambda n, s, d: nc.dram_tensor(n, s, d, kind="Internal").ap()
    qs = scr("qs", [S, QS_W], BF16)
    wis = scr("wis", [S, 4], F32)
    kaT_s = scr("kaT_s", [4, 128, S], BF16)
    x1 = scr("x1", [S, D], F32)

    top = ExitStack()
    with top:
        c = Ctx(nc, top)

        uid = [0]

        def sbt(st, name, shape, dt):
            uid[0] += 1
            return st.enter_context(nc.sbuf_tensor("%s_%d" % (name, uid[0]), shape, dt))

        def pst(st, name, shape, dt):
            uid[0] += 1
            return st.enter_context(nc.psum_tensor("%s_%d" % (name, uid[0]), shape, dt))

        ident_bf = sbt(top, "ident_bf", [128, 128], BF16); t_ident = T()
        irep_bf = sbt(top, "irep_bf", [128, 512], BF16); t_irep = T()
        causal = sbt(top, "causal", [128, 128], F32); t_causal = T()
        eall_bf = sbt(top, "eall_bf", [128, 2048], BF16); t_eall = T()
        bimg_hi = sbt(top, "bimg_hi", [128, 16, 256], BF16)
        t_bimg = T()
        v1a = sbt(top, "v1a", [128, NT, 8, 65], BF16); t_v1a = [T() for _ in range(NT)]
        v1b = sbt(top, "v1b", [128, NT, 65], BF16); t_v1b = [T() for _ in range(NT)]
        kbT = sbt(top, "kbT", [128, S], BF16); t_kbT = [T() for _ in range(NT)]
        kiT = sbt(top, "kiT", [128, S], BF16); t_kiT = [T() for _ in range(NT)]
        kmacc = sbt(top, "kmacc", [128, 4, 16], F32); t_kmacc = T()

        c.dma("pool", ident_bf[:], c_ident, writes=[t_ident])
        c.dma("pool", irep_bf[:], c_irep, writes=[t_irep])
        c.op("dve", lambda e: e.memset(eall_bf[:, :], 0.0), writes=[t_eall])
        c.dma("pool", eall_bf[0:16, :], c_eall, writes=[t_eall])
        t_kz = T()
        c.op("dve", lambda e: e.memset(kbT[64:128, :], 0.0), writes=[t_kz])
        c.op("dve", lambda e: e.memset(kiT[64:128, :], 0.0), writes=[t_kz])
        c.dma("sp", causal[:], c_causal, writes=[t_causal])
        t_ones = T()
        c.op("dve", lambda e: e.memset(v1a[:, :, :, 64:65], 1.0), writes=[t_ones])
        c.op("dve", lambda e: e.memset(v1b[:, :, 64:65], 1.0), writes=[t_ones])

        with ExitStack() as st:
            grev = sbt(st, "grev", [33, 384], F32); t_grev = T()
            reltab = sbt(st, "reltab", [33, 16], F32); t_rel = T()
            r31 = sbt(st, "r31", [32, 16], F32); t_r31 = T()
            bps = pst(st, "bps", [128, 2048], F32); t_bps = T()
            c.dma("sp", grev[:], c_grev, writes=[t_grev])
            c.dma("sp", reltab[0:32, :], rel_bias, writes=[t_rel])
            c.dma("sp", r31[:], rel_bias[31:32, :].to_broadcast([32, 16]), writes=[t_r31])
            c.op("dve", lambda e: e.tensor_tensor(out=reltab[0:32, :], in0=reltab[0:32, :], in1=r31[:], op=ALU.subtract),
                 reads=[t_r31, t_rel], writes=[t_rel])
            c.op("dve", lambda e: e.tensor_scalar(out=reltab[0:32, :], in0=reltab[0:32, :], scalar1=8.0, scalar2=None, op0=ALU.mult),
                 reads=[t_rel], writes=[t_rel])
            c.dma("sp", reltab[32:33, :], c_negrow, reads=[t_rel], writes=[t_rel])
            for half in range(2):
                for el in range(128):
                    e_ = half * 128 + el
                    c.op("pe", lambda e, el=el, e_=e_: e.matmul(bps[:, el * 16:(el + 1) * 16], lhsT=grev[0:33, 255 - e_:255 - e_ + 128],
                                                                 rhs=reltab[0:33, :], start=True, stop=True),
                         reads=[t_grev, t_rel], writes=[t_bps])
                src = bps[:, :].rearrange("p (e h) -> p h e", h=16)
                hi_v = bimg_hi[:, :, half * 128:(half + 1) * 128]
                c.op("dve", lambda e: e.tensor_copy(out=hi_v, in_=src), reads=[t_bps], wadd=[t_bimg])
            c.barrier()

        for l in range(DEPTH):
            x_src = x if l == 0 else x1
            o_dst = x1 if l == 0 else out
            with ExitStack() as st:
                w_bf = sbt(st, "w_bf", [128, 8, DIN], BF16)
                t_wc = {(c0, kc): T() for c0 in (0, 2048, 4096) for kc in range(8)}
                gbc = sbt(st, "gbc", [128, D], F32); t_gbc = T()
                gq_a = sbt(st, "gq_a", [128, 64], F32)
                gk_a = sbt(st, "gk_a", [128, 64], F32)
                gq_b = sbt(st, "gq_b", [128, 64], F32)
                gk_b = sbt(st, "gk_b", [128, 64], F32)
                t_gs = T()
                xb = [sbt(st, "xb%d" % i, [128, D], F32) for i in range(2)]; t_xb = [T(), T()]
                sqj = sbt(st, "sqj", [128, D], BF16); t_sqj = T()
                stat = sbt(st, "stat", [128, 8], F32); t_stat = T()
                hb = sbt(st, "hb", [128, D], BF16); t_hb = T()
                hT = [sbt(st, "hT%d" % i, [128, 8, 128], BF16) for i in range(2)]; t_hT = [T(), T()]
                qt = [sbt(st, "qt%d" % i, [128, QS_W], BF16) for i in range(2)]; t_qt = [T(), T()]
                sqf = sbt(st, "sqf", [128, 512], F32); t_sqf = T()
                hst = sbt(st, "hst", [128, 32], F32); t_hst = T()
                tmpf = sbt(st, "tmpf", [128, 512], F32); t_tmpf = T()
                kan = sbt(st, "kan", [128, 512], BF16); t_kan = T()
                kTt = [sbt(st, "kTt%d" % i, [128, 4, 128], BF16) for i in range(2)]; t_kTt = [T(), T()]
                kred = sbt(st, "kred", [128, 4], F32); t_kred = T()
                sm = sbt(st, "sm", [128, 128], BF16); t_sm = T()
                wit = [sbt(st, "witp%d" % i, [128, 4], F32) for i in range(2)]; t_wit = [T(), T()]
                tp = pst(st, "tp", [128, 1024], BF16); t_tp = T()
                tp2 = pst(st, "tp2", [128, 1024], BF16); t_tp2 = T()
                pj = [pst(st, "pj%d" % i, [128, 512], F32) for i in range(6)]; t_pj = [T() for _ in range(6)]

                for (c0, c1) in ((0, 2048), (2048, 4096), (4096, DIN)):
                    for kc in range(8):
                        c.dma("pool", w_bf[:, kc, c0:c1], w_in[l, kc * 128:(kc + 1) * 128, c0:c1], wadd=[t_wc[(c0, kc)]])
                c.dma("sp", gbc[:], norm_g[l:l + 1, :].to_broadcast([128, D]), writes=[t_gbc])
                for (g_sb, g_dr) in ((gq_a, qn_a), (gk_a, kn_a), (gq_b, qn_b), (gk_b, kn_b)):
                    c.dma("sp", g_sb[:], g_dr[l:l + 1, :].to_broadcast([128, 64]), wadd=[t_gs])
                c.op("dve", lambda e: e.memset(kmacc[:], 0.0), writes=[t_kmacc])

                pjn = [0]

                def project(i, col0, ncols):
                    b = pjn[0] % 6
                    pjn[0] += 1
                    for kc in range(8):
                        c.op("pe", lambda e, kc=kc: e.matmul(pj[b][:, 0:ncols], lhsT=hT[i % 2][:, kc, :],
                                                              rhs=w_bf[:, kc, col0:col0 + ncols],
                                                              start=(kc == 0), stop=(kc == 7)),
                             reads=[t_hT[i % 2]] + [t_wc[(cc, kc)] for cc in (0, 2048, 4096)
                                                      if cc < col0 + ncols and col0 < min(cc + 2048, DIN)],
                             writes=[t_pj[b]])
                    return pj[b], t_pj[b]

                def rsqrt_small(ap_in, ap_out, n, inv_n):
                    c.op("dve", lambda e: e.tensor_scalar(out=ap_out, in0=ap_in, scalar1=inv_n, scalar2=EPS,
                                                          op0=ALU.mult, op1=ALU.add), reads=[t_hst], writes=[t_hst])
                    c.op("act", lambda e: e.activation(out=ap_out, in_=ap_out, func=AF.Ln), reads=[t_hst], writes=[t_hst])
                    c.op("act", lambda e: e.activation(out=ap_out, in_=ap_out, func=AF.Exp, scale=-0.5),
                         reads=[t_hst], writes=[t_hst])

                def headnorm(ps_ap, t_ps, H, g_sb, out_ap, t_out, wadd_out=False):
                    W = H * 64
                    c.op("act", lambda e: e.activation(out=sqf[:, 0:W], in_=ps_ap, func=AF.Square),
                         reads=[t_ps], writes=[t_sqf])
                    c.op("dve", lambda e: e.tensor_reduce(out=hst[:, 0:H], in_=sqf[:, 0:W].rearrange("p (h d) -> p h d", d=64),
                                                          axis=AX.X, op=ALU.add), reads=[t_sqf], writes=[t_hst])
                    rsqrt_small(hst[:, 0:H], hst[:, 0:H], H, 1.0 / 64.0)
                    c.op("dve", lambda e: e.tensor_tensor(out=tmpf[:, 0:W].rearrange("p (h d) -> p h d", d=64),
                                                          in0=ps_ap.rearrange("p (h d) -> p h d", d=64),
                                                          in1=hst[:, 0:H].unsqueeze(2).to_broadcast([128, H, 64]), op=ALU.mult),
                         reads=[t_ps, t_hst], writes=[t_tmpf])
                    kw = dict(wadd=[t_out]) if wadd_out else dict(writes=[t_out])
                    c.op("dve", lambda e: e.tensor_tensor(out=out_ap.rearrange("p (h d) -> p h d", d=64),
                                                          in0=tmpf[:, 0:W].rearrange("p (h d) -> p h d", d=64),
                                                          in1=g_sb[:, :].unsqueeze(1).to_broadcast([128, H, 64]), op=ALU.mult),
                         reads=[t_tmpf, t_gs], **kw)

                def norm_act(i):
                    rows = slice(i * 128, (i + 1) * 128)
                    xt, t_xt = xb[i % 2], t_xb[i % 2]
                    c.dma("sp", xt[:], x_src[rows, :], writes=[t_xt])
                    c.op("act", lambda e: e.activation(out=sqj[:], in_=xt[:], func=AF.Square, accum_out=stat[:, 0:1]),
                         reads=[t_xt], writes=[t_sqj, t_stat])
                    c.op("dve", lambda e: e.tensor_scalar(out=stat[:, 1:2], in0=stat[:, 0:1], scalar1=1.0 / D, scalar2=EPS,
                                                          op0=ALU.mult, op1=ALU.add), reads=[t_stat], writes=[t_stat])
                    c.op("act", lambda e: e.activation(out=stat[:, 2:3], in_=stat[:, 1:2], func=AF.Ln), reads=[t_stat], writes=[t_stat])
                    c.op("act", lambda e: e.activation(out=stat[:, 3:4], in_=stat[:, 2:3], func=AF.Exp, scale=-0.5),
                         reads=[t_stat], writes=[t_stat])
                    c.op("dve", lambda e: e.scalar_tensor_tensor(out=hb[:], in0=xt[:], scalar=stat[:, 3:4], in1=gbc[:],
                                                                 op0=ALU.mult, op1=ALU.mult),
                         reads=[t_xt, t_stat, t_gbc], writes=[t_hb])

                def norm_pe(i):
                    for kc in range(8):
                        c.op("pe", lambda e, kc=kc: e.transpose(tp[:, kc * 128:(kc + 1) * 128], hb[:, kc * 128:(kc + 1) * 128], ident_bf[:]),
                             reads=[t_hb, t_ident], writes=[t_tp])
                    c.op("act", lambda e: e.activation(out=hT[i % 2][:, :, :], in_=tp[:, :].rearrange("p (k t) -> p k t", t=128), func=AF.Copy),
                         reads=[t_tp], writes=[t_hT[i % 2]])

                norm_act(0)
                norm_pe(0)
                for i in range(NT):
                    rows = slice(i * 128, (i + 1) * 128)
                    q, t_q = qt[i % 2], t_qt[i % 2]
                    kt, t_kt = kTt[i % 2], t_kTt[i % 2]
                    if i + 1 < NT:
                        norm_act(i + 1)
                    ps, tps = project(i, C_QA, 512)
                    headnorm(ps[:, 0:512], tps, 8, gq_a, q[:, 0:512], t_q)
                    ps, tps = project(i, C_KA, 512)
                    headnorm(ps[:, 0:512], tps, 8, gk_a, kan[:, :], t_kan)
                    ps, tps = project(i, C_QB, 512)
                    headnorm(ps[:, 0:512], tps, 8, gq_b, q[:, 512:1024], t_q, wadd_out=True)
                    ps, tps = project(i, C_KB, 128)
                    headnorm(ps[:, 0:64], tps, 1, gk_b, sm[:, 0:64], t_sm)
                    c.op("dve", lambda e: e.tensor_copy(out=v1b[:, i, 0:64], in_=ps[:, 64:128]), reads=[tps], writes=[t_v1b[i]])
                    if i + 1 < NT:
                        norm_pe(i + 1)
                    ps2, tps2 = project(i, C_QI, 324)
                    c.op("dve", lambda e: e.tensor_copy(out=q[:, 4096:4352], in_=ps2[:, 0:256]), reads=[tps2], wadd=[t_q])
                    c.op("dve", lambda e: e.tensor_copy(out=sm[:, 64:128], in_=ps2[:, 256:320]), reads=[tps2], wadd=[t_sm])
                    c.op("dve", lambda e: e.tensor_copy(out=wit[i % 2][:, :], in_=ps2[:, 320:324]), reads=[tps2], writes=[t_wit[i % 2]])
                    c.dma("pool", wis[rows, :], wit[i % 2][:, :], reads=[t_wit[i % 2]])
                    for p4 in range(4):
                        c.op("pe", lambda e, p4=p4: e.transpose(tp2[:, p4 * 128:(p4 + 1) * 128], kan[:, p4 * 128:(p4 + 1) * 128], ident_bf[:]),
                             reads=[t_kan, t_ident], writes=[t_tp2])
                    c.op("act", lambda e: e.activation(out=kt[:, :, :], in_=tp2[:, 0:512].rearrange("p (k t) -> p k t", t=128), func=AF.Copy),
                         reads=[t_tp2], writes=[t_kt])
                    for p4 in range(4):
                        c.dma("pool", kaT_s[p4, :, i * 128:(i + 1) * 128], kt[:, p4, :], reads=[t_kt])
                    c.op("dve", lambda e: e.tensor_reduce(out=kred[:, :], in_=kt[:, :, :], axis=AX.X, op=ALU.add),
                         reads=[t_kt], writes=[t_kred])
                    nblk = i // 2
                    c.op("dve", lambda e: e.tensor_tensor(out=kmacc[:, :, nblk], in0=kmacc[:, :, nblk], in1=kred[:, :], op=ALU.add),
                         reads=[t_kred, t_kmacc], writes=[t_kmacc])
                    ps, tps = project(i, C_VA, 512)
                    c.op("act", lambda e: e.activation(out=v1a[:, i, :, 0:64], in_=ps[:, 0:512].rearrange("p (h d) -> p h d", d=64), func=AF.Copy),
                         reads=[tps], writes=[t_v1a[i]])
                    c.op("pe", lambda e: e.transpose(tp2[0:64, 512:640], sm[:, 0:64], ident_bf[:]), reads=[t_sm, t_ident], writes=[t_tp2])
                    c.op("pe", lambda e: e.transpose(tp2[0:64, 640:768], sm[:, 64:128], ident_bf[:]), reads=[t_sm, t_ident], writes=[t_tp2])
                    c.op("act", lambda e: e.activation(out=kbT[0:64, i * 128:(i + 1) * 128], in_=tp2[0:64, 512:640], func=AF.Copy),
                         reads=[t_tp2], writes=[t_kbT[i]])
                    c.op("act", lambda e: e.activation(out=kiT[0:64, i * 128:(i + 1) * 128], in_=tp2[0:64, 640:768], func=AF.Copy),
                         reads=[t_tp2], writes=[t_kiT[i]])
                    ps, tps = project(i, C_GA, 512)
                    c.op("act", lambda e: e.activation(out=q[:, 1024:1536], in_=ps[:, 0:512], func=AF.Silu), reads=[tps], wadd=[t_q])
                    ps, tps = project(i, C_GB, 512)
                    c.op("act", lambda e: e.activation(out=q[:, 1536:2048], in_=ps[:, 0:512], func=AF.Silu), reads=[tps], wadd=[t_q])
                    for gi, cm in enumerate((C_MA, C_MA + 512, C_MB, C_MB + 512)):
                        ps, tps = project(i, cm, 512)
                        c.op("act", lambda e, gi=gi: e.activation(out=q[:, 2048 + gi * 512:2048 + (gi + 1) * 512], in_=ps[:, 0:512], func=AF.Sigmoid),
                             reads=[tps], wadd=[t_q])
                    c.dma("pool", qs[rows, :], q[:, :], reads=[t_q])
                c.barrier()

            with ExitStack() as st:
                kaT2 = sbt(st, "kaT2", [128, 4, S], BF16); t_kaT2 = T()
                km_bf = sbt(st, "km_bf", [128, 4, 16], BF16); t_km = T()
                wbra = sbt(st, "wbra", [128, 4, D], BF16); t_wbra = T()
                wbrb = sbt(st, "wbrb", [128, 4, D], BF16); t_wbrb = T()
                wo = sbt(st, "wo", [128, 8, D], BF16); t_wo = T()
                qm = sbt(st, "qm", [128, 1024], BF16); t_qm = T()
                qmi = sbt(st, "qmi", [128, 256], BF16); t_qmi = T()
                gt = sbt(st, "gt", [128, 3072], BF16); t_gt = T()
                wit2 = [sbt(st, "wit2%d" % k, [128, 4], F32) for k in range(2)]; t_wit2 = [T(), T()]
                xt = sbt(st, "xt", [128, D], F32); t_xt = T()
                qpad = sbt(st, "qpad", [128, 4, 2, 128], BF16); t_qpad = T()
                qbT = sbt(st, "qbT", [128, 8, 128], BF16); t_qbT = T()
                qiT = [sbt(st, "qiT%d" % k, [128, 4, 128], BF16) for k in range(2)]; t_qiT = [T(), T()]
                score = sbt(st, "score", [128, S], F32); t_score = T()
                mbias = [sbt(st, "mbias%d" % k, [128, S], BF16) for k in range(2)]; t_mbias = [T(), T()]
                rl2 = [sbt(st, "rl%d" % k, [128, 512], F32) for k in range(2)]; t_rl2 = [T(), T()]
                bs = sbt(st, "bs", [128, 8], F32)
                t_cnt = T(); t_tmp = T(); t_mid = T(); t_thr = T()
                sg = sbt(st, "sg", [128, 2], F32); t_sg = T()
                gatem = sbt(st, "gatem", [128, 8, 16], F32); t_gatem = T()
                top8 = sbt(st, "top8", [128, 8, 8], F32); t_top8 = T()
                thr3 = sbt(st, "thr3", [128, 8], F32); t_thr3 = T()
                mbf = sbt(st, "mbf", [128, 8, 16], F32); t_mbf = T()
                mb16 = sbt(st, "mb16", [128, 8, 16], BF16); t_mb16 = T()
                mbT = sbt(st, "mbT", [128, 8, 128], BF16); t_mbT = T()
                pT = [sbt(st, "pT%d" % k, [128, 512], BF16) for k in range(2)]; t_pT = [T(), T()]
                accs = [sbt(st, "accs%d" % k, [128, 520], F32) for k in range(2)]; t_accs = [T(), T()]
                rec = sbt(st, "rec", [128, 8], F32); t_rec = T()
                ytmp = sbt(st, "ytmp", [128, 512], F32); t_ytmp = T()
                yag = sbt(st, "yag", [128, 512], BF16); t_yag = T()
                ybg = sbt(st, "ybg", [128, 512], BF16); t_ybg = T()
                yaT = sbt(st, "yaT", [128, 4, 128], BF16); t_yaT = T()
                ybT = sbt(st, "ybT", [128, 4, 128], BF16); t_ybT = T()
                mtmp2 = rl2[1]; t_mtmp2 = t_rl2[1]
                mrg = sbt(st, "mrg", [128, D], BF16); t_mrg = T()
                mT = sbt(st, "mT", [128, 8, 128], BF16); t_mT = T()
                stp = [pst(st, "stp%d" % k, [128, 512], F32) for k in range(3)]; t_stp = [T(), T(), T()]
                acc = [pst(st, "acc%d" % k, [128, 512], F32) for k in range(2)]; t_acc = [T(), T()]
                m0 = pst(st, "m0", [128, 512], F32); t_m0 = T()
                m1 = pst(st, "m1", [128, 1024], BF16); t_m1 = T()
                mo = pst(st, "mo", [128, 512], F32); t_mo = T()

                for p4 in range(4):
                    c.dma("sp", kaT2[:, p4, :], kaT_s[p4, :, :], wadd=[t_kaT2])
                for kc in range(4):
                    c.dma("pool", wbra[:, kc, :], w_bra[l, kc * 128:(kc + 1) * 128, :], wadd=[t_wbra])
                    c.dma("pool", wbrb[:, kc, :], w_brb[l, kc * 128:(kc + 1) * 128, :], wadd=[t_wbrb])
                for kc in range(8):
                    c.dma("pool", wo[:, kc, :], w_out[l, kc * 128:(kc + 1) * 128, :], wadd=[t_wo])
                c.op("dve", lambda e: e.tensor_scalar(out=km_bf[:, :, :], in0=kmacc[:, :, :], scalar1=1.0 / 256.0, scalar2=None, op0=ALU.mult),
                     reads=[t_kmacc], writes=[t_km])
                c.op("dve", lambda e: e.memset(qpad[:, :, :, :], 0.0), writes=[t_qpad])
                c.op("dve", lambda e: e.memset(qbT[:, :, :], 0.0), writes=[t_qbT])
                c.op("dve", lambda e: e.memset(mbT[:, :, :], 0.0), writes=[t_mbT])
                for k in range(2):
                    c.op("dve", lambda e, k=k: e.memset(qiT[k][:, :, :], 0.0), writes=[t_qiT[k]])

                def rows_of(i):
                    return slice(i * 128, (i + 1) * 128)

                idxn = [0]

                def gen_A(i):
                    rows = rows_of(i)
                    nk = 128 * (i + 1)
                    b = i % 2
                    c.dma("sp", qmi[:, :], qs[rows, 4096:4352], writes=[t_qmi])
                    c.dma("sp", wit2[b][:, :], wis[rows, :], writes=[t_wit2[b]])
                    for h in range(4):
                        c.op("pe", lambda e, h=h: e.transpose(m1[0:64, h * 128:(h + 1) * 128], qmi[:, h * 64:(h + 1) * 64], ident_bf[:]),
                             reads=[t_qmi, t_ident], writes=[t_m1])
                    c.op("act", lambda e: e.activation(out=qiT[b][0:64, :, :], in_=m1[0:64, 0:512].rearrange("p (k t) -> p k t", t=128), func=AF.Copy),
                         reads=[t_m1], wadd=[t_qiT[b]])
                    yield
                    for c0 in range(0, nk, 512):
                        n = min(512, nk - c0)
                        kdeps = [t_kiT[jj] for jj in range(c0 // 128, (c0 + n) // 128)]
                        for pr in range(2):
                            bufs = ((m0, t_m0, rl2[0], t_rl2[0]), (mo, t_mo, rl2[1], t_rl2[1]))
                            for q2 in range(2):
                                hh = 2 * pr + q2
                                pm, t_pm, rl, t_rl = bufs[q2]
                                c.op("pe", lambda e, hh=hh, pm=pm: e.matmul(pm[:, 0:n], lhsT=qiT[b][:, hh, :], rhs=kiT[:, c0:c0 + n], start=True, stop=True),
                                     reads=[t_qiT[b]] + kdeps, writes=[t_pm])
                            for q2 in range(2):
                                pm, t_pm, rl, t_rl = bufs[q2]
                                c.op("act", lambda e, pm=pm, rl=rl: e.activation(out=rl[:, 0:n], in_=pm[:, 0:n], func=AF.Relu),
                                     reads=[t_pm], writes=[t_rl])
                            for q2 in range(2):
                                hh = 2 * pr + q2
                                pm, t_pm, rl, t_rl = bufs[q2]
                                if hh == 0:
                                    c.op("dve", lambda e, rl=rl: e.tensor_scalar(out=score[:, c0:c0 + n], in0=rl[:, 0:n], scalar1=wit2[b][:, 0:1],
                                                                                 scalar2=None, op0=ALU.mult),
                                         reads=[t_rl, t_wit2[b]], writes=[t_score])
                                else:
                                    c.op("dve", lambda e, hh=hh, rl=rl: e.scalar_tensor_tensor(out=score[:, c0:c0 + n], in0=rl[:, 0:n],
                                                                                              scalar=wit2[b][:, hh:hh + 1], in1=score[:, c0:c0 + n],
                                                                                              op0=ALU.mult, op1=ALU.add),
                                         reads=[t_rl, t_wit2[b], t_score], writes=[t_score])
                            yield
                    c.op("dve", lambda e: e.tensor_tensor(out=score[:, i * 128:(i + 1) * 128], in0=score[:, i * 128:(i + 1) * 128],
                                                          in1=causal[:, :], op=ALU.add),
                         reads=[t_score, t_causal], writes=[t_score])
                    if i >= 2:
                        n_act = 128 * (((i + 1) * ACT_SHARE_NUM) // ACT_SHARE_DEN)
                        n1 = nk - n_act
                        c.op("dve", lambda e: e.memset(bs[:, 2:3], MID0), writes=[t_mid])
                        w = W0
                        for k in range(NBIS):
                            c.op("dve", lambda e: e.tensor_scalar(out=mbias[b][:, 0:n1], in0=score[:, 0:n1], scalar1=bs[:, 2:3], scalar2=None,
                                                                  op0=ALU.is_ge, op1=ALU.add, accum_out=bs[:, 0:1]),
                                 reads=[t_score, t_mid], writes=[t_cnt], wadd=[t_mbias[b]])
                            if n_act > 0:
                                c.op("act", lambda e: e.activation(out=mbias[b][:, n1:nk], in_=score[:, n1:nk], func=AF.Sign,
                                                                   bias=bs[:, 2:3], scale=-1.0, accum_out=sg[:, 0:1]),
                                     reads=[t_score, t_mid], writes=[t_sg], wadd=[t_mbias[b]])
                                c.op("dve", lambda e: e.scalar_tensor_tensor(out=bs[:, 4:5], in0=bs[:, 0:1], scalar=2.0, in1=sg[:, 0:1],
                                                                             op0=ALU.mult, op1=ALU.subtract),
                                     reads=[t_cnt, t_sg], writes=[t_tmp])
                                c.op("dve", lambda e: e.tensor_scalar(out=bs[:, 1:2], in0=bs[:, 4:5], scalar1=2.0 * TOPK - n_act, scalar2=0.5,
                                                                      op0=ALU.is_ge, op1=ALU.subtract), reads=[t_tmp], writes=[t_tmp])
                            else:
                                c.op("dve", lambda e: e.tensor_scalar(out=bs[:, 1:2], in0=bs[:, 0:1], scalar1=TOPK, scalar2=0.5,
                                                                      op0=ALU.is_ge, op1=ALU.subtract), reads=[t_cnt], writes=[t_tmp])
                            c.op("dve", lambda e, w=w: e.scalar_tensor_tensor(out=bs[:, 2:3], in0=bs[:, 1:2], scalar=w, in1=bs[:, 2:3],
                                                                              op0=ALU.mult, op1=ALU.add), reads=[t_tmp, t_mid], writes=[t_mid])
                            w = w / 2.0
                            yield
                        c.op("dve", lambda e, w=w: e.tensor_scalar(out=bs[:, 3:4], in0=bs[:, 2:3], scalar1=-w, scalar2=None, op0=ALU.add),
                             reads=[t_mid], writes=[t_thr])
                    else:
                        c.op("dve", lambda e: e.memset(bs[:, 3:4], -1e29), writes=[t_thr])
                    c.op("dve", lambda e: e.tensor_scalar(out=mbias[b][:, 0:nk], in0=score[:, 0:nk], scalar1=bs[:, 3:4], scalar2=NEG,
                                                          op0=ALU.is_lt, op1=ALU.mult), reads=[t_score, t_thr], writes=[t_mbias[b]])
                    yield

                def prepB(i):
                    rows = rows_of(i)
                    qblk = i // 2
                    c.dma("sp", qm[:, :], qs[rows, 0:1024], writes=[t_qm])
                    for p4 in range(4):
                        c.op("pe", lambda e, p4=p4: e.transpose(m1[:, p4 * 128:(p4 + 1) * 128], qm[:, p4 * 128:(p4 + 1) * 128], ident_bf[:]),
                             reads=[t_qm, t_ident], writes=[t_m1])
                    c.op("act", lambda e: e.activation(out=qpad[0:64, :, 0, :], in_=m1[0:64, 0:512].rearrange("p (k t) -> p k t", t=128), func=AF.Copy),
                         reads=[t_m1], wadd=[t_qpad])
                    c.op("act", lambda e: e.activation(out=qpad[64:128, :, 1, :], in_=m1[64:128, 0:512].rearrange("p (k t) -> p k t", t=128), func=AF.Copy),
                         reads=[t_m1], wadd=[t_qpad])
                    for h in range(8):
                        c.op("pe", lambda e, h=h: e.transpose(m1[0:64, h * 128:(h + 1) * 128], qm[:, 512 + h * 64:512 + (h + 1) * 64], ident_bf[:]),
                             reads=[t_qm, t_ident], writes=[t_m1])
                    c.op("act", lambda e: e.activation(out=qbT[0:64, :, :], in_=m1[0:64, :].rearrange("p (k t) -> p k t", t=128), func=AF.Copy),
                         reads=[t_m1], wadd=[t_qbT])
                    for h in range(8):
                        c.op("pe", lambda e, h=h: e.matmul(m0[:, h * 16:(h + 1) * 16], lhsT=qpad[:, h // 2, h % 2, :], rhs=km_bf[:, h // 2, :],
                                                           start=True, stop=True),
                             reads=[t_qpad, t_km], writes=[t_m0])
                    c.op("dve", lambda e: e.tensor_copy(out=gatem[:, :, :], in_=m0[:, 0:128].rearrange("p (h n) -> p h n", n=16)),
                         reads=[t_m0], writes=[t_gatem])
                    c.op("dve", lambda e: e.memset(gatem[:, :, qblk:16], -1e30), reads=[t_gatem], writes=[t_gatem])
                    for h in range(8):
                        c.op("dve", lambda e, h=h: e.max(out=top8[:, h, :], in_=gatem[:, h, :]), reads=[t_gatem], wadd=[t_top8])
                    c.op("dve", lambda e: e.tensor_scalar(out=thr3[:, :].unsqueeze(2), in0=top8[:, :, 2:3], scalar1=-1e29, scalar2=None, op0=ALU.max),
                         reads=[t_top8], writes=[t_thr3])
                    c.op("dve", lambda e: e.tensor_tensor(out=mbf[:, :, :], in0=gatem[:, :, :],
                                                          in1=thr3[:, :].unsqueeze(2).to_broadcast([128, 8, 16]), op=ALU.is_lt),
                         reads=[t_gatem, t_thr3, t_top8], writes=[t_mbf])
                    c.op("dve", lambda e: e.tensor_scalar(out=mb16[:, :, :], in0=mbf[:, :, :], scalar1=NEG, scalar2=None, op0=ALU.mult),
                         reads=[t_mbf], writes=[t_mb16])
                    c.op("dve", lambda e: e.memset(mb16[:, :, qblk:qblk + 1], 0.0), reads=[t_mb16], writes=[t_mb16])
                    for h in range(8):
                        c.op("pe", lambda e, h=h: e.transpose(m1[0:16, h * 128:(h + 1) * 128], mb16[:, h, :], ident_bf[:]),
                             reads=[t_mb16, t_ident], writes=[t_m1])
                    c.op("act", lambda e: e.activation(out=mbT[0:16, :, :], in_=m1[0:16, :].rearrange("p (k t) -> p k t", t=128), func=AF.Copy),
                         reads=[t_m1], wadd=[t_mbT])

                def gen_C(i):
                    rows = rows_of(i)
                    c.dma("sp", gt[:, :], qs[rows, 1024:4096], writes=[t_gt])
                    c.dma("sp", xt[:, :], x_src[rows, :], writes=[t_xt])
                    for (br, g0, y_out, t_y) in ((0, 0, yag, t_yag), (1, 512, ybg, t_ybg)):
                        av = accs[br][:, :].rearrange("p (h d) -> p h d", d=65)
                        c.op("dve", lambda e: e.reciprocal(out=rec[:, :].unsqueeze(2), in_=av[:, :, 64:65]),
                             reads=[t_accs[br]], writes=[t_rec])
                        c.op("dve", lambda e: e.tensor_tensor(out=ytmp[:, :].rearrange("p (h d) -> p h d", d=64), in0=av[:, :, 0:64],
                                                              in1=rec[:, :].unsqueeze(2).to_broadcast([128, 8, 64]), op=ALU.mult),
                             reads=[t_accs[br], t_rec], writes=[t_ytmp])
                        c.op("dve", lambda e: e.tensor_tensor(out=y_out[:, :], in0=ytmp[:, :], in1=gt[:, g0:g0 + 512], op=ALU.mult),
                             reads=[t_ytmp, t_gt], writes=[t_y])
                        yield
                    for (ysrc, t_ys, yT, t_yT) in ((yag, t_yag, yaT, t_yaT), (ybg, t_ybg, ybT, t_ybT)):
                        for kc in range(4):
                            c.op("pe", lambda e, kc=kc, ysrc=ysrc: e.transpose(m1[:, kc * 128:(kc + 1) * 128], ysrc[:, kc * 128:(kc + 1) * 128], ident_bf[:]),
                                 reads=[t_ys, t_ident], writes=[t_m1])
                        c.op("act", lambda e, yT=yT: e.activation(out=yT[:, :, :], in_=m1[:, 0:512].rearrange("p (k t) -> p k t", t=128), func=AF.Copy),
                             reads=[t_m1], writes=[t_yT])
                        yield
                    for cc in range(2):
                        cs = slice(cc * 512, (cc + 1) * 512)
                        for kc in range(4):
                            c.op("pe", lambda e, kc=kc: e.matmul(mo[:, :], lhsT=yaT[:, kc, :], rhs=wbra[:, kc, cs], start=(kc == 0), stop=(kc == 3)),
                                 reads=[t_yaT, t_wbra], writes=[t_mo])
                        c.op("dve", lambda e: e.tensor_tensor(out=ytmp[:, :], in0=mo[:, :], in1=gt[:, 1024 + cc * 512:1024 + (cc + 1) * 512], op=ALU.mult),
                             reads=[t_mo, t_gt], writes=[t_ytmp])
                        yield
                        for kc in range(4):
                            c.op("pe", lambda e, kc=kc: e.matmul(mo[:, :], lhsT=ybT[:, kc, :], rhs=wbrb[:, kc, cs], start=(kc == 0), stop=(kc == 3)),
                                 reads=[t_ybT, t_wbrb], writes=[t_mo])
                        c.op("dve", lambda e: e.tensor_tensor(out=mtmp2[:, :], in0=mo[:, :], in1=gt[:, 2048 + cc * 512:2048 + (cc + 1) * 512], op=ALU.mult),
                             reads=[t_mo, t_gt], writes=[t_mtmp2])
                        c.op("dve", lambda e: e.tensor_tensor(out=mrg[:, cs], in0=ytmp[:, :], in1=mtmp2[:, :], op=ALU.add),
                             reads=[t_ytmp, t_mtmp2], wadd=[t_mrg])
                        yield
                    for kc in range(8):
                        c.op("pe", lambda e, kc=kc: e.transpose(m1[:, kc * 128:(kc + 1) * 128], mrg[:, kc * 128:(kc + 1) * 128], ident_bf[:]),
                             reads=[t_mrg, t_ident], writes=[t_m1])
                    c.op("act", lambda e: e.activation(out=mT[:, :, :], in_=m1[:, :].rearrange("p (k t) -> p k t", t=128), func=AF.Copy),
                         reads=[t_m1], writes=[t_mT])
                    yield
                    for cc in range(2):
                        cs = slice(cc * 512, (cc + 1) * 512)
                        for kc in range(8):
                            c.op("pe", lambda e, kc=kc: e.matmul(mo[:, :], lhsT=mT[:, kc, :], rhs=wo[:, kc, cs], start=(kc == 0), stop=(kc == 7)),
                                 reads=[t_mT, t_wo], writes=[t_mo])
                        c.op("dve", lambda e: e.tensor_tensor(out=xt[:, cs], in0=mo[:, :], in1=xt[:, cs], op=ALU.add),
                             reads=[t_mo], wadd=[t_xt])
                        yield
                    c.dma("pool", o_dst[rows, :], xt[:, :], reads=[t_xt])
                    yield

                ucnt = [0]
                pcnt = [0]

                def make_units(i):
                    units = []
                    for br in range(2):
                        for j in range(i + 1):
                            for hf in range(2):
                                units.append((br, j, hf))
                    return units

                def emit_scores(i, u, slot):
                    br, j, hf = u
                    near = j >= i - 1
                    e0 = 128 * (i - j)
                    o = stp[slot][:, :]
                    ts = t_stp[slot]
                    ks = slice(j * 128, (j + 1) * 128)
                    if br == 0:
                        c.op("pe", lambda e: e.matmul(o, lhsT=eall_bf[:, (j // 2) * 128:(j // 2 + 1) * 128],
                                                      rhs=mbT[:, hf * 4:(hf + 1) * 4, :].rearrange("p k t -> p (k t)"),
                                                      start=True, stop=False, skip_group_check=True),
                             reads=[t_eall, t_mbT], writes=[ts])
                        if near:
                            c.op("pe", lambda e: e.matmul(o, lhsT=ident_bf[:, :], rhs=bimg_hi[:, hf * 4:(hf + 1) * 4, e0:e0 + 128],
                                                          start=False, stop=False, skip_group_check=True),
                                 reads=[t_ident, t_bimg], writes=[ts])
                        for hq in range(4):
                            h = hf * 4 + hq
                            c.op("pe", lambda e, h=h, hq=hq: e.matmul(stp[slot][:, hq * 128:(hq + 1) * 128], lhsT=kaT2[:, h // 2, ks],
                                                                      rhs=qpad[:, h // 2, h % 2, :], start=False, stop=(hq == 3),
                                                                      skip_group_check=True),
                                 reads=[t_kaT2, t_qpad], writes=[ts])
                    else:
                        c.op("pe", lambda e: e.matmul(o, lhsT=kbT[:, ks], rhs=qbT[:, hf * 4:(hf + 1) * 4, :].rearrange("p k t -> p (k t)"),
                                                      start=True, stop=False, skip_group_check=True),
                             reads=[t_kbT[j], t_qbT], writes=[ts])
                        if near:
                            c.op("pe", lambda e: e.matmul(o, lhsT=ident_bf[:, :], rhs=bimg_hi[:, 8 + hf * 4:8 + (hf + 1) * 4, e0:e0 + 128],
                                                          start=False, stop=False, skip_group_check=True),
                                 reads=[t_ident, t_bimg], writes=[ts])
                        c.op("pe", lambda e: e.matmul(o, lhsT=mbias[i % 2][:, ks], rhs=irep_bf[:, :], start=False, stop=True,
                                                      skip_group_check=True),
                             reads=[t_mbias[i % 2], t_irep], writes=[ts])

                def emit_exp(slot, ps):
                    c.op("act", lambda e: e.activation(out=pT[ps][:, :], in_=stp[slot][:, :], func=AF.Exp, scale=0.125),
                         reads=[t_stp[slot]], writes=[t_pT[ps]])

                def emit_pv(i, u, slot):
                    br, j, hf = u
                    for hq in range(4):
                        h = hf * 4 + hq
                        rhs = v1a[:, j, h, :] if br == 0 else v1b[:, j, :]
                        tv = t_v1a[j] if br == 0 else t_v1b[j]
                        c.op("pe", lambda e, hq=hq, rhs=rhs: e.matmul(acc[hf][:, hq * 65:hq * 65 + 65], lhsT=pT[slot][:, hq * 128:(hq + 1) * 128],
                                                                      rhs=rhs, start=(j == 0 and hq == 0), stop=(j == i and hq == 3),
                                                                      skip_group_check=True),
                             reads=[t_pT[slot], tv, t_ones], writes=[t_acc[hf]])
                    if j == i:
                        c.op("act", lambda e: e.activation(out=accs[br][:, hf * 260:(hf + 1) * 260], in_=acc[hf][:, 0:260], func=AF.Copy),
                             reads=[t_acc[hf]], wadd=[t_accs[br]])

                def interleave(gens):
                    gens = list(gens)
                    while gens:
                        for g in list(gens):
                            try:
                                next(g)
                                yield
                            except StopIteration:
                                gens.remove(g)

                def bg_schedule(gA, gC, nidx):
                    if gC is not None:
                        for _ in range(2):
                            next(gC)
                            yield
                    if gA is not None:
                        for k in range(nidx):
                            try:
                                next(gA)
                                if k % 2 == 0:
                                    yield
                            except StopIteration:
                                break
                    yield from interleave([g for g in (gA, gC) if g is not None])

                def count_A(i):
                    nk = 128 * (i + 1)
                    return 3 + ((nk + 511) // 512) + (NBIS if i >= 2 else 0)

                for _ in gen_A(0):
                    pass
                prepB(0)
                for i in range(NT):
                    nbg = 0
                    gC = gA = None
                    if i - 1 >= 0:
                        gC = gen_C(i - 1)
                        nbg += 12
                    if i + 1 < NT:
                        gA = gen_A(i + 1)
                        nbg += count_A(i + 1)
                    bg = bg_schedule(gA, gC, 1 + 2 * ((128 * (i + 2) + 511) // 512))
                    units = make_units(i)
                    nU = len(units)
                    quota = -(-nbg // nU) if nU else nbg
                    slots = [(ucnt[0] + k) % 3 for k in range(nU)]
                    ucnt[0] += nU
                    emit_scores(i, units[0], slots[0])
                    for k in range(nU):
                        if k + 1 < nU:
                            emit_scores(i, units[k + 1], slots[k + 1])
                            if k + 2 == nU and i + 1 < NT:
                                prepB(i + 1)
                        emit_exp(slots[k], (pcnt[0] + k) % 2)
                        emit_pv(i, units[k], (pcnt[0] + k) % 2)
                        for _ in range(quota):
                            try:
                                next(bg)
                            except StopIteration:
                                break
                    pcnt[0] += nU
                    for _ in bg:
                        pass
                for _ in gen_C(NT - 1):
                    pass
                c.barrier()
        c.finish()
    return nc


_CACHE = {}


def kernel(x, norm_g, w_in, q_norm_a, k_norm_a, q_norm_b, k_norm_b, w_branch_a, w_branch_b, w_out, rel_bias):
    if "nc" not in _CACHE:
        _CACHE["nc"] = build_program()
    nc = _CACHE["nc"]
    f = lambda a: np.ascontiguousarray(np.asarray(a, dtype=np.float32))
    shared = {"norm_g": f(norm_g), "w_in": f(w_in), "q_norm_a": f(q_norm_a), "k_norm_a": f(k_norm_a),
              "q_norm_b": f(q_norm_b), "k_norm_b": f(k_norm_b), "w_branch_a": f(w_branch_a),
              "w_branch_b": f(w_branch_b), "w_out": f(w_out), "rel_bias": f(rel_bias)}
    shared.update(_host_consts())
    xs = f(x)
    in_maps = []
    for b in range(8):
        m = dict(shared)
        m["x"] = xs[b]
        in_maps.append(m)
    res = run_bass_kernel_spmd(nc, in_maps, core_ids=list(range(8)))
    return np.stack([np.asarray(r["out"]) for r in res.results], axis=0).astype(np.float32)
```
